# Optimizing a Trainium2 kernel written in Bass

```python
import numpy as np
import jax
import jax.numpy as jnp
from jax import lax

D_MODEL = 1024
BATCH = 8
SEQ = 4096
DEPTH = 2

GRID_W = 64
CTX_LEN = 256
EPS = 1e-6
N_DIR = 2

D_CONV = D_MODEL // 4
D_MLSTM = D_MODEL // 2
D_LRU = D_MODEL // 4
D_MIX = D_CONV + D_MLSTM + D_LRU
CONV_WIDTH = 31
MLSTM_HEADS = 4
MLSTM_HD = D_MLSTM // MLSTM_HEADS
MLSTM_CHUNK = 128
LRU_HEADS = 4
LRU_BW = D_LRU // LRU_HEADS
LRU_CONV_WIDTH = 4
LRU_C = 8.0
SPLITS = (D_CONV, D_CONV, D_MLSTM, D_MLSTM, D_MLSTM, D_MLSTM,
          N_DIR * MLSTM_HEADS, N_DIR * MLSTM_HEADS, D_LRU, D_LRU)
IN_COLS = sum(SPLITS)
N_GROUPS = 4
EXPERTS_PER_GROUP = 4
N_EXPERTS = N_GROUPS * EXPERTS_PER_GROUP
TOP_K = 2
D_EXPERT = 512

kernel_name = 'hybrid_conv_mlstm_rglru_hmoe_dit'


def _standardize(x):
    mu = jnp.mean(x, axis=-1, keepdims=True)
    xc = x - mu
    return xc * lax.rsqrt(jnp.mean(xc * xc, axis=-1, keepdims=True) + EPS)


def rms_norm(x, g):
    xf = x.astype(jnp.float32)
    y = xf * lax.rsqrt(jnp.mean(xf * xf, axis=-1, keepdims=True) + EPS)
    return (y * g.astype(jnp.float32)).astype(x.dtype)


def layer_norm(x, g, b):
    y = _standardize(x.astype(jnp.float32)) * g.astype(jnp.float32) + b.astype(jnp.float32)
    return y.astype(x.dtype)


def _rev(a, d, axis):
    return jnp.flip(a, axis=axis) if d == 1 else a


def to_scan_order(a, rows, layer):
    if layer % 2 == 0:
        return a
    b, t, d = a.shape
    return a.reshape(b, rows, GRID_W, d).transpose(0, 2, 1, 3).reshape(b, t, d)


def from_scan_order(a, rows, layer):
    if layer % 2 == 0:
        return a
    b, t, d = a.shape
    return a.reshape(b, GRID_W, rows, d).transpose(0, 2, 1, 3).reshape(b, t, d)


def depthwise_conv(x, w, b, pad):
    y = lax.conv_general_dilated(x, w[:, None, :].astype(x.dtype), (1,), [pad],
                                 dimension_numbers=('NWC', 'WIO', 'NWC'),
                                 feature_group_count=x.shape[-1])
    return y + b.astype(x.dtype)


def conformer_conv(val, gate, w, b, ln_g, ln_b):
    u = val * jax.nn.sigmoid(gate)
    u = depthwise_conv(u, w, b, (CONV_WIDTH // 2, CONV_WIDTH // 2))
    return jax.nn.silu(layer_norm(u, ln_g, ln_b))


def mlstm_chunkwise(q, k, v, ig, fg, state):
    bsz, nh, t, dh = q.shape
    nc = t // MLSTM_CHUNK

    def chunks(a):
        return jnp.moveaxis(a.reshape(bsz, nh, nc, MLSTM_CHUNK, *a.shape[3:]), 2, 0)

    logf = jax.nn.log_sigmoid(fg)
    k = k * (dh ** -0.5)
    mask = jnp.tril(jnp.ones((MLSTM_CHUNK, MLSTM_CHUNK), dtype=bool))

    def step(carry, inp):
        cmat, nvec, m = carry
        qc, kc, vc, ic, lf = inp
        b = jnp.cumsum(lf, axis=-1)
        log_d = jnp.where(mask, b[..., :, None] - b[..., None, :] + ic[..., None, :], -jnp.inf)
        m_inter = b + m[..., None]
        m_t = jnp.maximum(m_inter, jnp.max(log_d, axis=-1))
        dmat = jnp.exp(log_d - m_t[..., None])
        inter = jnp.exp(m_inter - m_t)
        s = jnp.einsum('bhtd,bhsd->bhts', qc, kc) * dmat
        num = jnp.einsum('bhts,bhsd->bhtd', s, vc) + inter[..., None] * jnp.einsum('bhtk,bhkv->bhtv', qc, cmat)
        den = jnp.sum(s, axis=-1) + inter * jnp.einsum('bhtk,bhk->bht', qc, nvec)
        h = num / jnp.maximum(jnp.abs(den), jnp.exp(-m_t))[..., None]
        b_last = b[..., -1]
        log_w = b_last[..., None] - b + ic
        m_new = jnp.maximum(b_last + m, jnp.max(log_w, axis=-1))
        wgt = jnp.exp(log_w - m_new[..., None])
        decay = jnp.exp(b_last + m - m_new)
        cmat = decay[..., None, None] * cmat + jnp.einsum('bhs,bhsk,bhsv->bhkv', wgt, kc, vc)
        nvec = decay[..., None] * nvec + jnp.einsum('bhs,bhsk->bhk', wgt, kc)
        return (cmat, nvec, m_new), h

    state, h = lax.scan(step, state, (chunks(q), chunks(k), chunks(v), chunks(ig), chunks(logf)))
    return jnp.moveaxis(h, 0, 2).reshape(bsz, nh, t, dh), state


def mlstm_group(z_ctx, z_lat, b_i, b_f, norm_g):
    def heads(a):
        b, t, _ = a.shape
        return a.astype(jnp.float32).reshape(b, t, MLSTM_HEADS, MLSTM_HD).transpose(0, 2, 1, 3)

    def gate(a, d, bias):
        b, t, _ = a.shape
        g = a.astype(jnp.float32).reshape(b, t, N_DIR, MLSTM_HEADS)[:, :, d] + bias[d]
        return g.transpose(0, 2, 1)

    def finish(h, o):
        b, _, t, _ = h.shape
        hn = _standardize(h) * norm_g.astype(jnp.float32).reshape(MLSTM_HEADS, 1, MLSTM_HD)
        hn = hn.transpose(0, 2, 1, 3).reshape(b, t, D_MLSTM)
        return (jax.nn.sigmoid(o.astype(jnp.float32)) * hn).astype(o.dtype)

    qc, kc, vc = (heads(a) for a in z_ctx[:3])
    ql, kl, vl = (heads(a) for a in z_lat[:3])
    bsz = qc.shape[0]
    h_ctx = jnp.zeros_like(qc)
    h_lat = jnp.zeros_like(ql)
    for d in range(N_DIR):
        state0 = (jnp.zeros((bsz, MLSTM_HEADS, MLSTM_HD, MLSTM_HD), jnp.float32),
                  jnp.zeros((bsz, MLSTM_HEADS, MLSTM_HD), jnp.float32),
                  jnp.zeros((bsz, MLSTM_HEADS), jnp.float32))
        out_c, state_c = mlstm_chunkwise(_rev(qc, d, 2), _rev(kc, d, 2), _rev(vc, d, 2),
                                         _rev(gate(z_ctx[4], d, b_i), d, 2),
                                         _rev(gate(z_ctx[5], d, b_f), d, 2), state0)
        out_l, _ = mlstm_chunkwise(_rev(ql, d, 2), _rev(kl, d, 2), _rev(vl, d, 2),
                                   _rev(gate(z_lat[4], d, b_i), d, 2),
                                   _rev(gate(z_lat[5], d, b_f), d, 2), state_c)
        h_ctx = h_ctx + _rev(out_c, d, 2)
        h_lat = h_lat + _rev(out_l, d, 2)
    return finish(h_ctx, z_ctx[3]), finish(h_lat, z_lat[3])


def rglru_scan(x, conv_w, conv_b, w_a, b_a, w_x, b_x, lam, h0):
    xc = depthwise_conv(x, conv_w, conv_b, (LRU_CONV_WIDTH - 1, 0)).astype(jnp.float32)
    bsz, t, _ = xc.shape
    xb = xc.reshape(bsz, t, LRU_HEADS, LRU_BW)
    r = jax.nn.sigmoid(jnp.einsum('btnc,ncd->btnd', xb, w_a.astype(jnp.float32)).reshape(bsz, t, D_LRU) + b_a)
    i = jax.nn.sigmoid(jnp.einsum('btnc,ncd->btnd', xb, w_x.astype(jnp.float32)).reshape(bsz, t, D_LRU) + b_x)
    log_a = -LRU_C * r * jax.nn.softplus(-lam.astype(jnp.float32))
    a = jnp.exp(log_a)
    u = jnp.sqrt(-jnp.expm1(2.0 * log_a)) * (i * xc)

    def combine(left, right):
        a1, b1 = left
        a2, b2 = right
        return a1 * a2, a2 * b1 + b2

    a_cum, h = lax.associative_scan(combine, (a, u), axis=1)
    h = h + a_cum * h0[:, None, :]
    return h, h[:, -1]


def rglru_group(x_ctx, g_ctx, x_lat, g_lat, conv_w, conv_b, w_a, b_a, w_x, b_x, lam):
    bsz = x_ctx.shape[0]
    h_ctx = jnp.zeros(x_ctx.shape, jnp.float32)
    h_lat = jnp.zeros(x_lat.shape, jnp.float32)
    for d in range(N_DIR):
        p = (conv_w[d], conv_b[d], w_a[d], b_a[d], w_x[d], b_x[d], lam[d])
        h0 = jnp.zeros((bsz, D_LRU), jnp.float32)
        out_c, state_c = rglru_scan(_rev(x_ctx, d, 1), *p, h0)
        out_l, _ = rglru_scan(_rev(x_lat, d, 1), *p, state_c)
        h_ctx = h_ctx + _rev(out_c, d, 1)
        h_lat = h_lat + _rev(out_l, d, 1)
    y_ctx = h_ctx * jax.nn.gelu(g_ctx.astype(jnp.float32))
    y_lat = h_lat * jax.nn.gelu(g_lat.astype(jnp.float32))
    return y_ctx.astype(x_ctx.dtype), y_lat.astype(x_lat.dtype)


def hier_moe(h, w_rg, b_rg, w_re, b_re, w_gate, w_up, w_down):
    shp = h.shape
    hf = h.reshape(-1, shp[-1])
    n = hf.shape[0]
    g_logit = (hf @ w_rg).astype(jnp.float32) + b_rg
    g_prob = jax.nn.softmax(g_logit, axis=-1)
    g_sel = jnp.argmax(g_logit, axis=-1)
    e_logit = ((hf @ w_re).astype(jnp.float32) + b_re).reshape(n, N_GROUPS, EXPERTS_PER_GROUP)
    e_logit = jnp.take_along_axis(e_logit, g_sel[:, None, None], axis=1)[:, 0]
    top_v, top_i = lax.top_k(e_logit, TOP_K)
    top_w = jax.nn.softmax(top_v, axis=-1) * jnp.take_along_axis(g_prob, g_sel[:, None], axis=1)
    expert_id = g_sel[:, None] * EXPERTS_PER_GROUP + top_i
    combine = jnp.sum(jax.nn.one_hot(expert_id, N_EXPERTS, dtype=jnp.float32) * top_w[..., None], axis=1)
    y = jnp.zeros(hf.shape, jnp.float32)
    for e in range(N_EXPERTS):
        ye = (jax.nn.silu(hf @ w_gate[e]) * (hf @ w_up[e])) @ w_down[e]
        y = y + combine[:, e:e + 1] * ye
    return y.astype(h.dtype).reshape(shp)


def setup_inputs(seed: int = 0) -> dict:
    key = jax.random.key(seed)
    ks = iter(jax.random.split(key, 48))

    def nrm(shape, s):
        return jax.random.normal(next(ks), shape, jnp.float32) * s

    L = DEPTH
    u = jax.random.uniform(next(ks), (L, N_DIR, D_LRU), jnp.float32, 0.9, 0.999)
    return {
        'x': nrm((BATCH, SEQ, D_MODEL), 1.0),
        'c': nrm((BATCH, D_MODEL), 1.0),
        'ctx': nrm((BATCH, CTX_LEN, D_MODEL), 1.0),
        'c_ctx': nrm((D_MODEL,), 1.0),
        'w_mod': nrm((L, D_MODEL, 6 * D_MODEL), 0.5 * D_MODEL ** -0.5),
        'b_mod': nrm((L, 6 * D_MODEL), 0.02),
        'norm1_g': 1.0 + nrm((L, D_MODEL), 0.02),
        'norm2_g': 1.0 + nrm((L, D_MODEL), 0.02),
        'w_in': nrm((L, D_MODEL, IN_COLS), D_MODEL ** -0.5),
        'conv_w': nrm((L, CONV_WIDTH, D_CONV), CONV_WIDTH ** -0.5),
        'conv_b': nrm((L, D_CONV), 0.02),
        'conv_ln_g': 1.0 + nrm((L, D_CONV), 0.02),
        'conv_ln_b': nrm((L, D_CONV), 0.02),
        'mlstm_b_i': nrm((L, N_DIR, MLSTM_HEADS), 0.1),
        'mlstm_b_f': jnp.linspace(3.0, 6.0, MLSTM_HEADS, dtype=jnp.float32) + nrm((L, N_DIR, MLSTM_HEADS), 0.1),
        'mlstm_norm_g': 1.0 + nrm((L, D_MLSTM), 0.02),
        'lru_conv_w': nrm((L, N_DIR, LRU_CONV_WIDTH, D_LRU), LRU_CONV_WIDTH ** -0.5),
        'lru_conv_b': nrm((L, N_DIR, D_LRU), 0.02),
        'lru_w_a': nrm((L, N_DIR, LRU_HEADS, LRU_BW, LRU_BW), LRU_BW ** -0.5),
        'lru_b_a': nrm((L, N_DIR, D_LRU), 0.02),
        'lru_w_x': nrm((L, N_DIR, LRU_HEADS, LRU_BW, LRU_BW), LRU_BW ** -0.5),
        'lru_b_x': nrm((L, N_DIR, D_LRU), 0.02),
        'lru_lambda': jnp.log(u) - jnp.log1p(-u),
        'w_out': nrm((L, D_MIX, D_MODEL), D_MIX ** -0.5),
        'w_rg': nrm((L, D_MODEL, N_GROUPS), D_MODEL ** -0.5),
        'b_rg': nrm((L, N_GROUPS), 0.01),
        'w_re': nrm((L, D_MODEL, N_EXPERTS), D_MODEL ** -0.5),
        'b_re': nrm((L, N_EXPERTS), 0.01),
        'w_gate': nrm((L, N_EXPERTS, D_MODEL, D_EXPERT), D_MODEL ** -0.5),
        'w_up': nrm((L, N_EXPERTS, D_MODEL, D_EXPERT), D_MODEL ** -0.5),
        'w_down': nrm((L, N_EXPERTS, D_EXPERT, D_MODEL), D_EXPERT ** -0.5),
        'final_g': 1.0 + nrm((D_MODEL,), 0.02),
    }


def reference(x, c, ctx, c_ctx, w_mod, b_mod, norm1_g, norm2_g, w_in, conv_w, conv_b, conv_ln_g,
              conv_ln_b, mlstm_b_i, mlstm_b_f, mlstm_norm_g, lru_conv_w, lru_conv_b, lru_w_a, lru_b_a,
              lru_w_x, lru_b_x, lru_lambda, w_out, w_rg, b_rg, w_re, b_re, w_gate, w_up, w_down, final_g):
    rows = x.shape[1] // GRID_W
    split_at = [int(s) for s in np.cumsum(SPLITS)[:-1]]
    h_lat, h_ctx = x, ctx
    for l in range(DEPTH):
        last = l == DEPTH - 1
        mod_lat = jnp.split((jax.nn.silu(c) @ w_mod[l] + b_mod[l])[:, None, :], 6, axis=-1)
        mod_ctx = jnp.split(jax.nn.silu(c_ctx) @ w_mod[l] + b_mod[l], 6, axis=-1)
        u_lat = to_scan_order(rms_norm(h_lat, norm1_g[l]) * (1 + mod_lat[1]) + mod_lat[0], rows, l)
        u_ctx = rms_norm(h_ctx, norm1_g[l]) * (1 + mod_ctx[1]) + mod_ctx[0]
        z_lat = jnp.split(u_lat @ w_in[l], split_at, axis=-1)
        z_ctx = jnp.split(u_ctx @ w_in[l], split_at, axis=-1)
        conv_p = (conv_w[l], conv_b[l], conv_ln_g[l], conv_ln_b[l])
        a_lat = conformer_conv(z_lat[0], z_lat[1], *conv_p)
        m_ctx, m_lat = mlstm_group(z_ctx[2:8], z_lat[2:8], mlstm_b_i[l], mlstm_b_f[l], mlstm_norm_g[l])
        r_ctx, r_lat = rglru_group(z_ctx[8], z_ctx[9], z_lat[8], z_lat[9], lru_conv_w[l], lru_conv_b[l],
                                   lru_w_a[l], lru_b_a[l], lru_w_x[l], lru_b_x[l], lru_lambda[l])
        y_lat = from_scan_order(jnp.concatenate([a_lat, m_lat, r_lat], axis=-1) @ w_out[l], rows, l)
        h_lat = h_lat + mod_lat[2] * y_lat
        moe_p = (w_rg[l], b_rg[l], w_re[l], b_re[l], w_gate[l], w_up[l], w_down[l])
        v_lat = rms_norm(h_lat, norm2_g[l]) * (1 + mod_lat[4]) + mod_lat[3]
        h_lat = h_lat + mod_lat[5] * hier_moe(v_lat, *moe_p)
        if not last:
            a_ctx = conformer_conv(z_ctx[0], z_ctx[1], *conv_p)
            y_ctx = jnp.concatenate([a_ctx, m_ctx, r_ctx], axis=-1) @ w_out[l]
            h_ctx = h_ctx + mod_ctx[2] * y_ctx
            v_ctx = rms_norm(h_ctx, norm2_g[l]) * (1 + mod_ctx[4]) + mod_ctx[3]
            h_ctx = h_ctx + mod_ctx[5] * hier_moe(v_ctx, *moe_p)
    return rms_norm(h_lat, final_g)
```

```python
import numpy as np
from contextlib import ExitStack
import concourse.bass as bass
import concourse.mybir as mybir
from concourse.bass_utils import run_bass_kernel_spmd

F32 = mybir.dt.float32
BF16 = mybir.dt.bfloat16
AF = mybir.ActivationFunctionType
ALU = mybir.AluOpType
AX = mybir.AxisListType

D = 1024
SEQ = 4096
CTX = 256
T = SEQ + CTX
NT = T // 128
DEPTH = 2
GRID_W = 64
EPS = 1e-6
IN_COLS = 3088
NE = 16
DE = 512
SL = 12
NCAP = SL * 512
I32 = mybir.dt.int32
SAME_ENG_SYNC = True
SAME_ENG_DIST = 0

PF = {}
_off = 0
def _pf(name, n):
    global _off
    PF[name] = (_off, n)
    _off += n
_pf('c2', 16)
_pf('b_mod', 2 * 48)
_pf('g1', 2 * 8)
_pf('g2', 2 * 8)
_pf('fg', 8)
_pf('conv_w', 2 * 2 * 31)
_pf('conv_b', 2 * 2)
_pf('conv_ln_g', 2 * 2)
_pf('conv_ln_b', 2 * 2)
_pf('mnorm_g', 2 * 4)
_pf('lru_cw', 2 * 2 * 2 * 4)
_pf('lru_cb', 2 * 2 * 2)
_pf('lru_ba', 2 * 2 * 2)
_pf('lru_bx', 2 * 2 * 2)
_pf('lru_lam', 2 * 2 * 2)
_pf('brb', 2 * 20)
_pf('gb', 2 * 2)
NPF = _off


class Sched:
    ENG = ['pe', 'act', 'dve', 'pool', 'sp']

    def __init__(self, nc, stack, nds=14):
        self.nc = nc
        self.e = {}
        for n in self.ENG:
            self.e[n] = dict(sem=stack.enter_context(nc.semaphore('sem_' + n)), cnt=0, ops=[], seen={})
        self.dsq = {}
        for q, cnt in [('sp', 12), ('pool', 8), ('act', 4), ('dve', 2), ('pe', 2)]:
            self.dsq[q] = dict(i=0, lst=[dict(sem=stack.enter_context(nc.semaphore('ds_%s%d' % (q, i))), n=0, name='ds_%s%d' % (q, i))
                                        for i in range(cnt)])
        self.ds = [d for q in self.dsq.values() for d in q['lst']]
        self.lastw = {}
        self.rd = {}
        self.nops = 0
        self.rec = None

    def reg(self, val):
        if val not in self.regs:
            self.regs[val] = self.cur_e.to_reg(val)
        return self.regs[val]

    def replay(self, lst):
        assert self.rec is None
        for (kind, en, a, R, W, kw) in lst:
            if kind == 'op':
                self.op(en, a, R, W)
            else:
                self.dma(en, a[0], a[1], R, W, **kw)

    def pipeline(self, iters):
        n = len(iters)
        ns = max(len(x) for x in iters)
        for t in range(n + ns - 1):
            act = []
            for st in range(ns - 1, -1, -1):
                it = t - st
                if 0 <= it < n and st < len(iters[it]) and iters[it][st]:
                    act.append(iters[it][st])
            tot = sum(len(x) for x in act)
            pos = [0] * len(act)
            for k in range(tot):
                best, bi = None, -1
                for i, lst in enumerate(act):
                    if pos[i] < len(lst):
                        f = (pos[i] + 0.5) / len(lst)
                        if best is None or f < best:
                            best, bi = f, i
                self.replay(act[bi][pos[bi]:pos[bi] + 1])
                pos[bi] += 1

    @staticmethod
    def _excl(k):
        return (isinstance(k, tuple) and k and k[0] in ('cP', 'cQ', 'cT')) or (isinstance(k, str) and len(k) == 3 and k.startswith('ps'))

    def _deps(self, R, W):
        if any(self._excl(r) for r in R):
            W = tuple(W) + tuple(r for r in R if self._excl(r))
        deps = []
        for r in R:
            if r in self.lastw:
                deps.append(self.lastw[r])
        for w in W:
            if w in self.lastw:
                deps.append(self.lastw[w])
            deps.extend(self.rd.get(w, ()))
        return deps

    def _commit(self, R, W, tok):
        if any(self._excl(r) for r in R):
            W = tuple(W) + tuple(r for r in R if self._excl(r))
            R = tuple(r for r in R if not self._excl(r))
        for r in R:
            self.rd.setdefault(r, []).append(tok)
        for w in W:
            self.lastw[w] = tok
            self.rd[w] = []

    def _waits(self, en, deps):
        E = self.e[en]
        out = []
        for (sname, sem, val, src) in deps:
            if src == en and en == 'pe':
                continue
            if src == en and (not SAME_ENG_SYNC) and en in ('act', 'dve'):
                continue
            if src == en and en in ('act', 'dve') and SAME_ENG_DIST > 0 and (E['cnt'] + 1 - val) >= SAME_ENG_DIST:
                continue
            if E['seen'].get(sname, 0) >= val:
                continue
            E['seen'][sname] = val
            out.append((sem, val))
        return out

    def op(self, en, fn, R=(), W=()):
        if self.rec is not None:
            self.rec.append(('op', en, fn, tuple(R), tuple(W), None))
            return
        E = self.e[en]
        waits = self._waits(en, self._deps(R, W))
        E['cnt'] += 1
        tok = ('sem_' + en, E['sem'], E['cnt'], en)
        E['ops'].append((waits, fn, (E['sem'], 1)))
        self._commit(R, W, tok)
        self.nops += 1

    def dma_fn(self, q, fn, R=(), W=()):
        self.dma(q, None, None, R, W, _fn=fn)

    def dma(self, q, out, in_, R=(), W=(), **kw):
        if self.rec is not None:
            self.rec.append(('dma', q, (out, in_), tuple(R), tuple(W), kw))
            return
        E = self.e[q]
        dq = self.dsq[q]
        d = dq['lst'][dq['i']]
        dq['i'] = (dq['i'] + 1) % len(dq['lst'])
        deps = self._deps(R, W)
        if d['n'] > 0:
            deps.append((d['name'], d['sem'], 16 * d['n'], 'dma'))
        waits = self._waits(q, deps)
        d['n'] += 1
        tok = (d['name'], d['sem'], 16 * d['n'], 'dma')
        if '_fn' in kw:
            E['ops'].append((waits, kw['_fn'], (d['sem'], 16)))
        else:
            E['ops'].append((waits, (lambda e: e.dma_start(out=out, in_=in_, **kw)), (d['sem'], 16)))
        self._commit(R, W, tok)
        self.nops += 1

    def barrier(self):
        toks = [('sem_' + n, self.e[n]['sem'], self.e[n]['cnt'], n) for n in self.ENG if self.e[n]['cnt'] > 0]
        toks += [(d['name'], d['sem'], 16 * d['n'], 'dma') for d in self.ds if d['n'] > 0]
        for n in self.ENG:
            E = self.e[n]
            waits = []
            for (sname, sem, val, src) in toks:
                if src == n:
                    continue
                if E['seen'].get(sname, 0) >= val:
                    continue
                E['seen'][sname] = val
                waits.append((sem, val))
            E['ops'].append((waits, None, None))
        self.lastw = {}
        self.rd = {}

    def flush(self):
        nc = self.nc
        with nc.Block() as block:
            for n, dec in [('sp', block.sync), ('act', block.scalar), ('dve', block.vector),
                           ('pool', block.gpsimd), ('pe', block.tensor)]:
                ops = self.e[n]['ops']

                def body(e, ops=ops, n=n):
                    self.regs = {}
                    self.cur_e = e
                    for waits, fn, inc in ops:
                        for sem, val in waits:
                            e.wait_ge(sem, val)
                        if fn is not None:
                            ins = fn(e)
                            ins.then_inc(inc[0], inc[1])
                    for r in self.regs.values():
                        e.free_register(r)
                    self.regs = {}
                dec(body)
                self.e[n]['ops'] = []


class Ring:
    suffix = ''

    def __init__(self, nc, stack, name, shape, dtype, n):
        self.t = [stack.enter_context(nc.sbuf_tensor('%s%d%s' % (name, i, Ring.suffix), shape, dtype)) for i in range(n)]
        self.k = [('%s%d' % (name, i)) for i in range(n)]
        self.i = 0

    def next(self):
        i = self.i
        self.i = (i + 1) % len(self.t)
        return self.t[i], self.k[i]


def tile_rows(l, j):
    if j < 2:
        return [(slice(128 * j, 128 * j + 128), 0, 128)]
    jl = j - 2
    if l % 2 == 0:
        return [(slice(CTX + 128 * jl, CTX + 128 * jl + 128), 0, 128)]
    return [(slice(CTX + 2 * jl + wi, CTX + SEQ, GRID_W), 64 * wi, 64) for wi in range(2)]


class K:
    pass


def build(debug=None, stop_after=None):
    nc = bass.Bass("TRN2", target_bir_lowering=False)
    g = K()
    g.nc = nc
    g.debug = debug or ()
    g.stop_after = stop_after

    def din(name, shape, dt=F32):
        return nc.dram_tensor(name, list(shape), dt, kind="ExternalInput").ap()

    def dscr(name, shape, dt=F32):
        kind = "ExternalOutput" if name in g.debug else "Internal"
        return nc.dram_tensor(name, list(shape), dt, kind=kind).ap()

    g.x = din('x', [SEQ, D])
    g.ctx = din('ctx', [CTX, D])
    g.pf = din('pf', [128, NPF])
    g.ident = din('ident', [128, 128])
    g.w_mod = din('w_mod', [DEPTH, D, 6 * D])
    g.w_in = din('w_in', [DEPTH, D, IN_COLS])
    g.lruW = din('lruW', [128, DEPTH * 2 * 2 * 2 * 128])
    g.w_out = din('w_out', [DEPTH, D, D])
    g.wr = din('wr', [128, DEPTH * 8 * 20])
    g.w_gate = din('w_gate', [DEPTH, NE, D, DE])
    g.w_up = din('w_up', [DEPTH, NE, D, DE])
    g.w_down = din('w_down', [DEPTH, NE, DE, D])
    g.tri = din('tri', [128, 128])
    g.cst = din('cst', [128, 32])
    g.out = nc.dram_tensor('out', [SEQ, D], F32, kind="ExternalOutput").ap()

    g.Hs = dscr('Hs', [T, D])
    g.ZC = dscr('ZC', [512, T])
    g.ZQK = dscr('ZQK', [1024, T], BF16)
    g.ZG = dscr('ZG', [16, T])
    g.ZL = dscr('ZL', [512, T])
    g.ZKV = dscr('ZKV', [T, 1024], BF16)
    g.ZO = dscr('ZO', [T, 512])
    g.HM = dscr('HM', [2, T, 512])
    g.MIX = dscr('MIX', [1024, T], BF16)
    g.VT = dscr('VT', [1024, T], BF16)
    g.CW = dscr('CW', [T, 16])
    g.WB = dscr('WB', [NE * 128, 12288], BF16)
    g.VTK = dscr('VTK', [T, 1032], BF16)
    g.GOH = dscr('GOH', [T, 4])
    g.VS = dscr('VS', [NCAP, 1032], BF16)
    g.YS = dscr('YS', [NCAP, 1024])
    g.RTD = dscr('RTD', [T, 64]) if 'RTD' in g.debug else None

    with ExitStack() as gs:
        S = Sched(nc, gs)
        g.S = S
        sb = lambda name, shape, dt=F32: gs.enter_context(nc.sbuf_tensor(name, list(shape), dt))
        g.PS = [gs.enter_context(nc.psum_tensor('ps%d' % i, [128, 512], F32)) for i in range(8)]
        g.psi = 0
        g.pft = sb('pft', [128, NPF])
        g.idf = sb('idf', [128, 128])
        g.idb = sb('idb', [128, 128], BF16)
        g.onesf = sb('onesf', [128, 128])
        g.onesb = sb('onesb', [128, 128], BF16)
        g.sc = sb('sc', [128, 16])
        g.modT = sb('modT', [128, 96])
        g.gs1 = sb('gs1', [128, 16])
        g.gs2 = sb('gs2', [128, 16])
        g.gateb = sb('gateb', [128, 4 * 1024])
        g.fgb = sb('fgb', [128, 1024])

        S.dma('sp', g.pft[:], g.pf[:, :], W=['pft'])
        S.dma('sp', g.idf[:], g.ident[:, :], W=['idf'])
        S.op('dve', lambda e: e.tensor_copy(out=g.idb[:], in_=g.idf[:]), R=['idf'], W=['idb'])
        S.op('dve', lambda e: e.memset(g.onesf[:], 1.0), W=['onesf'])
        S.op('dve', lambda e: e.memset(g.onesb[:], 1.0), W=['onesb'])
        c0 = PF['c2'][0]
        S.op('act', lambda e: e.activation(out=g.sc[:], in_=g.pft[:, c0:c0 + 16], func=AF.Silu),
             R=['pft'], W=['sc'])

        for l in range(DEPTH):
            last = (l == DEPTH - 1)
            Ring.suffix = '_L%d' % l
            with ExitStack() as stA:
                g.win = stA.enter_context(nc.sbuf_tensor('win' + Ring.suffix, [128, 8 * IN_COLS], BF16))
                for k in range(8):
                    S.dma('pool', g.win[:, k * IN_COLS:(k + 1) * IN_COLS], g.w_in[l, k * 128:(k + 1) * 128, :], W=['win'])
                phase_mod(g, l)
                phase_proj(g, l)
            if g.stop_after == ('A', l):
                break
            phase_convlru(g, l)
            if g.stop_after == ('B', l):
                break
            phase_mlstm(g, l)
            if g.stop_after == ('C', l):
                break
            phase_out(g, l)
            if g.stop_after == ('D', l):
                break
            phase_moe_sparse(g, l)
            if g.stop_after == ('E', l):
                break
        S.barrier()
        S.flush()
    return nc


def psum(g):
    i = g.psi
    g.psi = (i + 1) % 8
    return g.PS[i], 'ps%d' % i


def pfc(g, name, idx=0, n=1):
    o = PF[name][0] + idx
    return g.pft[:, o:o + n]


def phase_mod(g, l):
    nc, S = g.nc, g.S
    with ExitStack() as st:
        wm = Ring(nc, st, 'wm', [128, 8 * 512], BF16, 3)
        scb = st.enter_context(nc.sbuf_tensor('scb' + Ring.suffix, [128, 16], BF16))
        S.op('dve', lambda e: e.tensor_copy(out=scb[:], in_=g.sc[:]), R=['sc'], W=['scb'])
        dg = Ring(nc, st, 'dg', [128, 128], F32, 2)
        tmp = st.enter_context(nc.sbuf_tensor('modtmp' + Ring.suffix, [128, 16], F32))
        ps, pk = psum(g)
        for cb in range(12):
            wt, wk = wm.next()
            S.dma('pool', wt[:].rearrange("p (k n) -> p k n", k=8),
                  g.w_mod[l, :, cb * 512:(cb + 1) * 512].rearrange("(k p) n -> p k n", p=128), W=[wk])
            for j in range(4):
                m = cb * 4 + j

                def f(e, wt=wt, j=j, m=m):
                    for k in range(8):
                        ins = e.matmul(ps[:, 2 * m:2 * m + 2], lhsT=wt[:, k * 512 + j * 128:k * 512 + (j + 1) * 128],
                                       rhs=scb[:, 2 * k:2 * k + 2], start=(k == 0), stop=(k == 7))
                    return ins
                S.op('pe', f, R=[wk, 'scb'], W=[pk])
        bo = PF['b_mod'][0] + l * 48
        for s in range(2):
            S.op('dve', lambda e, s=s: e.tensor_tensor(out=g.modT[:, s:96:2], in0=ps[:, s:96:2],
                                                       in1=g.pft[:, bo:bo + 48], op=ALU.add),
                 R=[pk, 'pft'], W=['modT'])
        for (gsT, gname, sec) in [(g.gs1, 'g1', 1), (g.gs2, 'g2', 4)]:
            go = PF[gname][0] + l * 8
            S.op('dve', lambda e, sec=sec: e.tensor_scalar(out=tmp[:], in0=g.modT[:, sec * 16:sec * 16 + 16],
                                                           scalar1=1.0, scalar2=None, op0=ALU.add),
                 R=['modT'], W=['modtmp'])
            for s in range(2):
                S.op('dve', lambda e, s=s, gsT=gsT, go=go: e.tensor_tensor(out=gsT[:, s:16:2], in0=tmp[:, s:16:2],
                                                                           in1=g.pft[:, go:go + 8], op=ALU.mult),
                     R=['modtmp', 'pft'], W=['gs'])
        for gi, sec in enumerate([2, 5]):
            for s in range(2):
                pa, pak = psum(g)
                pb, pbk = psum(g)
                for k in range(8):
                    dt_, dk = dg.next()
                    col = sec * 16 + 2 * k + s
                    S.op('dve', lambda e, dt_=dt_, col=col: e.tensor_scalar(
                        out=dt_[:], in0=g.idf[:], scalar1=g.modT[:, col:col + 1], scalar2=None, op0=ALU.mult),
                        R=['idf', 'modT'], W=[dk])
                    pp, ppk = (pa, pak) if k < 4 else (pb, pbk)
                    S.op('pe', lambda e, pp=pp, dt_=dt_, k=k: e.matmul(
                        pp[:, (k % 4) * 128:(k % 4 + 1) * 128], lhsT=g.onesf[:], rhs=dt_[:], start=True, stop=True),
                        R=[dk, 'onesf'], W=[ppk])
                o = (gi * 2 + s) * 1024
                S.op('act', lambda e, o=o, pa=pa: e.copy(out=g.gateb[:, o:o + 512], in_=pa[:]), R=[pak], W=['gateb'])
                S.op('act', lambda e, o=o, pb=pb: e.copy(out=g.gateb[:, o + 512:o + 1024], in_=pb[:]), R=[pbk], W=['gateb'])
        if l == DEPTH - 1:
            fo = PF['fg'][0]
            pa, pak = psum(g)
            pb, pbk = psum(g)
            for k in range(8):
                dt_, dk = dg.next()
                S.op('dve', lambda e, dt_=dt_, k=k: e.tensor_scalar(
                    out=dt_[:], in0=g.idf[:], scalar1=g.pft[:, fo + k:fo + k + 1], scalar2=None, op0=ALU.mult),
                    R=['idf', 'pft'], W=[dk])
                pp, ppk = (pa, pak) if k < 4 else (pb, pbk)
                S.op('pe', lambda e, pp=pp, dt_=dt_, k=k: e.matmul(
                    pp[:, (k % 4) * 128:(k % 4 + 1) * 128], lhsT=g.onesf[:], rhs=dt_[:], start=True, stop=True),
                    R=[dk, 'onesf'], W=[ppk])
            S.op('act', lambda e, pa=pa: e.copy(out=g.fgb[:, 0:512], in_=pa[:]), R=[pak], W=['fgb'])
            S.op('act', lambda e, pb=pb: e.copy(out=g.fgb[:, 512:1024], in_=pb[:]), R=[pbk], W=['fgb'])
        S.barrier()
        S.flush()


def load_h_tile(g, l, j, dst, dk, src_is_input, q='sp'):
    S = g.S
    for (rs, p0, npart) in tile_rows(l, j):
        if src_is_input:
            if j < 2:
                src = g.ctx[rs, :]
            else:
                src = g.x[slice(rs.start - CTX, rs.stop - CTX, rs.step), :]
        else:
            src = g.Hs[rs, :]
        S.dma(q, dst[p0:p0 + npart, :], src, W=[dk])


def rms_rstd(g, ht, hk, junk, jk, ssq, sk, rstd, rk):
    S = g.S
    S.op('act', lambda e: e.activation(out=junk[:], in_=ht[:], func=AF.Square, accum_out=ssq[:]),
         R=[hk], W=[jk, sk])
    S.op('act', lambda e: e.activation(out=ssq[:], in_=ssq[:], func=AF.Ln, scale=1.0 / D, bias=g.epsc[:]),
         R=[sk], W=[sk])
    S.op('act', lambda e: e.activation(out=rstd[:], in_=ssq[:], func=AF.Exp, scale=-0.5), R=[sk], W=[rk])


def phase_proj(g, l):
    nc, S = g.nc, g.S
    with ExitStack() as st:
        sbt = lambda name, shape, dt=F32: st.enter_context(nc.sbuf_tensor(name + Ring.suffix, list(shape), dt))
        win = g.win
        g.epsc = sbt('epsc', [128, 1])
        S.op('dve', lambda e: e.memset(g.epsc[:], EPS), W=['epsc'])
        hT = Ring(nc, st, 'hT', [128, 1024], F32, 5)
        hn = Ring(nc, st, 'hn', [128, 1024], BF16, 3)
        junk = sbt('junk', [128, 1024], BF16)
        ssq = Ring(nc, st, 'ssq', [128, 1], F32, 4)
        rstd = Ring(nc, st, 'rstd', [128, 1], F32, 4)
        uT = Ring(nc, st, 'uT', [128, 8 * 512], BF16, 3)
        stC = Ring(nc, st, 'stC', [128, 4 * 512], F32, 2)
        stL = Ring(nc, st, 'stL', [128, 4 * 512], F32, 2)
        stQ = Ring(nc, st, 'stQ', [128, 8 * 512], BF16, 2)
        stG = Ring(nc, st, 'stG', [16, 512], F32, 2)
        stKV = Ring(nc, st, 'stKV', [128, 1024], BF16, 3)
        stO = Ring(nc, st, 'stO', [128, 512], F32, 3)
        evi = [0]

        def evac(out, in_, R, W):
            evi[0] += 1
            if evi[0] % 2 == 0:
                S.op('act', lambda e: e.copy(out=out, in_=in_), R=R, W=W)
            else:
                S.op('dve', lambda e: e.tensor_copy(out=out, in_=in_), R=R, W=W)

        blocks = [(0, 2)] + [(2 + 4 * i, 4) for i in range(8)]
        iters = []
        pji = [0]
        tci = [0]
        for (j0, ntile) in blocks:
            st1, st2 = [], []
            S.rec = st1
            NB = ntile * 128
            t0 = j0 * 128
            ut, uk = uT.next()
            s = 1 if j0 == 0 else 0
            for ti in range(ntile):
                j = j0 + ti
                ht, hk = hT.next()
                load_h_tile(g, l, j, ht, hk, l == 0)
                sq, sk = ssq.next()
                rs_, rk = rstd.next()
                rms_rstd(g, ht, hk, junk, 'junk', sq, sk, rs_, rk)
                hb, hbk = hn.next()
                S.op('dve', lambda e, hb=hb, ht=ht, rs_=rs_: e.tensor_scalar(
                    out=hb[:], in0=ht[:], scalar1=rs_[:, 0:1], scalar2=None, op0=ALU.mult),
                    R=[hk, rk], W=[hbk])
                tb = 2 * (tci[0] % 2)
                tci[0] += 1
                psA, pkA = g.PS[tb], 'ps%d' % tb
                psB, pkB = g.PS[tb + 1], 'ps%d' % (tb + 1)
                psbA = psA[:].bitcast(BF16)
                psbB = psB[:].bitcast(BF16)

                def ftr(e, hb=hb, psbA=psbA, psbB=psbB):
                    for k in range(8):
                        dst = psbA if k < 4 else psbB
                        ins = e.transpose(out=dst[:, (k % 4) * 128:(k % 4 + 1) * 128], in_=hb[:, k * 128:(k + 1) * 128],
                                          identity=g.idb[:])
                    return ins
                S.op('pe', ftr, R=[hbk, 'idb'], W=[pkA, pkB])
                for k in range(8):
                    col = 2 * k + s
                    sh = 0 * 16 + col
                    eng = 'act' if k < 4 else 'dve'
                    psb = psbA if k < 4 else psbB
                    pk = pkA if k < 4 else pkB
                    if eng == 'act':
                        S.op('act', lambda e, k=k, ti=ti, ut=ut, psb=psb, col=col, sh=sh: e.activation(
                            out=ut[:, k * 512 + ti * 128:k * 512 + (ti + 1) * 128], in_=psb[:, (k % 4) * 128:(k % 4 + 1) * 128],
                            func=AF.Identity, scale=g.gs1[:, col:col + 1], bias=g.modT[:, sh:sh + 1]),
                            R=[pk, 'gs', 'modT'], W=[uk])
                    else:
                        S.op('dve', lambda e, k=k, ti=ti, ut=ut, psb=psb, col=col, sh=sh: e.tensor_scalar(
                            out=ut[:, k * 512 + ti * 128:k * 512 + (ti + 1) * 128], in0=psb[:, (k % 4) * 128:(k % 4 + 1) * 128],
                            scalar1=g.gs1[:, col:col + 1], scalar2=g.modT[:, sh:sh + 1], op0=ALU.mult, op1=ALU.add),
                            R=[pk, 'gs', 'modT'], W=[uk])
            S.rec = st2
            groups = [
                ('C', stC, g.ZC, [(0 + 128 * i, 128) for i in range(4)], F32),
                ('Q', stQ, g.ZQK, [(512 + 128 * i, 128) for i in range(8)], BF16),
                ('G', stG, g.ZG, [(2560, 16)], F32),
                ('L', stL, g.ZL, [(2576 + 128 * i, 128) for i in range(4)], F32),
            ]
            for (gname, ring, dst, chunks, dt_) in groups:
                stt, stk = ring.next()
                for ci, (c0, M) in enumerate(chunks):
                    ps, pk = g.PS[4 + pji[0] % 4], 'ps%d' % (4 + pji[0] % 4)
                    pji[0] += 1

                    def fmm(e, ps=ps, c0=c0, M=M, ut=ut, NB=NB):
                        for k in range(8):
                            ins = e.matmul(ps[0:M, 0:NB], lhsT=win[:, k * IN_COLS + c0:k * IN_COLS + c0 + M],
                                           rhs=ut[:, k * 512:k * 512 + NB], start=(k == 0), stop=(k == 7))
                        return ins
                    S.op('pe', fmm, R=['win', uk], W=[pk])
                    evac(stt[0:M, ci * 512:ci * 512 + NB], ps[0:M, 0:NB], [pk], [stk])
                nch = len(chunks)
                M = chunks[0][1]
                if nch == 1:
                    S.dma('pool', dst[0:M, t0:t0 + NB], stt[0:M, 0:NB], R=[stk])
                else:
                    S.dma('pool', dst[:, t0:t0 + NB].rearrange("(c p) n -> p c n", p=128),
                          stt[:].rearrange("p (c n) -> p c n", c=nch)[:, :, 0:NB], R=[stk])
            for ti in range(ntile):
                r0 = t0 + ti * 128
                kv, kvk = stKV.next()
                ot, ok = stO.next()
                for ci, c0 in enumerate([1024, 1536, 2048]):
                    ps, pk = g.PS[4 + pji[0] % 4], 'ps%d' % (4 + pji[0] % 4)
                    pji[0] += 1

                    def fmm(e, ps=ps, c0=c0, ut=ut, ti=ti):
                        for k in range(8):
                            ins = e.matmul(ps[:, :], lhsT=ut[:, k * 512 + ti * 128:k * 512 + (ti + 1) * 128],
                                           rhs=win[:, k * IN_COLS + c0:k * IN_COLS + c0 + 512],
                                           start=(k == 0), stop=(k == 7))
                        return ins
                    S.op('pe', fmm, R=['win', uk], W=[pk])
                    if ci < 2:
                        evac(kv[:, ci * 512:(ci + 1) * 512], ps[:, :], [pk], [kvk])
                    else:
                        evac(ot[:, :], ps[:, :], [pk], [ok])
                S.dma('pool', g.ZKV[r0:r0 + 128, :], kv[:], R=[kvk])
                S.dma('pool', g.ZO[r0:r0 + 128, :], ot[:], R=[ok])
            S.rec = None
            iters.append([st1, st2])
        S.pipeline(iters)
        S.barrier()
        S.flush()


def phase_convlru(g, l):
    phase_conv(g, l)
    phase_lru(g, l)


SEGS = [(0, CTX), (CTX, SEQ)]


def seg_blocks(s0, n):
    return [(s0 + o, min(512, n - o)) for o in range(0, n, 512)]


def phase_conv(g, l):
    nc, S = g.nc, g.S
    last = (l == DEPTH - 1)
    with ExitStack() as st:
        sbt = lambda name, shape, dt=F32: st.enter_context(nc.sbuf_tensor(name + Ring.suffix, list(shape), dt))
        DG = sbt('DG', [128, 2 * 31 * 128], BF16)
        UPl = [sbt('UPl%d' % c, [128, SEQ + 30], BF16) for c in range(2)]
        UPc = [sbt('UPc%d' % c, [128, CTX + 30], BF16) for c in range(2)]
        epsc = sbt('epsc2', [128, 1])
        S.op('dve', lambda e: e.memset(epsc[:], EPS), W=['epsc2'])
        vin = Ring(nc, st, 'vin', [128, 512], F32, 3)
        gin = Ring(nc, st, 'gin', [128, 512], F32, 3)
        sgr = Ring(nc, st, 'sgr', [128, 512], F32, 2)
        yb = [Ring(nc, st, 'yb%d' % c, [128, 512], F32, 3) for c in range(2)]
        y2 = [Ring(nc, st, 'y2%d' % c, [128, 512], F32, 3) for c in range(2)]
        mean = Ring(nc, st, 'mean', [128, 512], F32, 2)
        msq = Ring(nc, st, 'msq', [128, 512], F32, 2)
        var = Ring(nc, st, 'var', [128, 512], F32, 2)
        t1 = Ring(nc, st, 't1', [128, 512], F32, 3)
        t2 = Ring(nc, st, 't2', [128, 512], F32, 3)
        ost = Ring(nc, st, 'ost', [128, 2 * 512], BF16, 2)
        cwo = PF['conv_w'][0] + l * 62
        for c in range(2):
            for k in range(31):
                S.op('dve', lambda e, c=c, k=k: e.tensor_scalar(
                    out=DG[:, (c * 31 + k) * 128:(c * 31 + k + 1) * 128], in0=g.idf[:],
                    scalar1=g.pft[:, cwo + c * 31 + k:cwo + c * 31 + k + 1], scalar2=None, op0=ALU.mult),
                    R=['idf', 'pft'], W=['DG'])
        segs = [SEGS[1]] if last else SEGS
        for (s0, n) in segs:
            UP = UPc if s0 == 0 else UPl
            ukey = 'UPc' if s0 == 0 else 'UPl'
            for c in range(2):
                S.op('pool', lambda e, c=c, UP=UP: e.memset(UP[c][:, 0:15], 0.0), W=[ukey])
                S.op('pool', lambda e, c=c, UP=UP, n=n: e.memset(UP[c][:, 15 + n:30 + n], 0.0), W=[ukey])
            for (t0, nb) in seg_blocks(s0, n):
                for c in range(2):
                    vt, vk = vin.next()
                    gt, gk = gin.next()
                    S.dma('sp', vt[:, 0:nb], g.ZC[c * 128:(c + 1) * 128, t0:t0 + nb], W=[vk])
                    S.dma('sp', gt[:, 0:nb], g.ZC[256 + c * 128:256 + (c + 1) * 128, t0:t0 + nb], W=[gk])
                    sg, sgk = sgr.next()
                    S.op('act', lambda e, sg=sg, gt=gt, nb=nb: e.activation(out=sg[:, 0:nb], in_=gt[:, 0:nb], func=AF.Sigmoid),
                         R=[gk], W=[sgk])
                    o0 = 15 + t0 - s0
                    S.op('dve', lambda e, c=c, UP=UP, vt=vt, sg=sg, nb=nb, o0=o0: e.tensor_tensor(
                        out=UP[c][:, o0:o0 + nb], in0=vt[:, 0:nb], in1=sg[:, 0:nb], op=ALU.mult),
                        R=[vk, sgk], W=[ukey])
            iters = []
            for bi_, (t0, nb) in enumerate(seg_blocks(s0, n)):
                st1, st2 = [], []
                S.rec = st1
                o0 = t0 - s0
                ybt = []
                y2t = []
                for c in range(2):
                    ps, pk = g.PS[2 * (bi_ % 2) + c], 'ps%d' % (2 * (bi_ % 2) + c)

                    def fconv(e, ps=ps, c=c, UP=UP, o0=o0, nb=nb):
                        for k in range(31):
                            ins = e.matmul(ps[:, 0:nb], lhsT=DG[:, (c * 31 + k) * 128:(c * 31 + k + 1) * 128],
                                           rhs=UP[c][:, o0 + k:o0 + k + nb], start=(k == 0), stop=(k == 30))
                        return ins
                    S.op('pe', fconv, R=['DG', ukey], W=[pk])
                    a, ak = yb[c].next()
                    b, bk = y2[c].next()
                    cb = pfc(g, 'conv_b', l * 2 + c)
                    S.op('act', lambda e, a=a, ps=ps, cb=cb, nb=nb: e.activation(
                        out=a[:, 0:nb], in_=ps[:, 0:nb], func=AF.Identity, bias=cb, scale=1.0), R=[pk, 'pft'], W=[ak])
                    S.op('act', lambda e, b=b, ps=ps, cb=cb, nb=nb: e.activation(
                        out=b[:, 0:nb], in_=ps[:, 0:nb], func=AF.Square, bias=cb, scale=1.0), R=[pk, 'pft'], W=[bk])
                    ybt.append((a, ak))
                    y2t.append((b, bk))
                S.rec = st2
                p1, p1k = g.PS[4 + 2 * (bi_ % 2)], 'ps%d' % (4 + 2 * (bi_ % 2))
                p2, p2k = g.PS[5 + 2 * (bi_ % 2)], 'ps%d' % (5 + 2 * (bi_ % 2))

                def fst(e, p1=p1, p2=p2, ybt=ybt, y2t=y2t, nb=nb):
                    for c in range(2):
                        e.matmul(p1[:, 0:nb], lhsT=g.onesf[:], rhs=ybt[c][0][:, 0:nb], start=(c == 0), stop=(c == 1))
                    for c in range(2):
                        ins = e.matmul(p2[:, 0:nb], lhsT=g.onesf[:], rhs=y2t[c][0][:, 0:nb], start=(c == 0), stop=(c == 1))
                    return ins
                S.op('pe', fst, R=['onesf', ybt[0][1], ybt[1][1], y2t[0][1], y2t[1][1]], W=[p1k, p2k])
                mt, mk = mean.next()
                qt, qk = msq.next()
                vt_, vk_ = var.next()
                S.op('act', lambda e, mt=mt, p1=p1, nb=nb: e.activation(out=mt[:, 0:nb], in_=p1[:, 0:nb], func=AF.Copy, scale=1.0 / 256),
                     R=[p1k], W=[mk])
                S.op('pool', lambda e, mt=mt, qt=qt, nb=nb: e.tensor_tensor(out=qt[:, 0:nb], in0=mt[:, 0:nb], in1=mt[:, 0:nb], op=ALU.mult),
                     R=[mk], W=[qk])
                S.op('dve', lambda e, vt_=vt_, p2=p2, qt=qt, nb=nb: e.scalar_tensor_tensor(
                    out=vt_[:, 0:nb], in0=p2[:, 0:nb], scalar=1.0 / 256, in1=qt[:, 0:nb], op0=ALU.mult, op1=ALU.subtract),
                    R=[p2k, qk], W=[vk_])
                S.op('act', lambda e, vt_=vt_, nb=nb: e.activation(out=vt_[:, 0:nb], in_=vt_[:, 0:nb], func=AF.Ln, bias=epsc[:], scale=1.0),
                     R=[vk_, 'epsc2'], W=[vk_])
                S.op('act', lambda e, vt_=vt_, nb=nb: e.activation(out=vt_[:, 0:nb], in_=vt_[:, 0:nb], func=AF.Exp, scale=-0.5),
                     R=[vk_], W=[vk_])
                os_, osk = ost.next()
                for c in range(2):
                    a, ak = ybt[c]
                    x1, x1k = t1.next()
                    x2, x2k = t2.next()
                    S.op('dve', lambda e, x1=x1, a=a, mt=mt, nb=nb: e.tensor_tensor(out=x1[:, 0:nb], in0=a[:, 0:nb], in1=mt[:, 0:nb], op=ALU.subtract),
                         R=[ak, mk], W=[x1k])
                    S.op('pool', lambda e, x2=x2, x1=x1, vt_=vt_, nb=nb: e.tensor_tensor(out=x2[:, 0:nb], in0=x1[:, 0:nb], in1=vt_[:, 0:nb], op=ALU.mult),
                         R=[x1k, vk_], W=[x2k])
                    lg = pfc(g, 'conv_ln_g', l * 2 + c)
                    lb = pfc(g, 'conv_ln_b', l * 2 + c)
                    S.op('act', lambda e, os_=os_, x2=x2, c=c, lg=lg, lb=lb, nb=nb: e.activation(
                        out=os_[:, c * 512:c * 512 + nb], in_=x2[:, 0:nb], func=AF.Silu, scale=lg, bias=lb),
                        R=[x2k, 'pft'], W=[osk])
                S.dma('sp', g.MIX[0:256, t0:t0 + nb].rearrange("(c p) n -> p c n", p=128),
                      os_[:].rearrange("p (c n) -> p c n", c=2)[:, :, 0:nb], R=[osk])
                S.rec = None
                iters.append([st1, st2])
            S.pipeline(iters)
        S.barrier()
        S.flush()


def phase_lru(g, l):
    nc, S = g.nc, g.S
    last = (l == DEPTH - 1)
    with ExitStack() as st:
        sbt = lambda name, shape, dt=F32: st.enter_context(nc.sbuf_tensor(name + Ring.suffix, list(shape), dt))
        XPl = [sbt('XPl%d' % c, [128, SEQ + 6]) for c in range(2)]
        XPc = [sbt('XPc%d' % c, [128, CTX + 6]) for c in range(2)]
        HF = [sbt('HF%d' % c, [128, T]) for c in range(2)]
        LW = sbt('LW', [128, 2 * 2 * 2 * 128])
        DG4 = sbt('DG4', [128, 2 * 2 * 4 * 128])
        nsp = sbt('nsp', [128, 8])
        onec = sbt('onec', [128, 1])
        hinit = sbt('hinit', [128, 8])
        S.op('dve', lambda e: e.memset(onec[:], 1.0), W=['onec'])
        S.dma('sp', LW[:], g.lruW[:, l * 1024:(l + 1) * 1024], W=['LW'])
        lo = PF['lru_lam'][0] + l * 4
        tmpa = sbt('lrutmp', [128, 4])
        S.op('act', lambda e: e.activation(out=tmpa[:], in_=g.pft[:, lo:lo + 4], func=AF.Exp, scale=-1.0), R=['pft'], W=['lrutmp'])
        S.op('act', lambda e: e.activation(out=tmpa[:], in_=tmpa[:], func=AF.Ln, bias=onec[:], scale=1.0), R=['lrutmp', 'onec'], W=['lrutmp'])
        S.op('dve', lambda e: e.tensor_scalar(out=nsp[:, 0:4], in0=tmpa[:], scalar1=-8.0, scalar2=None, op0=ALU.mult), R=['lrutmp'], W=['nsp'])
        S.op('dve', lambda e: e.tensor_scalar(out=nsp[:, 4:8], in0=tmpa[:], scalar1=-16.0, scalar2=None, op0=ALU.mult), R=['lrutmp'], W=['nsp'])
        wo = PF['lru_cw'][0] + l * 16
        for d in range(2):
            for c in range(2):
                for j in range(4):
                    idx = (d * 2 + c) * 4 + j
                    S.op('dve', lambda e, idx=idx: e.tensor_scalar(
                        out=DG4[:, idx * 128:(idx + 1) * 128], in0=g.idf[:], scalar1=g.pft[:, wo + idx:wo + idx + 1],
                        scalar2=None, op0=ALU.mult), R=['idf', 'pft'], W=['DG4'])
        for c in range(2):
            for XP, n, s0, key in [(XPc, CTX, 0, 'XPc'), (XPl, SEQ, CTX, 'XPl')]:
                S.op('pool', lambda e, XP=XP, c=c: e.memset(XP[c][:, 0:3], 0.0), W=[key])
                S.op('pool', lambda e, XP=XP, c=c, n=n: e.memset(XP[c][:, 3 + n:6 + n], 0.0), W=[key])
                S.dma('sp', XP[c][:, 3:3 + n], g.ZL[c * 128:(c + 1) * 128, s0:s0 + n], W=[key])
        W5c = []
        ostc = []
        for cc in range(2):
            W5 = {}
            for nm in ['xc', 'r', 'i', 'a', 'om', 'ix', 'u', 'hb', 'gt', 'g2', 'g3', 'sg', 'gl', 'ysum']:
                W5[nm] = Ring(nc, st, 'l%d_' % cc + nm, [128, 512], F32, 2 if nm in ('xc', 'i', 'a', 'om') else 1)
            W5c.append(W5)
            ostc.append(Ring(nc, st, 'lost%d' % cc, [128, 512], BF16, 2))
        for d in range(2):
            merged = None
            for c in range(2):
                W5 = W5c[c]
                ost = ostc[c]
                order = []
                for (s0, n) in SEGS:
                    bl = seg_blocks(s0, n)
                    if d == 1:
                        bl = bl[::-1]
                    order += [(s0, t0, nb) for (t0, nb) in bl]
                dc = d * 2 + c
                prev = None
                iters = []
                for bi_, (s0, t0, nb) in enumerate(order):
                    st0, st1 = [], []
                    S.rec = st0
                    XP = XPc if s0 == 0 else XPl
                    xkey = 'XPc' if s0 == 0 else 'XPl'
                    o0 = t0 - s0
                    pbase = 3 * c
                    ps, pk = g.PS[pbase], 'ps%d' % pbase

                    def fc4(e, ps=ps, XP=XP, o0=o0, nb=nb, d=d, c=c, dc=dc):
                        for j in range(4):
                            off = o0 + j if d == 0 else o0 + 6 - j
                            ins = e.matmul(ps[:, 0:nb], lhsT=DG4[:, (dc * 4 + j) * 128:(dc * 4 + j + 1) * 128],
                                           rhs=XP[c][:, off:off + nb], start=(j == 0), stop=(j == 3))
                        return ins
                    S.op('pe', fc4, R=['DG4', xkey], W=[pk])
                    xc, xck = W5['xc'].next()
                    cb = pfc(g, 'lru_cb', l * 4 + dc)
                    S.op('act', lambda e, xc=xc, ps=ps, cb=cb, nb=nb: e.activation(
                        out=xc[:, 0:nb], in_=ps[:, 0:nb], func=AF.Identity, bias=cb, scale=1.0), R=[pk, 'pft'], W=[xck])
                    gts = []
                    for gi, (nm, bn) in enumerate([('r', 'lru_ba'), ('i', 'lru_bx')]):
                        pg_, pgk = g.PS[pbase + 1 + gi], 'ps%d' % (pbase + 1 + gi)
                        lwo = ((d * 2 + gi) * 2 + c) * 128
                        S.op('pe', lambda e, pg_=pg_, lwo=lwo, xc=xc, nb=nb: e.matmul(
                            pg_[:, 0:nb], lhsT=LW[:, lwo:lwo + 128], rhs=xc[:, 0:nb], start=True, stop=True),
                            R=['LW', xck], W=[pgk])
                        gt_, gtk = W5[nm].next()
                        bb = pfc(g, bn, l * 4 + dc)
                        S.op('act', lambda e, gt_=gt_, pg_=pg_, bb=bb, nb=nb: e.activation(
                            out=gt_[:, 0:nb], in_=pg_[:, 0:nb], func=AF.Sigmoid, bias=bb, scale=1.0), R=[pgk, 'pft'], W=[gtk])
                        gts.append((gt_, gtk))
                    (r_, rk), (i_, ik) = gts
                    a_, ak = W5['a'].next()
                    om, omk = W5['om'].next()
                    S.op('act', lambda e, a_=a_, r_=r_, dc=dc, nb=nb: e.activation(
                        out=a_[:, 0:nb], in_=r_[:, 0:nb], func=AF.Exp, scale=nsp[:, dc:dc + 1]), R=[rk, 'nsp'], W=[ak])
                    S.op('act', lambda e, om=om, r_=r_, dc=dc, nb=nb: e.activation(
                        out=om[:, 0:nb], in_=r_[:, 0:nb], func=AF.Exp, scale=nsp[:, 4 + dc:5 + dc]), R=[rk, 'nsp'], W=[omk])
                    S.op('act', lambda e, om=om, nb=nb: e.activation(
                        out=om[:, 0:nb], in_=om[:, 0:nb], func=AF.Sqrt, scale=-1.0, bias=onec[:]), R=[omk, 'onec'], W=[omk])
                    S.rec = st1
                    ix, ixk = W5['ix'].next()
                    u_, uk = W5['u'].next()
                    S.op('pool', lambda e, ix=ix, i_=i_, xc=xc, nb=nb: e.tensor_tensor(
                        out=ix[:, 0:nb], in0=i_[:, 0:nb], in1=xc[:, 0:nb], op=ALU.mult), R=[ik, xck], W=[ixk])
                    S.op('pool', lambda e, u_=u_, ix=ix, om=om, nb=nb: e.tensor_tensor(
                        out=u_[:, 0:nb], in0=ix[:, 0:nb], in1=om[:, 0:nb], op=ALU.mult), R=[ixk, omk], W=[uk])
                    if d == 0:
                        dst = HF[c][:, t0:t0 + nb]
                        dkey = 'HF%d' % c
                        init = 0.0 if prev is None else HF[c][:, prev:prev + 1]
                        S.op('dve', lambda e, dst=dst, a_=a_, u_=u_, init=init, nb=nb: e.tensor_tensor_scan(
                            out=dst, data0=a_[:, 0:nb], data1=u_[:, 0:nb], initial=init, op0=ALU.mult, op1=ALU.add),
                            R=[ak, uk, dkey], W=[dkey])
                        prev = t0 + nb - 1
                    else:
                        hb, hbk = W5['hb'].next()
                        hi = hinit[:, dc:dc + 1]
                        init = 0.0 if prev is None else hi
                        S.op('dve', lambda e, hb=hb, a_=a_, u_=u_, init=init, nb=nb: e.tensor_tensor_scan(
                            out=hb[:, nb - 1::-1] if False else hb[:, 0:nb][:, ::-1], data0=a_[:, 0:nb][:, ::-1],
                            data1=u_[:, 0:nb][:, ::-1], initial=init, op0=ALU.mult, op1=ALU.add),
                            R=[ak, uk, 'hinit%d' % dc], W=[hbk])
                        S.op('dve', lambda e, hb=hb, hi=hi: e.tensor_copy(out=hi, in_=hb[:, 0:1]), R=[hbk], W=['hinit%d' % dc])
                        prev = 1
                        if last and s0 == 0:
                            S.rec = None
                            iters.append([st0, st1])
                            continue
                        gt, gk = W5['gt'].next()
                        S.dma('sp', gt[:, 0:nb], g.ZL[256 + c * 128:256 + (c + 1) * 128, t0:t0 + nb], W=[gk])
                        g2, g2k = W5['g2'].next()
                        g3, g3k = W5['g3'].next()
                        sg, sgk = W5['sg'].next()
                        gl, glk = W5['gl'].next()
                        ys, ysk = W5['ysum'].next()
                        S.op('pool', lambda e, g2=g2, gt=gt, nb=nb: e.tensor_tensor(out=g2[:, 0:nb], in0=gt[:, 0:nb], in1=gt[:, 0:nb], op=ALU.mult),
                             R=[gk], W=[g2k])
                        S.op('pool', lambda e, g2=g2, nb=nb: e.tensor_scalar(out=g2[:, 0:nb], in0=g2[:, 0:nb], scalar1=0.044715, scalar2=1.0,
                                                                             op0=ALU.mult, op1=ALU.add), R=[g2k], W=[g2k])
                        S.op('pool', lambda e, g3=g3, g2=g2, gt=gt, nb=nb: e.tensor_tensor(out=g3[:, 0:nb], in0=g2[:, 0:nb], in1=gt[:, 0:nb], op=ALU.mult),
                             R=[g2k, gk], W=[g3k])
                        S.op('act', lambda e, sg=sg, g3=g3, nb=nb: e.activation(out=sg[:, 0:nb], in_=g3[:, 0:nb], func=AF.Sigmoid, scale=1.5957691216057308),
                             R=[g3k], W=[sgk])
                        S.op('pool', lambda e, gl=gl, sg=sg, gt=gt, nb=nb: e.tensor_tensor(out=gl[:, 0:nb], in0=sg[:, 0:nb], in1=gt[:, 0:nb], op=ALU.mult),
                             R=[sgk, gk], W=[glk])
                        S.op('dve', lambda e, ys=ys, hb=hb, c=c, t0=t0, nb=nb: e.tensor_tensor(
                            out=ys[:, 0:nb], in0=hb[:, 0:nb], in1=HF[c][:, t0:t0 + nb], op=ALU.add), R=[hbk, 'HF%d' % c], W=[ysk])
                        os_, osk = ost.next()
                        S.op('pool', lambda e, os_=os_, ys=ys, gl=gl, nb=nb: e.tensor_tensor(
                            out=os_[:, 0:nb], in0=ys[:, 0:nb], in1=gl[:, 0:nb], op=ALU.mult), R=[ysk, glk], W=[osk])
                        S.dma('sp', g.MIX[768 + c * 128:768 + (c + 1) * 128, t0:t0 + nb], os_[:, 0:nb], R=[osk])
                    S.rec = None
                    iters.append([st0, st1])
                if merged is None:
                    merged = iters
                else:
                    merged = [merged[i_] + iters[i_] for i_ in range(len(iters))]
            S.pipeline(merged)
        S.barrier()
        S.flush()


def phase_mlstm(g, l):
    nc, S = g.nc, g.S
    last = (l == DEPTH - 1)
    QS_SCALE = 128 ** -0.5
    with ExitStack() as st:
        sbt = lambda name, shape, dt=F32: st.enter_context(nc.sbuf_tensor(name + Ring.suffix, list(shape), dt))
        TMP = sbt('gTMP', [64, T])
        GI = sbt('gGI', [64, T])
        GF = sbt('gGF', [64, T])
        ones64 = sbt('ones64', [64, 128])
        onec = sbt('onec2', [64, 1])
        mcar = sbt('mcar', [64, NT + 1])
        SEL = sbt('SEL', [64, 8 * 128])
        C32 = sbt('C32', [128, 8 * 130])
        CB = sbt('CB', [128, 8 * 130], BF16)
        S.op('dve', lambda e: e.memset(ones64[:], 1.0), W=['ones64'])
        S.op('dve', lambda e: e.memset(onec[:], 1.0), W=['onec2'])
        S.op('dve', lambda e: e.memset(mcar[:], 0.0), W=['mcar'])
        S.op('dve', lambda e: e.memset(C32[:], 0.0), W=['C32%d' % u for u in range(8)])
        S.op('dve', lambda e: e.memset(CB[:], 0.0), W=['CB%d' % u for u in range(8)])
        for d in range(2):
            for h in range(4):
                p = 32 * d + h
                u = d * 4 + h
                S.op('dve', lambda e, p=p, u=u: e.tensor_scalar(
                    out=SEL[:, u * 128:(u + 1) * 128], in0=ones64[:], scalar1=g.idf[0:64, p:p + 1], scalar2=None, op0=ALU.mult),
                    R=['ones64', 'idf'], W=['SEL'])
        SELb = sbt('SELb', [64, 8 * 128], BF16)
        S.op('dve', lambda e: e.tensor_copy(out=SELb[:], in_=SEL[:]), R=['SEL'], W=['SELb'])
        gbo = PF['gb'][0] + l * 2
        for (dst, dkey, r0, bcol) in [(GI, 'gGI', 0, gbo), (GF, 'gGF', 8, gbo + 1)]:
            S.op('pool', lambda e: e.memset(TMP[:], 0.0), W=['gTMP'])
            S.dma('sp', TMP[0:4, :], g.ZG[r0:r0 + 4, :], W=['gTMP'])
            S.dma('sp', TMP[32:36, :], g.ZG[r0 + 4:r0 + 8, :], W=['gTMP'])
            bias = g.pft[0:64, bcol:bcol + 1]
            S.op('dve', lambda e, dst=dst, bias=bias: e.tensor_scalar(
                out=dst[0:32, :], in0=TMP[0:32, :], scalar1=bias[0:32, :], scalar2=None, op0=ALU.add),
                R=['gTMP', 'pft'], W=[dkey])
            S.op('dve', lambda e, dst=dst, bias=bias: e.tensor_scalar(
                out=dst[32:64, 0:CTX], in0=TMP[32:64, 0:CTX][:, ::-1], scalar1=bias[32:64, :], scalar2=None, op0=ALU.add),
                R=['gTMP', 'pft'], W=[dkey])
            S.op('dve', lambda e, dst=dst, bias=bias: e.tensor_scalar(
                out=dst[32:64, CTX:T], in0=TMP[32:64, CTX:T][:, ::-1], scalar1=bias[32:64, :], scalar2=None, op0=ALU.add),
                R=['gTMP', 'pft'], W=[dkey])
        S.op('act', lambda e: e.activation(out=GF[:], in_=GF[:], func=AF.Exp, scale=-1.0), R=['gGF'], W=['gGF'])
        S.op('act', lambda e: e.activation(out=GF[:], in_=GF[:], func=AF.Ln, bias=onec[:], scale=1.0), R=['gGF', 'onec2'], W=['gGF'])

        BN = Ring(nc, st, 'mBN', [64, 128], F32, 2)
        AA = Ring(nc, st, 'mAA', [64, 128], F32, 2)
        MX = Ring(nc, st, 'mMX', [64, 128], F32, 2)
        NML = Ring(nc, st, 'mNML', [64, 1], F32, 2)
        TE = Ring(nc, st, 'mTE', [64, 128], F32, 2)
        RB = Ring(nc, st, 'mRB', [64, 258], F32, 3)
        CL = Ring(nc, st, 'mCL', [64, 3 * 128], F32, 3)
        RBH = Ring(nc, st, 'mRBH', [64, 2 * 258], BF16, 3)
        COL = Ring(nc, st, 'mCOL', [128, 192], F32, 4)
        QK = Ring(nc, st, 'mQK', [128, 8 * 128], BF16, 5)
        KV = Ring(nc, st, 'mKV', [128, 1024], BF16, 7)
        VXs = [sbt('mVX%d' % i, [128, 4 * 130], BF16) for i in range(6)]
        for i in range(6):
            S.op('dve', lambda e, i=i: e.memset(VXs[i][:], 0.0), W=['mVX%d' % i])
            S.op('dve', lambda e, i=i: e.memset(VXs[i][:].rearrange("p (h n) -> p h n", h=4)[:, :, 128:129], 1.0), W=['mVX%d' % i])
        vxi = [0]
        EE = Ring(nc, st, 'mEE', [128, 128], F32, 4)
        EM = Ring(nc, st, 'mEM', [128, 128], F32, 4)
        PT = Ring(nc, st, 'mPT', [128, 128], BF16, 16)
        QS = Ring(nc, st, 'mQS', [128, 128], BF16, 16)
        KW = Ring(nc, st, 'mKW', [128, 128], BF16, 4)
        DN = Ring(nc, st, 'mDN', [128, 1], F32, 8)
        RC = Ring(nc, st, 'mRC', [128, 1], F32, 8)
        DC = Ring(nc, st, 'mDC', [128, 1], F32, 16)
        HO = Ring(nc, st, 'mHO', [128, 512], F32, 4)
        WS = Ring(nc, st, 'mWS', [128, 12288], BF16, 2)

        uc = [0]
        iters = []
        for i in range(NT):
            st0, st1, st2 = [], [], []
            S.rec = st0
            ld = {}
            for d in range(2):
                j = i if d == 0 else ((1 - i) if i < 2 else (35 - i))
                qk, qkk = QK.next()
                kv, kvk = KV.next()
                S.dma('sp', qk[:].rearrange("p (c n) -> p c n", c=8),
                      g.ZQK[:, 128 * j:128 * j + 128].rearrange("(c p) n -> p c n", p=128), W=[qkk])
                S.dma('sp', kv[:], g.ZKV[128 * j:128 * j + 128, :], W=[kvk])
                ld[d] = (j, qk, qkk, kv, kvk)
            cs = slice(128 * i, 128 * i + 128)
            bn, bnk = BN.next()
            aa, aak = AA.next()
            mx, mxk = MX.next()
            nml, nmlk = NML.next()
            te, tek = TE.next()
            rb, rbk = RB.next()
            cl, clk = CL.next()
            S.op('dve', lambda e, bn=bn, cs=cs: e.tensor_tensor_scan(
                out=bn[:], data0=ones64[:], data1=GF[:, cs], initial=0.0, op0=ALU.mult, op1=ALU.add),
                R=['ones64', 'gGF'], W=[bnk])
            S.op('dve', lambda e, aa=aa, bn=bn, cs=cs: e.tensor_tensor(out=aa[:], in0=GI[:, cs], in1=bn[:], op=ALU.add),
                 R=['gGI', bnk], W=[aak])
            S.op('dve', lambda e, mx=mx, aa=aa, i=i: e.tensor_tensor_scan(
                out=mx[:], data0=aa[:], data1=aa[:], initial=mcar[:, i:i + 1], op0=ALU.max, op1=ALU.max),
                R=[aak, 'mcar'], W=[mxk])
            S.op('dve', lambda e, mx=mx, bn=bn, i=i: e.tensor_tensor(
                out=mcar[:, i + 1:i + 2], in0=mx[:, 127:128], in1=bn[:, 127:128], op=ALU.subtract),
                R=[mxk, bnk, 'mcar'], W=['mcar'])
            S.op('dve', lambda e, nml=nml, mx=mx: e.tensor_scalar(
                out=nml[:], in0=mx[:, 127:128], scalar1=-1.0, scalar2=None, op0=ALU.mult), R=[mxk], W=[nmlk])
            S.op('pool', lambda e, te=te, bn=bn, mx=mx: e.tensor_tensor(out=te[:], in0=bn[:], in1=mx[:], op=ALU.subtract),
                 R=[bnk, mxk], W=[tek])
            for (p0, p1, rv) in [(0, 32, False), (32, 64, True)]:
                def o(ap, rv=rv):
                    return ap[:, ::-1] if rv else ap
                S.op('dve', lambda e, p0=p0, p1=p1, o=o, rb=rb, mx=mx: e.tensor_scalar(
                    out=o(rb[p0:p1, 0:128]), in0=mx[p0:p1, :], scalar1=-1.0, scalar2=None, op0=ALU.mult),
                    R=[mxk], W=[rbk])
                S.op('act', lambda e, p0=p0, p1=p1, o=o, rb=rb, mx=mx, i=i: e.activation(
                    out=o(rb[p0:p1, 128:256]), in_=mx[p0:p1, :], func=AF.Exp, scale=-1.0, bias=mcar[p0:p1, i:i + 1]),
                    R=[mxk, 'mcar'], W=[rbk])
                S.op('pool', lambda e, p0=p0, p1=p1, o=o, cl=cl, aa=aa: e.tensor_copy(out=o(cl[p0:p1, 0:128]), in_=aa[p0:p1, :]),
                     R=[aak], W=[clk])
                S.op('act', lambda e, p0=p0, p1=p1, o=o, cl=cl, aa=aa, nml=nml: e.activation(
                    out=o(cl[p0:p1, 128:256]), in_=aa[p0:p1, :], func=AF.Exp, scale=1.0, bias=nml[p0:p1, :]),
                    R=[aak, nmlk], W=[clk])
                S.op('act', lambda e, p0=p0, p1=p1, o=o, cl=cl, te=te: e.activation(
                    out=o(cl[p0:p1, 256:384]), in_=te[p0:p1, :], func=AF.Exp), R=[tek], W=[clk])
            S.op('act', lambda e, rb=rb, nml=nml, i=i: e.activation(
                out=rb[:, 256:257], in_=mcar[:, i:i + 1], func=AF.Exp, scale=1.0, bias=nml[:]), R=['mcar', nmlk], W=[rbk])
            S.op('act', lambda e, rb=rb, nml=nml, i=i: e.activation(
                out=rb[:, 257:258], in_=mcar[:, i:i + 1], func=AF.Exp, scale=1.0, bias=nml[:]), R=['mcar', nmlk], W=[rbk])
            rbh, rbhk = RBH.next()
            S.op('pool', lambda e, rbh=rbh, rb=rb: e.tensor_copy(out=rbh[:, 0:258], in_=rb[:, 0:258]), R=[rbk], W=[rbhk])
            S.op('pool', lambda e, rbh=rbh, rb=rb: e.tensor_tensor(out=rbh[:, 258:516], in0=rb[:, 0:258], in1=rbh[:, 0:258], op=ALU.subtract),
                 R=[rbk, rbhk], W=[rbhk])
            pT = g.PS[7][:, 0:192]
            pTk = ('cT', 0)

            def ftr(e, pT=pT, cl=cl):
                for q in range(3):
                    ins = e.transpose(out=pT[:, q * 64:(q + 1) * 64], in_=cl[:, q * 128:(q + 1) * 128], identity=g.idf[0:64, 0:64])
                return ins
            S.op('pe', ftr, R=[clk, 'idf'], W=[pTk])
            col, colk = COL.next()
            S.op('act', lambda e, col=col, pT=pT: e.copy(out=col[:], in_=pT[:, 0:192]), R=[pTk], W=[colk])
            for d in range(2):
                j, qk, qkk, kv, kvk = ld[d]
                S.rec = st1
                vx = VXs[vxi[0]]
                vxk = 'mVX%d' % vxi[0]
                vxi[0] = (vxi[0] + 1) % len(VXs)
                S.op('pool', lambda e, vx=vx, kv=kv: e.tensor_copy(
                    out=vx[:].rearrange("p (h n) -> p h n", h=4)[:, :, 0:128],
                    in_=kv[:, 512:1024].rearrange("p (h n) -> p h n", h=4)), R=[kvk], W=[vxk])
                need_out = not (last and j < 2)
                ho, hok = HO.next()
                tails = []
                for h in range(4):
                    u = d * 4 + h
                    pc = 32 * d + h
                    ui = uc[0] % 3
                    uc[0] += 1
                    ps_s, sk = g.PS[ui][:, 0:128], ('cP', ui)
                    ps_b, bk = g.PS[ui][:, 128:386], ('cP', ui)
                    ps_n, nk = g.PS[3 + h][:, 0:130], ('cQ', h)
                    ps_c, ck = g.PS[3 + h][:, 130:260], ('cQ', h)
                    S.rec = st1
                    S.op('pe', lambda e, ps_s=ps_s, qk=qk, h=h: e.matmul(
                        ps_s[:, 0:128], lhsT=qk[:, (4 + h) * 128:(5 + h) * 128], rhs=qk[:, h * 128:(h + 1) * 128], start=True, stop=True),
                        R=[qkk], W=[sk])
                    def fb(e, ps_b=ps_b, rbh=rbh, u=u):
                        e.matmul(ps_b[:, 0:258], lhsT=SELb[:, u * 128:(u + 1) * 128], rhs=rbh[:, 0:258], start=True, stop=False)
                        return e.matmul(ps_b[:, 0:258], lhsT=SELb[:, u * 128:(u + 1) * 128], rhs=rbh[:, 258:516], start=False, stop=True)
                    S.op('pe', fb, R=[rbhk, 'SELb'], W=[bk])
                    ee, eek = EE.next()
                    em, emk = EM.next()
                    S.op('act', lambda e, ee=ee, ps_b=ps_b, col=col, pc=pc: e.activation(
                        out=ee[:], in_=ps_b[:, 0:128], func=AF.Exp, scale=1.0, bias=col[:, pc:pc + 1]), R=[bk, colk], W=[eek])
                    dc, dck = DC.next()
                    S.op('act', lambda e, dc=dc, ps_b=ps_b: e.copy(out=dc[:], in_=ps_b[:, 256:257]), R=[bk], W=[dck])
                    if d == 0:
                        S.op('pool', lambda e, em=em, ee=ee: e.affine_select(
                            out=em[:], in_=ee[:], pattern=[[1, 128]], compare_op=ALU.is_ge, fill=0.0, base=0, channel_multiplier=-1),
                            R=[eek], W=[emk])
                    else:
                        S.op('pool', lambda e, em=em, ee=ee: e.affine_select(
                            out=em[:], in_=ee[:], pattern=[[-1, 128]], compare_op=ALU.is_ge, fill=0.0, base=0, channel_multiplier=1),
                            R=[eek], W=[emk])
                    pt, ptk = PT.next()
                    S.op('dve', lambda e, pt=pt, ps_s=ps_s, em=em: e.scalar_tensor_tensor(
                        out=pt[:], in0=ps_s[:, 0:128], scalar=QS_SCALE, in1=em[:], op0=ALU.mult, op1=ALU.mult), R=[sk, emk], W=[ptk])
                    qs, qsk = QS.next()
                    S.op('dve', lambda e, qs=qs, qk=qk, h=h, ps_b=ps_b: e.scalar_tensor_tensor(
                        out=qs[:], in0=qk[:, h * 128:(h + 1) * 128], scalar=QS_SCALE, in1=ps_b[:, 128:256], op0=ALU.mult, op1=ALU.mult),
                        R=[qkk, bk], W=[qsk])
                    S.rec = st2
                    kw, kwk = KW.next()
                    S.op('act', lambda e, kw=kw, kv=kv, h=h, col=col, pc=pc: e.activation(
                        out=kw[:], in_=kv[:, h * 128:(h + 1) * 128], func=AF.Copy, scale=col[:, 64 + pc:65 + pc]), R=[kvk, colk], W=[kwk])

                    def fnum(e, ps_n=ps_n, pt=pt, qs=qs, vx=vx, h=h, u=u):
                        e.matmul(ps_n[:, 0:130], lhsT=pt[:], rhs=vx[:, h * 130:(h + 1) * 130], start=True, stop=False)
                        return e.matmul(ps_n[:, 0:130], lhsT=qs[:], rhs=CB[:, u * 130:(u + 1) * 130], start=False, stop=True)
                    S.op('pe', fnum, R=[ptk, qsk, vxk, 'CB%d' % u], W=[nk])
                    S.op('pe', lambda e, ps_c=ps_c, kw=kw, vx=vx, h=h: e.matmul(
                        ps_c[:, 0:130], lhsT=kw[:], rhs=vx[:, h * 130:(h + 1) * 130], start=True, stop=True), R=[kwk, vxk], W=[ck])
                    if need_out:
                        dn, dnk = DN.next()
                        rc, rck = RC.next()
                        S.op('act', lambda e, dn=dn, ps_n=ps_n: e.activation(out=dn[:], in_=ps_n[:, 128:129], func=AF.Abs),
                             R=[nk], W=[dnk])
                    S.op('dve', lambda e, u=u, dc=dc, ps_c=ps_c: e.scalar_tensor_tensor(
                        out=C32[:, u * 130:(u + 1) * 130], in0=C32[:, u * 130:(u + 1) * 130], scalar=dc[:, 0:1], in1=ps_c[:, 0:130],
                        op0=ALU.mult, op1=ALU.add), R=['C32%d' % u, dck, ck], W=['C32%d' % u])
                    if need_out:
                        S.op('dve', lambda e, dn=dn, col=col, pc=pc: e.tensor_tensor(
                            out=dn[:], in0=dn[:], in1=col[:, 128 + pc:129 + pc], op=ALU.max), R=[dnk, colk], W=[dnk])
                        S.op('dve', lambda e, dn=dn, rc=rc: e.reciprocal(out=rc[:], in_=dn[:]), R=[dnk], W=[rck])
                        tails.append((h, ps_n, nk, rc, rck))
                    S.op('pool', lambda e, u=u: e.tensor_copy(out=CB[:, u * 130:(u + 1) * 130], in_=C32[:, u * 130:(u + 1) * 130]),
                         R=['C32%d' % u], W=['CB%d' % u])
                S.rec = st2
                for (h, ps_n, nk, rc, rck) in tails:
                    S.op('act', lambda e, ho=ho, h=h, ps_n=ps_n, rc=rc: e.activation(
                        out=ho[:, h * 128:(h + 1) * 128], in_=ps_n[:, 0:128], func=AF.Copy, scale=rc[:, 0:1]), R=[nk, rck], W=[hok])
                if need_out:
                    S.dma('sp', g.HM[d, 128 * j:128 * j + 128, :], ho[:], R=[hok])
            S.rec = None
            st3 = []
            if i < NE:
                S.rec = st3
                e_ = i
                W, wk = WS.next()
                S.dma('pool', W[:, 0:4096].rearrange("p (k n) -> p k n", k=8),
                      g.w_gate[l, e_].rearrange("(k p) n -> p k n", p=128), W=[(wk, 'g')])
                S.dma('pool', W[:, 4096:8192].rearrange("p (k n) -> p k n", k=8),
                      g.w_up[l, e_].rearrange("(k p) n -> p k n", p=128), W=[(wk, 'u')])
                S.dma('pool', W[:, 8192:12288].rearrange("p (k n) -> p k n", k=4),
                      g.w_down[l, e_].rearrange("(k p) n -> p k n", p=128), W=[(wk, 'd')])
                S.dma('sp', g.WB[e_ * 128:(e_ + 1) * 128, :], W[:], R=[(wk, 'g'), (wk, 'u'), (wk, 'd')])
                S.rec = None
            iters.append([st0, st1, st2, st3])
        S.pipeline(iters)
        S.barrier()
        S.flush()


def bcast_gate(g, gi, s):
    o = (gi * 2 + s) * 1024
    return g.gateb[:, o:o + 1024]


def phase_out(g, l):
    nc, S = g.nc, g.S
    last = (l == DEPTH - 1)
    with ExitStack() as st:
        sbt = lambda name, shape, dt=F32: st.enter_context(nc.sbuf_tensor(name + Ring.suffix, list(shape), dt))
        wout = sbt('wout', [128, 8 * 1024], BF16)
        wr32 = sbt('wr32', [128, 160])
        g.epsc = sbt('epsc3', [128, 1])
        S.op('dve', lambda e: e.memset(g.epsc[:], EPS), W=['epsc'])
        for k in range(8):
            S.dma('pool', wout[:, k * 1024:(k + 1) * 1024], g.w_out[l, k * 128:(k + 1) * 128, :], W=['wout'])
        S.dma('sp', wr32[:], g.wr[:, l * 160:(l + 1) * 160], W=['wr32'])
        H0 = Ring(nc, st, 'oH0', [128, 512], F32, 3)
        H1 = Ring(nc, st, 'oH1', [128, 512], F32, 3)
        OO = Ring(nc, st, 'oOO', [128, 512], F32, 3)
        HSm = Ring(nc, st, 'oHS', [128, 512], F32, 2)
        XN = Ring(nc, st, 'oXN', [128, 512], F32, 2)
        MMb = Ring(nc, st, 'oMM', [128, 512], BF16, 3)
        ST6 = Ring(nc, st, 'oST6', [128, 24], F32, 2)
        MV = Ring(nc, st, 'oMV', [128, 8], F32, 2)
        SD = Ring(nc, st, 'oSD', [128, 4], F32, 2)
        RS = Ring(nc, st, 'oRS', [128, 4], F32, 2)
        MT = Ring(nc, st, 'oMT', [128, 1024], BF16, 4)
        HT = Ring(nc, st, 'oHT', [128, 1024], F32, 4)
        TM = Ring(nc, st, 'oTM', [128, 1024], F32, 2)
        HN = Ring(nc, st, 'oHN', [128, 1024], F32, 3)
        HN2 = Ring(nc, st, 'oHN2', [128, 1024], F32, 3)
        V32 = Ring(nc, st, 'oV32', [128, 1024], F32, 2)
        V16 = Ring(nc, st, 'oV16', [128, 1024], BF16, 3)
        junk = sbt('ojunk', [128, 1024], BF16)
        SSQ = Ring(nc, st, 'oSSQ', [128, 1], F32, 3)
        RSTD = Ring(nc, st, 'oRSTD', [128, 1], F32, 3)
        RT = Ring(nc, st, 'oRT', [128, 64], F32, 3)
        CWt = Ring(nc, st, 'oCW', [128, 16], F32, 3)
        VTKr = Ring(nc, st, 'oVTK', [128, 1032], BF16, 3)
        bro = PF['brb'][0] + l * 20
        ngo = PF['mnorm_g'][0] + l * 4
        iters = []
        jstart = 2 if last else 0
        for j in range(jstart, NT):
            st0, st1, st2, st3, st4 = [], [], [], [], []
            S.rec = st0
            s = 1 if j < 2 else 0
            rows = slice(128 * j, 128 * j + 128)
            h0, h0k = H0.next()
            h1, h1k = H1.next()
            oo, ook = OO.next()
            S.dma('sp', h0[:], g.HM[0, rows, :], W=[h0k])
            S.dma('sp', h1[:], g.HM[1, rows, :], W=[h1k])
            S.dma('sp', oo[:], g.ZO[rows, :], W=[ook])
            mt, mtk = MT.next()
            S.dma('sp', mt[:, 0:256].rearrange("p (c n) -> p c n", c=2),
                  g.MIX[0:256, rows].rearrange("(c p) n -> p c n", p=128), W=[mtk])
            S.dma('sp', mt[:, 768:1024].rearrange("p (c n) -> p c n", c=2),
                  g.MIX[768:1024, rows].rearrange("(c p) n -> p c n", p=128), W=[mtk])
            ht, htk = HT.next()
            load_h_tile(g, l, j, ht, htk, l == 0)
            S.rec = st1
            hs, hsk = HSm.next()
            S.op('pool', lambda e, hs=hs, h0=h0, h1=h1: e.tensor_tensor(out=hs[:], in0=h0[:], in1=h1[:], op=ALU.add), R=[h0k, h1k], W=[hsk])
            s6, s6k = ST6.next()
            mv, mvk = MV.next()
            for hh in range(4):
                S.op('dve', lambda e, s6=s6, hs=hs, hh=hh: e.bn_stats(out=s6[:, hh * 6:(hh + 1) * 6], in_=hs[:, hh * 128:(hh + 1) * 128]),
                     R=[hsk], W=[s6k])
                S.op('dve', lambda e, s6=s6, mv=mv, hh=hh: e.bn_aggr(out=mv[:, hh * 2:(hh + 1) * 2], in_=s6[:, hh * 6:(hh + 1) * 6]),
                     R=[s6k], W=[mvk])
            sd, sdk = SD.next()
            rs, rsk = RS.next()
            S.op('act', lambda e, sd=sd, mv=mv: e.activation(out=sd[:], in_=mv[:, 1:8:2], func=AF.Ln, bias=g.epsc[:], scale=1.0),
                 R=[mvk, 'epsc'], W=[sdk])
            S.op('act', lambda e, rs=rs, sd=sd: e.activation(out=rs[:], in_=sd[:], func=AF.Exp, scale=-0.5), R=[sdk], W=[rsk])
            xn, xnk = XN.next()
            for hh in range(4):
                S.op('dve', lambda e, xn=xn, hs=hs, mv=mv, rs=rs, hh=hh: e.tensor_scalar(
                    out=xn[:, hh * 128:(hh + 1) * 128], in0=hs[:, hh * 128:(hh + 1) * 128], scalar1=mv[:, 2 * hh:2 * hh + 1],
                    scalar2=rs[:, hh:hh + 1], op0=ALU.subtract, op1=ALU.mult), R=[hsk, mvk, rsk], W=[xnk])
            S.op('act', lambda e, oo=oo: e.activation(out=oo[:], in_=oo[:], func=AF.Sigmoid), R=[ook], W=[ook])
            mm, mmk = MMb.next()
            S.op('pool', lambda e, mm=mm, xn=xn, oo=oo: e.tensor_tensor(out=mm[:], in0=xn[:], in1=oo[:], op=ALU.mult), R=[xnk, ook], W=[mmk])
            ps, pk = g.PS[j % 2], 'ps%d' % (j % 2)
            psb = ps[:].bitcast(BF16)

            def ftr(e, mm=mm, psb=psb):
                for hh in range(4):
                    ins = e.transpose(out=psb[:, hh * 128:(hh + 1) * 128], in_=mm[:, hh * 128:(hh + 1) * 128], identity=g.idb[:])
                return ins
            S.op('pe', ftr, R=[mmk, 'idb'], W=[pk])
            for hh in range(4):
                S.op('act', lambda e, mt=mt, psb=psb, hh=hh: e.activation(
                    out=mt[:, (2 + hh) * 128:(3 + hh) * 128], in_=psb[:, hh * 128:(hh + 1) * 128], func=AF.Copy,
                    scale=g.pft[:, ngo + hh:ngo + hh + 1]), R=[pk, 'pft'], W=[mtk])
            S.rec = st2
            tm, tmk = TM.next()
            hn, hnk = HN.next()
            g1b = bcast_gate(g, 0, s)
            for half in range(2):
                py, pyk = g.PS[2 + half], 'ps%d' % (2 + half)

                def fy(e, py=py, mt=mt, half=half):
                    for k in range(8):
                        ins = e.matmul(py[:, :], lhsT=mt[:, k * 128:(k + 1) * 128],
                                       rhs=wout[:, k * 1024 + half * 512:k * 1024 + (half + 1) * 512], start=(k == 0), stop=(k == 7))
                    return ins
                S.op('pe', fy, R=[mtk, 'wout'], W=[pyk])
                hsl = slice(half * 512, (half + 1) * 512)
                S.op('dve', lambda e, tm=tm, py=py, hsl=hsl, g1b=g1b: e.tensor_tensor(out=tm[:, hsl], in0=py[:, :], in1=g1b[:, hsl], op=ALU.mult),
                     R=[pyk, 'gateb'], W=[tmk])
            S.op('pool', lambda e, hn=hn, tm=tm, ht=ht: e.tensor_tensor(out=hn[:], in0=tm[:], in1=ht[:], op=ALU.add), R=[tmk, htk], W=[hnk])
            for (rs_, p0, npart) in tile_rows(l, j):
                S.dma('sp', g.Hs[rs_, :], hn[p0:p0 + npart, :], R=[hnk])
            sq, sqk = SSQ.next()
            rstd, rstdk = RSTD.next()
            rms_rstd(g, hn, hnk, junk, 'ojunk', sq, sqk, rstd, rstdk)
            h2, h2k = HN2.next()
            S.op('dve', lambda e, h2=h2, hn=hn, rstd=rstd: e.tensor_scalar(out=h2[:], in0=hn[:], scalar1=rstd[:, 0:1], scalar2=None, op0=ALU.mult),
                 R=[hnk, rstdk], W=[h2k])
            S.rec = st3
            v32, v32k = V32.next()
            v16, v16k = V16.next()
            for half in range(2):
                pt_, ptk_ = g.PS[4 + half], 'ps%d' % (4 + half)

                def ft2(e, pt_=pt_, h2=h2, half=half):
                    for kk in range(4):
                        k = half * 4 + kk
                        ins = e.transpose(out=pt_[:, kk * 128:(kk + 1) * 128], in_=h2[:, k * 128:(k + 1) * 128], identity=g.idf[:])
                    return ins
                S.op('pe', ft2, R=[h2k, 'idf'], W=[ptk_])
                for kk in range(4):
                    k = half * 4 + kk
                    col = 2 * k + s
                    sh = 3 * 16 + col
                    if half == 0:
                        S.op('act', lambda e, v32=v32, pt_=pt_, k=k, kk=kk, col=col, sh=sh: e.activation(
                            out=v32[:, k * 128:(k + 1) * 128], in_=pt_[:, kk * 128:(kk + 1) * 128], func=AF.Identity,
                            scale=g.gs2[:, col:col + 1], bias=g.modT[:, sh:sh + 1]), R=[ptk_, 'gs', 'modT'], W=[v32k])
                    else:
                        S.op('dve', lambda e, v32=v32, pt_=pt_, k=k, kk=kk, col=col, sh=sh: e.tensor_scalar(
                            out=v32[:, k * 128:(k + 1) * 128], in0=pt_[:, kk * 128:(kk + 1) * 128],
                            scalar1=g.gs2[:, col:col + 1], scalar2=g.modT[:, sh:sh + 1], op0=ALU.mult, op1=ALU.add),
                            R=[ptk_, 'gs', 'modT'], W=[v32k])
            S.op('pool', lambda e, v16=v16, v32=v32: e.tensor_copy(out=v16[:], in_=v32[:]), R=[v32k], W=[v16k])
            pv, pvk = g.PS[7], 'ps7'
            pvb = pv[:].bitcast(BF16)

            def ftv(e, v16=v16, pvb=pvb):
                for k in range(8):
                    ins = e.transpose(out=pvb[:, k * 128:(k + 1) * 128], in_=v16[:, k * 128:(k + 1) * 128], identity=g.idb[:])
                return ins
            S.op('pe', ftv, R=[v16k, 'idb'], W=[pvk])
            vtk, vtkk = VTKr.next()
            S.op('act', lambda e, vtk=vtk, pvb=pvb: e.copy(out=vtk[:, 0:1024], in_=pvb[:, 0:1024]), R=[pvk], W=[vtkk])
            pl, plk = g.PS[6], 'ps6'

            def frt(e, pl=pl, v32=v32):
                for k in range(8):
                    ins = e.matmul(pl[:, 0:20], lhsT=v32[:, k * 128:(k + 1) * 128], rhs=wr32[:, k * 20:(k + 1) * 20],
                                   start=(k == 0), stop=(k == 7))
                return ins
            S.op('pe', frt, R=[v32k, 'wr32'], W=[plk])
            rt, rtk = RT.next()
            cw, cwk = CWt.next()

            def route(rt, rtk, cw, cwk, pl, plk, vtk, vtkk):
                LG = rt[:, 0:20]; GMAX = rt[:, 20:21]; OHG = rt[:, 21:25]; NGM = rt[:, 25:26]; EG = rt[:, 26:30]
                SUMG = rt[:, 30:31]; PG = rt[:, 31:32]; ES = rt[:, 32:36]; M1 = rt[:, 36:37]; OH1 = rt[:, 37:41]
                ES2 = rt[:, 41:45]; M2 = rt[:, 45:46]; OH2 = rt[:, 46:50]; DD = rt[:, 50:51]; W1 = rt[:, 51:52]; W2 = rt[:, 52:53]
                CW4 = rt[:, 53:57]

                first = [True]

                def R_(fn, eng='dve'):
                    if first[0]:
                        S.op(eng, fn, R=[rtk, plk, 'pft'], W=[rtk])
                        first[0] = False
                    else:
                        S.op(eng, fn, R=[rtk, 'pft'], W=[rtk])
                R_(lambda e: e.tensor_tensor(out=LG, in0=pl[:, 0:20], in1=g.pft[:, bro:bro + 20], op=ALU.add))
                S.rec = st4
                R_(lambda e: e.tensor_reduce(out=GMAX, in_=LG[:, 0:4], axis=AX.X, op=ALU.max))
                R_(lambda e: e.tensor_scalar(out=OHG, in0=LG[:, 0:4], scalar1=GMAX, scalar2=None, op0=ALU.is_equal))
                R_(lambda e: e.tensor_scalar(out=NGM, in0=GMAX, scalar1=-1.0, scalar2=None, op0=ALU.mult))
                R_(lambda e: e.activation(out=EG, in_=LG[:, 0:4], func=AF.Exp, bias=NGM, scale=1.0, accum_out=SUMG), 'act')
                R_(lambda e: e.reciprocal(out=PG, in_=SUMG))
                R_(lambda e: e.tensor_scalar(out=ES, in0=LG[:, 4:8], scalar1=OHG[:, 0:1], scalar2=None, op0=ALU.mult))
                for gg in range(1, 4):
                    R_(lambda e, gg=gg: e.scalar_tensor_tensor(out=ES, in0=LG[:, 4 + 4 * gg:8 + 4 * gg], scalar=OHG[:, gg:gg + 1], in1=ES,
                                                               op0=ALU.mult, op1=ALU.add))
                R_(lambda e: e.tensor_reduce(out=M1, in_=ES, axis=AX.X, op=ALU.max))
                R_(lambda e: e.tensor_scalar(out=OH1, in0=ES, scalar1=M1, scalar2=None, op0=ALU.is_equal))
                R_(lambda e: e.scalar_tensor_tensor(out=ES2, in0=OH1, scalar=-1e30, in1=ES, op0=ALU.mult, op1=ALU.add))
                R_(lambda e: e.tensor_reduce(out=M2, in_=ES2, axis=AX.X, op=ALU.max))
                R_(lambda e: e.tensor_scalar(out=OH2, in0=ES2, scalar1=M2, scalar2=None, op0=ALU.is_equal))
                R_(lambda e: e.tensor_tensor(out=DD, in0=M1, in1=M2, op=ALU.subtract))
                R_(lambda e: e.activation(out=W2, in_=DD, func=AF.Exp, scale=-1.0), 'act')
                R_(lambda e: e.tensor_scalar(out=W1, in0=W2, scalar1=1.0, scalar2=None, op0=ALU.add))
                R_(lambda e: e.reciprocal(out=W1, in_=W1))
                R_(lambda e: e.tensor_tensor(out=W2, in0=W2, in1=W1, op=ALU.mult))
                R_(lambda e: e.tensor_tensor(out=W1, in0=W1, in1=PG, op=ALU.mult))
                R_(lambda e: e.tensor_tensor(out=W2, in0=W2, in1=PG, op=ALU.mult))
                R_(lambda e: e.tensor_scalar(out=CW4, in0=OH1, scalar1=W1, scalar2=None, op0=ALU.mult))
                R_(lambda e: e.scalar_tensor_tensor(out=CW4, in0=OH2, scalar=W2, in1=CW4, op0=ALU.mult, op1=ALU.add))
                S.op('dve', lambda e: e.tensor_copy(out=vtk[:, 1024:1032].bitcast(F32), in_=CW4), R=[rtk], W=[vtkk])
                S.op('dve', lambda e: e.tensor_copy(out=cw[:, 0:4], in_=OHG), R=[rtk], W=[cwk])

            route(rt, rtk, cw, cwk, pl, plk, vtk, vtkk)
            S.dma('sp', g.VTK[rows, :], vtk[:], R=[vtkk])
            S.dma('sp', g.GOH[rows, :], cw[:, 0:4], R=[cwk])
            if g.RTD is not None:
                S.dma('sp', g.RTD[rows, :], rt[:], R=[rtk])
            S.rec = None
            iters.append([st0, st1, st2, st3, st4])
        S.pipeline(iters)
        S.barrier()
        S.flush()


def phase_moe_sparse(g, l):
    nc, S = g.nc, g.S
    last = (l == DEPTH - 1)
    jstart = 2 if last else 0
    with ExitStack() as st:
        sbt = lambda name, shape, dt=F32: st.enter_context(nc.sbuf_tensor(name + Ring.suffix, list(shape), dt))
        posI = sbt('sPOSI', [128, NT], I32)
        widxI = sbt('sWIDX', [128, SL * 4], I32)
        g.epsc = sbt('epsc4', [128, 1])
        S.op('dve', lambda e: e.memset(g.epsc[:], EPS), W=['epsc'])
        with ExitStack() as st1:
            sb1 = lambda name, shape, dt=F32: st1.enter_context(nc.sbuf_tensor(name + Ring.suffix, list(shape), dt))
            N4 = NT * 4
            tri = sb1('sTRI', [128, 128])
            cst = sb1('sCST', [128, 32])
            goh = sb1('sGOH', [128, N4])
            cum = sb1('sCUM', [128, N4])
            totb = sb1('sTOT', [128, N4])
            incl = sb1('sINC', [128, N4])
            onesr = sb1('sONE', [128, NT])
            ng = sb1('sNG', [128, 4])
            cmp9 = sb1('sCMP', [128, 16])
            ns = sb1('sNS', [128, 4])
            gst = sb1('sGST', [128, 4])
            posf = sb1('sPOSF', [128, NT])
            gidf = sb1('sGID', [128, SL])
            gtmp = sb1('sGTMP', [128, SL])
            widxf = sb1('sWIDXF', [128, SL * 4])
            S.dma('sp', tri[:], g.tri[:, :], W=['sTRI'])
            S.dma('sp', cst[:], g.cst[:, :], W=['sCST'])
            S.dma('sp', goh[:].rearrange("p (j q) -> p j q", q=4), g.GOH[:, :].rearrange("(j p) q -> p j q", p=128), W=['sGOH'])
            if last:
                S.op('dve', lambda e: e.memset(goh[:, 0:8], 0.0), R=['sGOH'], W=['sGOH'])
            S.op('dve', lambda e: e.memset(onesr[:], 1.0), W=['sONE'])
            p1, p1k = g.PS[0], 'ps0'
            p2, p2k = g.PS[1], 'ps1'
            S.op('pe', lambda e: e.matmul(p1[:, 0:N4], lhsT=tri[:], rhs=goh[:], start=True, stop=True), R=['sTRI', 'sGOH'], W=[p1k])
            S.op('pe', lambda e: e.matmul(p2[:, 0:N4], lhsT=g.onesf[:], rhs=goh[:], start=True, stop=True), R=['onesf', 'sGOH'], W=[p2k])
            S.op('act', lambda e: e.copy(out=cum[:], in_=p1[:, 0:N4]), R=[p1k], W=['sCUM'])
            S.op('act', lambda e: e.copy(out=totb[:], in_=p2[:, 0:N4]), R=[p2k], W=['sTOT'])
            for q in range(4):
                S.op('dve', lambda e, q=q: e.tensor_tensor_scan(out=incl[:, q:N4:4], data0=onesr[:], data1=totb[:, q:N4:4], initial=0.0,
                                                                op0=ALU.mult, op1=ALU.add), R=['sONE', 'sTOT'], W=['sINC'])
            S.op('dve', lambda e: e.tensor_copy(out=ng[:], in_=incl[:, N4 - 4:N4]), R=['sINC'], W=['sNG'])
            for q in range(4):
                S.op('dve', lambda e, q=q: e.tensor_scalar(out=cmp9[:, 0:9], in0=cst[:, 0:9], scalar1=ng[:, q:q + 1], scalar2=None, op0=ALU.is_lt),
                     R=['sCST', 'sNG'], W=['sCMP'])
                S.op('dve', lambda e, q=q: e.tensor_reduce(out=ns[:, q:q + 1], in_=cmp9[:, 0:9], axis=AX.X, op=ALU.add), R=['sCMP'], W=['sNS'])
            S.op('dve', lambda e: e.tensor_scalar(out=ns[:], in0=ns[:], scalar1=512.0, scalar2=None, op0=ALU.mult), R=['sNS'], W=['sNS'])
            S.op('dve', lambda e: e.memset(gst[:, 0:1], 0.0), W=['sGST'])
            for q in range(1, 4):
                S.op('dve', lambda e, q=q: e.tensor_tensor(out=gst[:, q:q + 1], in0=gst[:, q - 1:q], in1=ns[:, q - 1:q], op=ALU.add),
                     R=['sGST', 'sNS'], W=['sGST'])
            S.op('dve', lambda e: e.tensor_tensor(out=incl[:], in0=incl[:], in1=totb[:], op=ALU.subtract), R=['sINC', 'sTOT'], W=['sINC'])
            S.op('dve', lambda e: e.tensor_tensor(out=cum[:], in0=cum[:], in1=incl[:], op=ALU.add), R=['sCUM', 'sINC'], W=['sCUM'])
            for q in range(4):
                S.op('dve', lambda e, q=q: e.tensor_scalar(out=cum[:, q:N4:4], in0=cum[:, q:N4:4], scalar1=gst[:, q:q + 1], scalar2=-1.0,
                                                           op0=ALU.add, op1=ALU.add), R=['sCUM', 'sGST'], W=['sCUM'])
            S.op('dve', lambda e: e.tensor_tensor(out=cum[:], in0=cum[:], in1=goh[:], op=ALU.mult), R=['sCUM', 'sGOH'], W=['sCUM'])
            S.op('dve', lambda e: e.tensor_reduce(out=posf[:], in_=cum[:].rearrange("p (j q) -> p j q", q=4), axis=AX.X, op=ALU.add),
                 R=['sCUM'], W=['sPOSF'])
            S.op('dve', lambda e: e.tensor_copy(out=posI[:], in_=posf[:]), R=['sPOSF'], W=['sPOSI'])
            S.op('dve', lambda e: e.memset(gidf[:], 0.0), W=['sGID'])
            for q in range(1, 4):
                S.op('dve', lambda e, q=q: e.tensor_scalar(out=gtmp[:], in0=cst[:, 9:9 + SL], scalar1=gst[:, q:q + 1], scalar2=None, op0=ALU.is_ge),
                     R=['sCST', 'sGST'], W=['sGTMP'])
                S.op('dve', lambda e: e.tensor_tensor(out=gidf[:], in0=gidf[:], in1=gtmp[:], op=ALU.add), R=['sGID', 'sGTMP'], W=['sGID'])
            for q in range(4):
                S.op('dve', lambda e, q=q: e.tensor_scalar(out=widxf[:, q:SL * 4:4], in0=gidf[:], scalar1=512.0, scalar2=cst[:, 22 + q:23 + q],
                                                           op0=ALU.mult, op1=ALU.add), R=['sGID', 'sCST'], W=['sWIDXF'])
            S.op('dve', lambda e: e.tensor_copy(out=widxI[:], in_=widxf[:]), R=['sWIDXF'], W=['sWIDX'])
            VL = Ring(nc, st1, 'sVL', [128, 1032], BF16, 8)
            for j in range(jstart, NT):
                vl, vlk = VL.next()
                S.dma('sp', vl[:], g.VTK[128 * j:128 * j + 128, :], W=[vlk])
                S.dma_fn('pool', lambda e, vl=vl, j=j: e.indirect_dma_start(
                    out=g.VS[:, :], out_offset=bass.IndirectOffsetOnAxis(ap=posI[:, j:j + 1], axis=0), in_=vl[:, :], in_offset=None,
                    bounds_check=S.reg(NCAP - 1), oob_is_err=False), R=[vlk, 'sPOSI'], W=[])
            S.barrier()
            S.flush()
        with ExitStack() as st2:
            VSr = Ring(nc, st2, 'sVS', [128, 1032], BF16, 8)
            VTr = Ring(nc, st2, 'sVT', [128, 8 * 512], BF16, 3)
            CWr = Ring(nc, st2, 'sCW', [128, 16], F32, 3)
            WW = Ring(nc, st2, 'sW', [128, 12288], BF16, 3)
            SG = Ring(nc, st2, 'sSG', [128, 512], F32, 3)
            AT = Ring(nc, st2, 'sAT', [128, 4 * 512], BF16, 3)
            YA = Ring(nc, st2, 'sYA', [128, 4 * 1024], F32, 2)
            iters = []
            cnt = [0, 0, 0, 0]
            for sl in range(SL - 1 if last else SL):
                s0, s1, s2 = [], [], []
                S.rec = s0
                vts = []
                for ti in range(4):
                    vs, vsk = VSr.next()
                    r0 = sl * 512 + ti * 128
                    S.dma('sp', vs[:], g.VS[r0:r0 + 128, :], W=[vsk])
                    vts.append((vs, vsk))
                S.rec = s1
                vT, vtk_ = VTr.next()
                cwr, cwrk = CWr.next()
                for ti in range(4):
                    vs, vsk = vts[ti]
                    pb_ = 0
                    pt_, ptk_ = g.PS[pb_], 'ps%d' % pb_
                    ptb = pt_[:].bitcast(BF16)

                    def ftr(e, vs=vs, ptb=ptb):
                        for k in range(8):
                            ins = e.transpose(out=ptb[:, k * 128:(k + 1) * 128], in_=vs[:, k * 128:(k + 1) * 128], identity=g.idb[:])
                        return ins
                    S.op('pe', ftr, R=[vsk, 'idb'], W=[ptk_])
                    eng = 'act' if ti % 2 == 0 else 'dve'
                    if eng == 'act':
                        S.op('act', lambda e, vT=vT, ptb=ptb, ti=ti: e.copy(
                            out=vT[:].rearrange("p (k n) -> p k n", k=8)[:, :, ti * 128:(ti + 1) * 128],
                            in_=ptb[:, 0:1024].rearrange("p (k n) -> p k n", k=8)), R=[ptk_], W=[vtk_])
                    else:
                        S.op('dve', lambda e, vT=vT, ptb=ptb, ti=ti: e.tensor_copy(
                            out=vT[:].rearrange("p (k n) -> p k n", k=8)[:, :, ti * 128:(ti + 1) * 128],
                            in_=ptb[:, 0:1024].rearrange("p (k n) -> p k n", k=8)), R=[ptk_], W=[vtk_])
                    S.op('act', lambda e, cwr=cwr, vs=vs, ti=ti: e.copy(out=cwr[:, ti * 4:(ti + 1) * 4], in_=vs[:, 1024:1032].bitcast(F32)),
                         R=[vsk], W=[cwrk])
                S.rec = s2
                ya, yak = YA.next()
                for q in range(4):
                    W, wk = WW.next()
                    S.dma_fn('pool', lambda e, W=W, sl=sl, q=q: e.indirect_dma_start(
                        out=W[:, :], out_offset=None, in_=g.WB[:, :],
                        in_offset=bass.IndirectOffsetOnAxis(ap=widxI[:, sl * 4 + q:sl * 4 + q + 1], axis=0),
                        bounds_check=S.reg(NE * 128 - 1), oob_is_err=False), R=['sWIDX'], W=[wk])
                    at, atk = AT.next()
                    for c in range(4):
                        pa, pak = g.PS[1 + cnt[1] % 2], 'ps%d' % (1 + cnt[1] % 2)
                        pb, pbk = g.PS[3 + cnt[1] % 2], 'ps%d' % (3 + cnt[1] % 2)
                        cnt[1] += 1

                        def fgu(e, pa=pa, pb=pb, W=W, c=c, vT=vT):
                            for k in range(8):
                                e.matmul(pa[:, :], lhsT=W[:, k * 512 + c * 128:k * 512 + (c + 1) * 128],
                                         rhs=vT[:, k * 512:(k + 1) * 512], start=(k == 0), stop=(k == 7))
                            for k in range(8):
                                ins = e.matmul(pb[:, :], lhsT=W[:, 4096 + k * 512 + c * 128:4096 + k * 512 + (c + 1) * 128],
                                               rhs=vT[:, k * 512:(k + 1) * 512], start=(k == 0), stop=(k == 7))
                            return ins
                        S.op('pe', fgu, R=[wk, vtk_], W=[pak, pbk])
                        sg, sgk = SG.next()
                        S.op('act', lambda e, sg=sg, pa=pa: e.activation(out=sg[:], in_=pa[:, :], func=AF.Silu), R=[pak], W=[sgk])
                        S.op('dve', lambda e, at=at, sg=sg, pb=pb, c=c: e.tensor_tensor(
                            out=at[:, c * 512:(c + 1) * 512], in0=pb[:, :], in1=sg[:], op=ALU.mult), R=[pbk, sgk], W=[atk])
                    for ts in range(4):
                        for half in range(2):
                            pd, pdk = g.PS[5 + cnt[2] % 3], 'ps%d' % (5 + cnt[2] % 3)
                            cnt[2] += 1

                            def fd(e, pd=pd, at=at, W=W, ts=ts, half=half):
                                for kc in range(4):
                                    ins = e.matmul(pd[:, :], lhsT=at[:, kc * 512 + ts * 128:kc * 512 + (ts + 1) * 128],
                                                   rhs=W[:, 8192 + kc * 1024 + half * 512:8192 + kc * 1024 + (half + 1) * 512],
                                                   start=(kc == 0), stop=(kc == 3))
                                return ins
                            S.op('pe', fd, R=[atk, wk], W=[pdk])
                            yv = ya[:, ts * 1024 + half * 512:ts * 1024 + (half + 1) * 512]
                            cwc = cwr[:, ts * 4 + q:ts * 4 + q + 1]
                            if q == 0:
                                S.op('act', lambda e, yv=yv, pd=pd, cwc=cwc: e.activation(out=yv, in_=pd[:, :], func=AF.Copy, scale=cwc),
                                     R=[pdk, cwrk], W=[yak])
                            else:
                                S.op('dve', lambda e, yv=yv, pd=pd, cwc=cwc: e.scalar_tensor_tensor(out=yv, in0=pd[:, :], scalar=cwc, in1=yv, op0=ALU.mult, op1=ALU.add),
                                     R=[pdk, cwrk, yak], W=[yak])
                S.dma('sp', g.YS[sl * 512:(sl + 1) * 512, :].rearrange("(t p) n -> p t n", p=128),
                      ya[:].rearrange("p (t n) -> p t n", t=4), R=[yak])
                S.rec = None
                iters.append([s0, s1, s2])
            S.pipeline(iters)
            S.barrier()
            S.flush()
        with ExitStack() as st3:
            YG = Ring(nc, st3, 'sYG', [128, 1024], F32, 5)
            HT = Ring(nc, st3, 'sHT', [128, 1024], F32, 5)
            TM = Ring(nc, st3, 'sTM', [128, 1024], F32, 4)
            HN = Ring(nc, st3, 'sHN', [128, 1024], F32, 4)
            junk = st3.enter_context(nc.sbuf_tensor('sjunk' + Ring.suffix, [128, 1024], BF16))
            SSQ = Ring(nc, st3, 'sSSQ', [128, 1], F32, 4)
            RSTD = Ring(nc, st3, 'sRSTD', [128, 1], F32, 4)
            iters = []
            for j in range(jstart, NT):
                e0, e1, e2 = [], [], []
                S.rec = e0
                s = 1 if j < 2 else 0
                yg, ygk = YG.next()
                S.dma_fn('pool', lambda e, yg=yg, j=j: e.indirect_dma_start(
                    out=yg[:, :], out_offset=None, in_=g.YS[:, :],
                    in_offset=bass.IndirectOffsetOnAxis(ap=posI[:, j:j + 1], axis=0),
                    bounds_check=S.reg(NCAP - 1), oob_is_err=False), R=['sPOSI'], W=[ygk])
                ht, htk = HT.next()
                load_h_tile(g, l, j, ht, htk, False)
                S.rec = e1
                tm, tmk = TM.next()
                hn, hnk = HN.next()
                g2b = bcast_gate(g, 1, s)
                S.op('dve', lambda e, tm=tm, yg=yg, g2b=g2b: e.tensor_tensor(out=tm[:], in0=yg[:], in1=g2b, op=ALU.mult),
                     R=[ygk, 'gateb'], W=[tmk])
                S.op('dve', lambda e, hn=hn, tm=tm, ht=ht: e.tensor_tensor(out=hn[:], in0=tm[:], in1=ht[:], op=ALU.add), R=[tmk, htk], W=[hnk])
                if not last:
                    for (rs_, p0, npart) in tile_rows(l, j):
                        S.dma('sp', g.Hs[rs_, :], hn[p0:p0 + npart, :], R=[hnk])
                else:
                    S.rec = e2
                    sq, sqk = SSQ.next()
                    rstd, rstdk = RSTD.next()
                    rms_rstd(g, hn, hnk, junk, 'sjunk', sq, sqk, rstd, rstdk)
                    S.op('dve', lambda e, tm=tm, hn=hn, rstd=rstd: e.scalar_tensor_tensor(
                        out=tm[:], in0=hn[:], scalar=rstd[:, 0:1], in1=g.fgb[:], op0=ALU.mult, op1=ALU.mult), R=[hnk, rstdk, 'fgb'], W=[tmk])
                    for (rs_, p0, npart) in tile_rows(l, j):
                        ro = slice(rs_.start - CTX, rs_.stop - CTX, rs_.step)
                        S.dma('sp', g.out[ro, :], tm[p0:p0 + npart, :], R=[tmk])
                S.rec = None
                iters.append([e0, e1, e2])
            S.pipeline(iters)
            S.barrier()
            S.flush()


def col128(v):
    v = np.asarray(v, np.float32)
    return np.ascontiguousarray(v.reshape(-1, 128).T)


def make_pf(inp, b):
    pf = np.zeros((128, NPF), np.float32)

    def put(name, arr):
        o, n = PF[name]
        arr = np.asarray(arr, np.float32).reshape(128, -1)
        assert arr.shape[1] == n, (name, arr.shape, n)
        pf[:, o:o + n] = arr
    c2 = np.stack([col128(inp['c'][b]), col128(inp['c_ctx'])], axis=-1)
    put('c2', c2)
    put('b_mod', np.stack([col128(inp['b_mod'][l]) for l in range(DEPTH)], axis=1))
    put('g1', np.stack([col128(inp['norm1_g'][l]) for l in range(DEPTH)], axis=1))
    put('g2', np.stack([col128(inp['norm2_g'][l]) for l in range(DEPTH)], axis=1))
    put('fg', col128(inp['final_g']))
    cw = np.zeros((128, DEPTH, 2, 31), np.float32)
    for l in range(DEPTH):
        for c in range(2):
            cw[:, l, c, :] = inp['conv_w'][l][:, c * 128:(c + 1) * 128].T
    put('conv_w', cw)
    for nm, key in [('conv_b', 'conv_b'), ('conv_ln_g', 'conv_ln_g'), ('conv_ln_b', 'conv_ln_b')]:
        put(nm, np.stack([col128(inp[key][l]) for l in range(DEPTH)], axis=1))
    put('mnorm_g', np.stack([col128(inp['mlstm_norm_g'][l]) for l in range(DEPTH)], axis=1))
    lcw = np.zeros((128, DEPTH, 2, 2, 4), np.float32)
    for l in range(DEPTH):
        for d in range(2):
            for c in range(2):
                lcw[:, l, d, c, :] = inp['lru_conv_w'][l, d][:, c * 128:(c + 1) * 128].T
    put('lru_cw', lcw)
    for nm, key in [('lru_cb', 'lru_conv_b'), ('lru_ba', 'lru_b_a'), ('lru_bx', 'lru_b_x'), ('lru_lam', 'lru_lambda')]:
        a = np.zeros((128, DEPTH, 2, 2), np.float32)
        for l in range(DEPTH):
            for d in range(2):
                a[:, l, d, :] = col128(inp[key][l, d])
        put(nm, a)
    brb = np.zeros((128, DEPTH, 20), np.float32)
    for l in range(DEPTH):
        brb[:, l, 0:4] = inp['b_rg'][l][None, :]
        brb[:, l, 4:20] = inp['b_re'][l][None, :]
    put('brb', brb)
    gb = np.zeros((128, DEPTH, 2), np.float32)
    for l in range(DEPTH):
        for d in range(2):
            gb[32 * d:32 * d + 4, l, 0] = inp['mlstm_b_i'][l, d]
            gb[32 * d:32 * d + 4, l, 1] = inp['mlstm_b_f'][l, d]
    put('gb', gb)
    return pf


def make_shared(inp):
    sh = {}
    sh['ident'] = np.eye(128, dtype=np.float32)
    sh['tri'] = np.triu(np.ones((128, 128), np.float32))
    cst = np.zeros((128, 32), np.float32)
    cst[:, 0:9] = 512.0 * np.arange(9)[None, :]
    cst[:, 9:9 + SL] = 512.0 * np.arange(SL)[None, :]
    for e in range(4):
        cst[:, 22 + e] = e * 128 + np.arange(128)
    sh['cst'] = cst
    for k in ['w_mod', 'w_in', 'w_out', 'w_gate', 'w_up', 'w_down']:
        sh[k] = np.ascontiguousarray(np.asarray(inp[k], np.float32))
    lw = np.zeros((128, DEPTH, 2, 2, 2, 128), np.float32)
    for l in range(DEPTH):
        for d in range(2):
            for gi, key in enumerate(['lru_w_a', 'lru_w_x']):
                for c in range(2):
                    for a in range(2):
                        lw[64 * a:64 * a + 64, l, d, gi, c, 64 * a:64 * a + 64] = inp[key][l, d, 2 * c + a]
    sh['lruW'] = lw.reshape(128, -1)
    wr = np.zeros((128, DEPTH, 8, 20), np.float32)
    for l in range(DEPTH):
        wcat = np.concatenate([inp['w_rg'][l], inp['w_re'][l]], axis=1)
        wr[:, l] = wcat.reshape(8, 128, 20).transpose(1, 0, 2)
    sh['wr'] = wr.reshape(128, -1)
    return sh


def kernel(**inp):
    inp = {k: np.asarray(v) for k, v in inp.items()}
    nc = build()
    sh = make_shared(inp)
    in_maps = []
    for b in range(8):
        m = dict(sh)
        m['x'] = np.ascontiguousarray(inp['x'][b], np.float32)
        m['ctx'] = np.ascontiguousarray(inp['ctx'][b], np.float32)
        m['pf'] = make_pf(inp, b)
        in_maps.append(m)
    res = run_bass_kernel_spmd(nc, in_maps, core_ids=list(range(8)))
    return np.stack([np.asarray(r['out'], np.float32) for r in res.results], axis=0)
```

```python
import numpy as np
from contextlib import ExitStack
import concourse.bass as bass
import concourse.mybir as mybir
from concourse.bass_utils import run_bass_kernel_spmd

F32 = mybir.dt.float32
BF16 = mybir.dt.bfloat16
AF = mybir.ActivationFunctionType
ALU = mybir.AluOpType
AX = mybir.AxisListType

D = 1024
SEQ = 4096
CTX = 256
T = SEQ + CTX
NT = T // 128
DEPTH = 2
GRID_W = 64
EPS = 1e-6
IN_COLS = 3088
NE = 16
DE = 512
SL = 12
NCAP = SL * 512
I32 = mybir.dt.int32
SAME_ENG_SYNC = True
SAME_ENG_DIST = 0

PF = {}
_off = 0
def _pf(name, n):
    global _off
    PF[name] = (_off, n)
    _off += n
_pf('c2', 16)
_pf('b_mod', 2 * 48)
_pf('g1', 2 * 8)
_pf('g2', 2 * 8)
_pf('fg', 8)
_pf('conv_w', 2 * 2 * 31)
_pf('conv_b', 2 * 2)
_pf('conv_ln_g', 2 * 2)
_pf('conv_ln_b', 2 * 2)
_pf('mnorm_g', 2 * 4)
_pf('lru_cw', 2 * 2 * 2 * 4)
_pf('lru_cb', 2 * 2 * 2)
_pf('lru_ba', 2 * 2 * 2)
_pf('lru_bx', 2 * 2 * 2)
_pf('lru_lam', 2 * 2 * 2)
_pf('brb', 2 * 20)
_pf('gb', 2 * 2)
NPF = _off


class Sched:
    ENG = ['pe', 'act', 'dve', 'pool', 'sp']

    def __init__(self, nc, stack, nds=14):
        self.nc = nc
        self.e = {}
        for n in self.ENG:
            self.e[n] = dict(sem=stack.enter_context(nc.semaphore('sem_' + n)), cnt=0, ops=[], seen={})
        self.dsq = {}
        for q, cnt in [('sp', 12), ('pool', 8), ('act', 4), ('dve', 2), ('pe', 2)]:
            self.dsq[q] = dict(i=0, lst=[dict(sem=stack.enter_context(nc.semaphore('ds_%s%d' % (q, i))), n=0, name='ds_%s%d' % (q, i))
                                        for i in range(cnt)])
        self.ds = [d for q in self.dsq.values() for d in q['lst']]
        self.lastw = {}
        self.rd = {}
        self.nops = 0
        self.rec = None

    def reg(self, val):
        if val not in self.regs:
            self.regs[val] = self.cur_e.to_reg(val)
        return self.regs[val]

    def replay(self, lst):
        assert self.rec is None
        for (kind, en, a, R, W, kw) in lst:
            if kind == 'op':
                self.op(en, a, R, W)
            else:
                self.dma(en, a[0], a[1], R, W, **kw)

    def pipeline(self, iters):
        n = len(iters)
        ns = max(len(x) for x in iters)
        for t in range(n + ns - 1):
            act = []
            for st in range(ns - 1, -1, -1):
                it = t - st
                if 0 <= it < n and st < len(iters[it]) and iters[it][st]:
                    act.append(iters[it][st])
            tot = sum(len(x) for x in act)
            pos = [0] * len(act)
            for k in range(tot):
                best, bi = None, -1
                for i, lst in enumerate(act):
                    if pos[i] < len(lst):
                        f = (pos[i] + 0.5) / len(lst)
                        if best is None or f < best:
                            best, bi = f, i
                self.replay(act[bi][pos[bi]:pos[bi] + 1])
                pos[bi] += 1

    @staticmethod
    def _excl(k):
        return (isinstance(k, tuple) and k and k[0] in ('cP', 'cQ', 'cT')) or (isinstance(k, str) and len(k) == 3 and k.startswith('ps'))

    def _deps(self, R, W):
        if any(self._excl(r) for r in R):
            W = tuple(W) + tuple(r for r in R if self._excl(r))
        deps = []
        for r in R:
            if r in self.lastw:
                deps.append(self.lastw[r])
        for w in W:
            if w in self.lastw:
                deps.append(self.lastw[w])
            deps.extend(self.rd.get(w, ()))
        return deps

    def _commit(self, R, W, tok):
        if any(self._excl(r) for r in R):
            W = tuple(W) + tuple(r for r in R if self._excl(r))
            R = tuple(r for r in R if not self._excl(r))
        for r in R:
            self.rd.setdefault(r, []).append(tok)
        for w in W:
            self.lastw[w] = tok
            self.rd[w] = []

    def _waits(self, en, deps):
        E = self.e[en]
        out = []
        for (sname, sem, val, src) in deps:
            if src == en and en == 'pe':
                continue
            if src == en and (not SAME_ENG_SYNC) and en in ('act', 'dve'):
                continue
            if src == en and en in ('act', 'dve') and SAME_ENG_DIST > 0 and (E['cnt'] + 1 - val) >= SAME_ENG_DIST:
                continue
            if E['seen'].get(sname, 0) >= val:
                continue
            E['seen'][sname] = val
            out.append((sem, val))
        return out

    def op(self, en, fn, R=(), W=()):
        if self.rec is not None:
            self.rec.append(('op', en, fn, tuple(R), tuple(W), None))
            return
        E = self.e[en]
        waits = self._waits(en, self._deps(R, W))
        E['cnt'] += 1
        tok = ('sem_' + en, E['sem'], E['cnt'], en)
        E['ops'].append((waits, fn, (E['sem'], 1)))
        self._commit(R, W, tok)
        self.nops += 1

    def dma_fn(self, q, fn, R=(), W=()):
        self.dma(q, None, None, R, W, _fn=fn)

    def dma(self, q, out, in_, R=(), W=(), **kw):
        if self.rec is not None:
            self.rec.append(('dma', q, (out, in_), tuple(R), tuple(W), kw))
            return
        E = self.e[q]
        dq = self.dsq[q]
        d = dq['lst'][dq['i']]
        dq['i'] = (dq['i'] + 1) % len(dq['lst'])
        deps = self._deps(R, W)
        if d['n'] > 0:
            deps.append((d['name'], d['sem'], 16 * d['n'], 'dma'))
        waits = self._waits(q, deps)
        d['n'] += 1
        tok = (d['name'], d['sem'], 16 * d['n'], 'dma')
        if '_fn' in kw:
            E['ops'].append((waits, kw['_fn'], (d['sem'], 16)))
        else:
            E['ops'].append((waits, (lambda e: e.dma_start(out=out, in_=in_, **kw)), (d['sem'], 16)))
        self._commit(R, W, tok)
        self.nops += 1

    def barrier(self):
        toks = [('sem_' + n, self.e[n]['sem'], self.e[n]['cnt'], n) for n in self.ENG if self.e[n]['cnt'] > 0]
        toks += [(d['name'], d['sem'], 16 * d['n'], 'dma') for d in self.ds if d['n'] > 0]
        for n in self.ENG:
            E = self.e[n]
            waits = []
            for (sname, sem, val, src) in toks:
                if src == n:
                    continue
                if E['seen'].get(sname, 0) >= val:
                    continue
                E['seen'][sname] = val
                waits.append((sem, val))
            E['ops'].append((waits, None, None))
        self.lastw = {}
        self.rd = {}

    def flush(self):
        nc = self.nc
        with nc.Block() as block:
            for n, dec in [('sp', block.sync), ('act', block.scalar), ('dve', block.vector),
                           ('pool', block.gpsimd), ('pe', block.tensor)]:
                ops = self.e[n]['ops']

                def body(e, ops=ops, n=n):
                    self.regs = {}
                    self.cur_e = e
                    for waits, fn, inc in ops:
                        for sem, val in waits:
                            e.wait_ge(sem, val)
                        if fn is not None:
                            ins = fn(e)
                            ins.then_inc(inc[0], inc[1])
                    for r in self.regs.values():
                        e.free_register(r)
                    self.regs = {}
                dec(body)
                self.e[n]['ops'] = []


class Ring:
    suffix = ''

    def __init__(self, nc, stack, name, shape, dtype, n):
        self.t = [stack.enter_context(nc.sbuf_tensor('%s%d%s' % (name, i, Ring.suffix), shape, dtype)) for i in range(n)]
        self.k = [('%s%d' % (name, i)) for i in range(n)]
        self.i = 0

    def next(self):
        i = self.i
        self.i = (i + 1) % len(self.t)
        return self.t[i], self.k[i]


def tile_rows(l, j):
    if j < 2:
        return [(slice(128 * j, 128 * j + 128), 0, 128)]
    jl = j - 2
    if l % 2 == 0:
        return [(slice(CTX + 128 * jl, CTX + 128 * jl + 128), 0, 128)]
    return [(slice(CTX + 2 * jl + wi, CTX + SEQ, GRID_W), 64 * wi, 64) for wi in range(2)]


class K:
    pass


def build(debug=None, stop_after=None):
    nc = bass.Bass("TRN2", target_bir_lowering=False)
    g = K()
    g.nc = nc
    g.debug = debug or ()
    g.stop_after = stop_after

    def din(name, shape, dt=F32):
        return nc.dram_tensor(name, list(shape), dt, kind="ExternalInput").ap()

    def dscr(name, shape, dt=F32):
        kind = "ExternalOutput" if name in g.debug else "Internal"
        return nc.dram_tensor(name, list(shape), dt, kind=kind).ap()

    g.x = din('x', [SEQ, D])
    g.ctx = din('ctx', [CTX, D])
    g.pf = din('pf', [128, NPF])
    g.ident = din('ident', [128, 128])
    g.w_mod = din('w_mod', [DEPTH, D, 6 * D])
    g.w_in = din('w_in', [DEPTH, D, IN_COLS])
    g.lruW = din('lruW', [128, DEPTH * 2 * 2 * 2 * 128])
    g.w_out = din('w_out', [DEPTH, D, D])
    g.wr = din('wr', [128, DEPTH * 8 * 20])
    g.w_gate = din('w_gate', [DEPTH, NE, D, DE])
    g.w_up = din('w_up', [DEPTH, NE, D, DE])
    g.w_down = din('w_down', [DEPTH, NE, DE, D])
    g.tri = din('tri', [128, 128])
    g.cst = din('cst', [128, 32])
    g.out = nc.dram_tensor('out', [SEQ, D], F32, kind="ExternalOutput").ap()

    g.Hs = dscr('Hs', [T, D])
    g.ZC = dscr('ZC', [512, T])
    g.ZQK = dscr('ZQK', [1024, T], BF16)
    g.ZG = dscr('ZG', [16, T])
    g.ZL = dscr('ZL', [512, T])
    g.ZKV = dscr('ZKV', [T, 1024], BF16)
    g.ZO = dscr('ZO', [T, 512])
    g.HM = dscr('HM', [2, T, 512])
    g.MIX = dscr('MIX', [1024, T], BF16)
    g.VT = dscr('VT', [1024, T], BF16)
    g.CW = dscr('CW', [T, 16])
    g.WB = dscr('WB', [NE * 128, 12288], BF16)
    g.VTK = dscr('VTK', [T, 1032], BF16)
    g.GOH = dscr('GOH', [T, 4])
    g.VS = dscr('VS', [NCAP, 1032], BF16)
    g.YS = dscr('YS', [NCAP, 1024])
    g.RTD = dscr('RTD', [T, 64]) if 'RTD' in g.debug else None

    with ExitStack() as gs:
        S = Sched(nc, gs)
        g.S = S
        sb = lambda name, shape, dt=F32: gs.enter_context(nc.sbuf_tensor(name, list(shape), dt))
        g.PS = [gs.enter_context(nc.psum_tensor('ps%d' % i, [128, 512], F32)) for i in range(8)]
        g.psi = 0
        g.pft = sb('pft', [128, NPF])
        g.idf = sb('idf', [128, 128])
        g.idb = sb('idb', [128, 128], BF16)
        g.onesf = sb('onesf', [128, 128])
        g.onesb = sb('onesb', [128, 128], BF16)
        g.sc = sb('sc', [128, 16])
        g.modT = sb('modT', [128, 96])
        g.gs1 = sb('gs1', [128, 16])
        g.gs2 = sb('gs2', [128, 16])
        g.gateb = sb('gateb', [128, 4 * 1024])
        g.fgb = sb('fgb', [128, 1024])

        S.dma('sp', g.pft[:], g.pf[:, :], W=['pft'])
        S.dma('sp', g.idf[:], g.ident[:, :], W=['idf'])
        S.op('dve', lambda e: e.tensor_copy(out=g.idb[:], in_=g.idf[:]), R=['idf'], W=['idb'])
        S.op('dve', lambda e: e.memset(g.onesf[:], 1.0), W=['onesf'])
        S.op('dve', lambda e: e.memset(g.onesb[:], 1.0), W=['onesb'])
        c0 = PF['c2'][0]
        S.op('act', lambda e: e.activation(out=g.sc[:], in_=g.pft[:, c0:c0 + 16], func=AF.Silu),
             R=['pft'], W=['sc'])

        for l in range(DEPTH):
            last = (l == DEPTH - 1)
            Ring.suffix = '_L%d' % l
            with ExitStack() as stA:
                g.win = stA.enter_context(nc.sbuf_tensor('win' + Ring.suffix, [128, 8 * IN_COLS], BF16))
                for k in range(8):
                    S.dma('pool', g.win[:, k * IN_COLS:(k + 1) * IN_COLS], g.w_in[l, k * 128:(k + 1) * 128, :], W=['win'])
                phase_mod(g, l)
                phase_proj(g, l)
            if g.stop_after == ('A', l):
                break
            phase_convlru(g, l)
            if g.stop_after == ('B', l):
                break
            phase_mlstm(g, l)
            if g.stop_after == ('C', l):
                break
            phase_out(g, l)
            if g.stop_after == ('D', l):
                break
            phase_moe_sparse(g, l)
            if g.stop_after == ('E', l):
                break
        S.barrier()
        S.flush()
    return nc


def psum(g):
    i = g.psi
    g.psi = (i + 1) % 8
    return g.PS[i], 'ps%d' % i


def pfc(g, name, idx=0, n=1):
    o = PF[name][0] + idx
    return g.pft[:, o:o + n]


def phase_mod(g, l):
    nc, S = g.nc, g.S
    with ExitStack() as st:
        wm = Ring(nc, st, 'wm', [128, 8 * 512], BF16, 3)
        scb = st.enter_context(nc.sbuf_tensor('scb' + Ring.suffix, [128, 16], BF16))
        S.op('dve', lambda e: e.tensor_copy(out=scb[:], in_=g.sc[:]), R=['sc'], W=['scb'])
        dg = Ring(nc, st, 'dg', [128, 128], F32, 2)
        tmp = st.enter_context(nc.sbuf_tensor('modtmp' + Ring.suffix, [128, 16], F32))
        ps, pk = psum(g)
        for cb in range(12):
            wt, wk = wm.next()
            S.dma('pool', wt[:].rearrange("p (k n) -> p k n", k=8),
                  g.w_mod[l, :, cb * 512:(cb + 1) * 512].rearrange("(k p) n -> p k n", p=128), W=[wk])
            for j in range(4):
                m = cb * 4 + j

                def f(e, wt=wt, j=j, m=m):
                    for k in range(8):
                        ins = e.matmul(ps[:, 2 * m:2 * m + 2], lhsT=wt[:, k * 512 + j * 128:k * 512 + (j + 1) * 128],
                                       rhs=scb[:, 2 * k:2 * k + 2], start=(k == 0), stop=(k == 7))
                    return ins
                S.op('pe', f, R=[wk, 'scb'], W=[pk])
        bo = PF['b_mod'][0] + l * 48
        for s in range(2):
            S.op('dve', lambda e, s=s: e.tensor_tensor(out=g.modT[:, s:96:2], in0=ps[:, s:96:2],
                                                       in1=g.pft[:, bo:bo + 48], op=ALU.add),
                 R=[pk, 'pft'], W=['modT'])
        for (gsT, gname, sec) in [(g.gs1, 'g1', 1), (g.gs2, 'g2', 4)]:
            go = PF[gname][0] + l * 8
            S.op('dve', lambda e, sec=sec: e.tensor_scalar(out=tmp[:], in0=g.modT[:, sec * 16:sec * 16 + 16],
                                                           scalar1=1.0, scalar2=None, op0=ALU.add),
                 R=['modT'], W=['modtmp'])
            for s in range(2):
                S.op('dve', lambda e, s=s, gsT=gsT, go=go: e.tensor_tensor(out=gsT[:, s:16:2], in0=tmp[:, s:16:2],
                                                                           in1=g.pft[:, go:go + 8], op=ALU.mult),
                     R=['modtmp', 'pft'], W=['gs'])
        for gi, sec in enumerate([2, 5]):
            for s in range(2):
                pa, pak = psum(g)
                pb, pbk = psum(g)
                for k in range(8):
                    dt_, dk = dg.next()
                    col = sec * 16 + 2 * k + s
                    S.op('dve', lambda e, dt_=dt_, col=col: e.tensor_scalar(
                        out=dt_[:], in0=g.idf[:], scalar1=g.modT[:, col:col + 1], scalar2=None, op0=ALU.mult),
                        R=['idf', 'modT'], W=[dk])
                    pp, ppk = (pa, pak) if k < 4 else (pb, pbk)
                    S.op('pe', lambda e, pp=pp, dt_=dt_, k=k: e.matmul(
                        pp[:, (k % 4) * 128:(k % 4 + 1) * 128], lhsT=g.onesf[:], rhs=dt_[:], start=True, stop=True),
                        R=[dk, 'onesf'], W=[ppk])
                o = (gi * 2 + s) * 1024
                S.op('act', lambda e, o=o, pa=pa: e.copy(out=g.gateb[:, o:o + 512], in_=pa[:]), R=[pak], W=['gateb'])
                S.op('act', lambda e, o=o, pb=pb: e.copy(out=g.gateb[:, o + 512:o + 1024], in_=pb[:]), R=[pbk], W=['gateb'])
        if l == DEPTH - 1:
            fo = PF['fg'][0]
            pa, pak = psum(g)
            pb, pbk = psum(g)
            for k in range(8):
                dt_, dk = dg.next()
                S.op('dve', lambda e, dt_=dt_, k=k: e.tensor_scalar(
                    out=dt_[:], in0=g.idf[:], scalar1=g.pft[:, fo + k:fo + k + 1], scalar2=None, op0=ALU.mult),
                    R=['idf', 'pft'], W=[dk])
                pp, ppk = (pa, pak) if k < 4 else (pb, pbk)
                S.op('pe', lambda e, pp=pp, dt_=dt_, k=k: e.matmul(
                    pp[:, (k % 4) * 128:(k % 4 + 1) * 128], lhsT=g.onesf[:], rhs=dt_[:], start=True, stop=True),
                    R=[dk, 'onesf'], W=[ppk])
            S.op('act', lambda e, pa=pa: e.copy(out=g.fgb[:, 0:512], in_=pa[:]), R=[pak], W=['fgb'])
            S.op('act', lambda e, pb=pb: e.copy(out=g.fgb[:, 512:1024], in_=pb[:]), R=[pbk], W=['fgb'])
        S.barrier()
        S.flush()


def load_h_tile(g, l, j, dst, dk, src_is_input, q='sp'):
    S = g.S
    for (rs, p0, npart) in tile_rows(l, j):
        if src_is_input:
            if j < 2:
                src = g.ctx[rs, :]
            else:
                src = g.x[slice(rs.start - CTX, rs.stop - CTX, rs.step), :]
        else:
            src = g.Hs[rs, :]
        S.dma(q, dst[p0:p0 + npart, :], src, W=[dk])


def rms_rstd(g, ht, hk, junk, jk, ssq, sk, rstd, rk):
    S = g.S
    S.op('act', lambda e: e.activation(out=junk[:], in_=ht[:], func=AF.Square, accum_out=ssq[:]),
         R=[hk], W=[jk, sk])
    S.op('act', lambda e: e.activation(out=ssq[:], in_=ssq[:], func=AF.Ln, scale=1.0 / D, bias=g.epsc[:]),
         R=[sk], W=[sk])
    S.op('act', lambda e: e.activation(out=rstd[:], in_=ssq[:], func=AF.Exp, scale=-0.5), R=[sk], W=[rk])


def phase_proj(g, l):
    nc, S = g.nc, g.S
    with ExitStack() as st:
        sbt = lambda name, shape, dt=F32: st.enter_context(nc.sbuf_tensor(name + Ring.suffix, list(shape), dt))
        win = g.win
        g.epsc = sbt('epsc', [128, 1])
        S.op('dve', lambda e: e.memset(g.epsc[:], EPS), W=['epsc'])
        hT = Ring(nc, st, 'hT', [128, 1024], F32, 5)
        hn = Ring(nc, st, 'hn', [128, 1024], BF16, 3)
        junk = sbt('junk', [128, 1024], BF16)
        ssq = Ring(nc, st, 'ssq', [128, 1], F32, 4)
        rstd = Ring(nc, st, 'rstd', [128, 1], F32, 4)
        uT = Ring(nc, st, 'uT', [128, 8 * 512], BF16, 3)
        stC = Ring(nc, st, 'stC', [128, 4 * 512], F32, 2)
        stL = Ring(nc, st, 'stL', [128, 4 * 512], F32, 2)
        stQ = Ring(nc, st, 'stQ', [128, 8 * 512], BF16, 2)
        stG = Ring(nc, st, 'stG', [16, 512], F32, 2)
        stKV = Ring(nc, st, 'stKV', [128, 1024], BF16, 3)
        stO = Ring(nc, st, 'stO', [128, 512], F32, 3)
        evi = [0]

        def evac(out, in_, R, W):
            evi[0] += 1
            if evi[0] % 2 == 0:
                S.op('act', lambda e: e.copy(out=out, in_=in_), R=R, W=W)
            else:
                S.op('dve', lambda e: e.tensor_copy(out=out, in_=in_), R=R, W=W)

        blocks = [(0, 2)] + [(2 + 4 * i, 4) for i in range(8)]
        iters = []
        pji = [0]
        tci = [0]
        for (j0, ntile) in blocks:
            st1, st2 = [], []
            S.rec = st1
            NB = ntile * 128
            t0 = j0 * 128
            ut, uk = uT.next()
            s = 1 if j0 == 0 else 0
            for ti in range(ntile):
                j = j0 + ti
                ht, hk = hT.next()
                load_h_tile(g, l, j, ht, hk, l == 0)
                sq, sk = ssq.next()
                rs_, rk = rstd.next()
                rms_rstd(g, ht, hk, junk, 'junk', sq, sk, rs_, rk)
                hb, hbk = hn.next()
                S.op('dve', lambda e, hb=hb, ht=ht, rs_=rs_: e.tensor_scalar(
                    out=hb[:], in0=ht[:], scalar1=rs_[:, 0:1], scalar2=None, op0=ALU.mult),
                    R=[hk, rk], W=[hbk])
                tb = 2 * (tci[0] % 2)
                tci[0] += 1
                psA, pkA = g.PS[tb], 'ps%d' % tb
                psB, pkB = g.PS[tb + 1], 'ps%d' % (tb + 1)
                psbA = psA[:].bitcast(BF16)
                psbB = psB[:].bitcast(BF16)

                def ftr(e, hb=hb, psbA=psbA, psbB=psbB):
                    for k in range(8):
                        dst = psbA if k < 4 else psbB
                        ins = e.transpose(out=dst[:, (k % 4) * 128:(k % 4 + 1) * 128], in_=hb[:, k * 128:(k + 1) * 128],
                                          identity=g.idb[:])
                    return ins
                S.op('pe', ftr, R=[hbk, 'idb'], W=[pkA, pkB])
                for k in range(8):
                    col = 2 * k + s
                    sh = 0 * 16 + col
                    eng = 'act' if k < 4 else 'dve'
                    psb = psbA if k < 4 else psbB
                    pk = pkA if k < 4 else pkB
                    if eng == 'act':
                        S.op('act', lambda e, k=k, ti=ti, ut=ut, psb=psb, col=col, sh=sh: e.activation(
                            out=ut[:, k * 512 + ti * 128:k * 512 + (ti + 1) * 128], in_=psb[:, (k % 4) * 128:(k % 4 + 1) * 128],
                            func=AF.Identity, scale=g.gs1[:, col:col + 1], bias=g.modT[:, sh:sh + 1]),
                            R=[pk, 'gs', 'modT'], W=[uk])
                    else:
                        S.op('dve', lambda e, k=k, ti=ti, ut=ut, psb=psb, col=col, sh=sh: e.tensor_scalar(
                            out=ut[:, k * 512 + ti * 128:k * 512 + (ti + 1) * 128], in0=psb[:, (k % 4) * 128:(k % 4 + 1) * 128],
                            scalar1=g.gs1[:, col:col + 1], scalar2=g.modT[:, sh:sh + 1], op0=ALU.mult, op1=ALU.add),
                            R=[pk, 'gs', 'modT'], W=[uk])
            S.rec = st2
            groups = [
                ('C', stC, g.ZC, [(0 + 128 * i, 128) for i in range(4)], F32),
                ('Q', stQ, g.ZQK, [(512 + 128 * i, 128) for i in range(8)], BF16),
                ('G', stG, g.ZG, [(2560, 16)], F32),
                ('L', stL, g.ZL, [(2576 + 128 * i, 128) for i in range(4)], F32),
            ]
            for (gname, ring, dst, chunks, dt_) in groups:
                stt, stk = ring.next()
                for ci, (c0, M) in enumerate(chunks):
                    ps, pk = g.PS[4 + pji[0] % 4], 'ps%d' % (4 + pji[0] % 4)
                    pji[0] += 1

                    def fmm(e, ps=ps, c0=c0, M=M, ut=ut, NB=NB):
                        for k in range(8):
                            ins = e.matmul(ps[0:M, 0:NB], lhsT=win[:, k * IN_COLS + c0:k * IN_COLS + c0 + M],
                                           rhs=ut[:, k * 512:k * 512 + NB], start=(k == 0), stop=(k == 7))
                        return ins
                    S.op('pe', fmm, R=['win', uk], W=[pk])
                    evac(stt[0:M, ci * 512:ci * 512 + NB], ps[0:M, 0:NB], [pk], [stk])
                nch = len(chunks)
                M = chunks[0][1]
                if nch == 1:
                    S.dma('pool', dst[0:M, t0:t0 + NB], stt[0:M, 0:NB], R=[stk])
                else:
                    S.dma('pool', dst[:, t0:t0 + NB].rearrange("(c p) n -> p c n", p=128),
                          stt[:].rearrange("p (c n) -> p c n", c=nch)[:, :, 0:NB], R=[stk])
            for ti in range(ntile):
                r0 = t0 + ti * 128
                kv, kvk = stKV.next()
                ot, ok = stO.next()
                for ci, c0 in enumerate([1024, 1536, 2048]):
                    ps, pk = g.PS[4 + pji[0] % 4], 'ps%d' % (4 + pji[0] % 4)
                    pji[0] += 1

                    def fmm(e, ps=ps, c0=c0, ut=ut, ti=ti):
                        for k in range(8):
                            ins = e.matmul(ps[:, :], lhsT=ut[:, k * 512 + ti * 128:k * 512 + (ti + 1) * 128],
                                           rhs=win[:, k * IN_COLS + c0:k * IN_COLS + c0 + 512],
                                           start=(k == 0), stop=(k == 7))
                        return ins
                    S.op('pe', fmm, R=['win', uk], W=[pk])
                    if ci < 2:
                        evac(kv[:, ci * 512:(ci + 1) * 512], ps[:, :], [pk], [kvk])
                    else:
                        evac(ot[:, :], ps[:, :], [pk], [ok])
                S.dma('pool', g.ZKV[r0:r0 + 128, :], kv[:], R=[kvk])
                S.dma('pool', g.ZO[r0:r0 + 128, :], ot[:], R=[ok])
            S.rec = None
            iters.append([st1, st2])
        S.pipeline(iters)
        S.barrier()
        S.flush()


def phase_convlru(g, l):
    phase_conv(g, l)
    phase_lru(g, l)


SEGS = [(0, CTX), (CTX, SEQ)]


def seg_blocks(s0, n):
    return [(s0 + o, min(512, n - o)) for o in range(0, n, 512)]


def phase_conv(g, l):
    nc, S = g.nc, g.S
    last = (l == DEPTH - 1)
    with ExitStack() as st:
        sbt = lambda name, shape, dt=F32: st.enter_context(nc.sbuf_tensor(name + Ring.suffix, list(shape), dt))
        DG = sbt('DG', [128, 2 * 31 * 128], BF16)
        UPl = [sbt('UPl%d' % c, [128, SEQ + 30], BF16) for c in range(2)]
        UPc = [sbt('UPc%d' % c, [128, CTX + 30], BF16) for c in range(2)]
        epsc = sbt('epsc2', [128, 1])
        S.op('dve', lambda e: e.memset(epsc[:], EPS), W=['epsc2'])
        vin = Ring(nc, st, 'vin', [128, 512], F32, 3)
        gin = Ring(nc, st, 'gin', [128, 512], F32, 3)
        sgr = Ring(nc, st, 'sgr', [128, 512], F32, 2)
        yb = [Ring(nc, st, 'yb%d' % c, [128, 512], F32, 3) for c in range(2)]
        y2 = [Ring(nc, st, 'y2%d' % c, [128, 512], F32, 3) for c in range(2)]
        mean = Ring(nc, st, 'mean', [128, 512], F32, 2)
        msq = Ring(nc, st, 'msq', [128, 512], F32, 2)
        var = Ring(nc, st, 'var', [128, 512], F32, 2)
        t1 = Ring(nc, st, 't1', [128, 512], F32, 3)
        t2 = Ring(nc, st, 't2', [128, 512], F32, 3)
        ost = Ring(nc, st, 'ost', [128, 2 * 512], BF16, 2)
        cwo = PF['conv_w'][0] + l * 62
        for c in range(2):
            for k in range(31):
                S.op('dve', lambda e, c=c, k=k: e.tensor_scalar(
                    out=DG[:, (c * 31 + k) * 128:(c * 31 + k + 1) * 128], in0=g.idf[:],
                    scalar1=g.pft[:, cwo + c * 31 + k:cwo + c * 31 + k + 1], scalar2=None, op0=ALU.mult),
                    R=['idf', 'pft'], W=['DG'])
        segs = [SEGS[1]] if last else SEGS
        for (s0, n) in segs:
            UP = UPc if s0 == 0 else UPl
            ukey = 'UPc' if s0 == 0 else 'UPl'
            for c in range(2):
                S.op('pool', lambda e, c=c, UP=UP: e.memset(UP[c][:, 0:15], 0.0), W=[ukey])
                S.op('pool', lambda e, c=c, UP=UP, n=n: e.memset(UP[c][:, 15 + n:30 + n], 0.0), W=[ukey])
            for (t0, nb) in seg_blocks(s0, n):
                for c in range(2):
                    vt, vk = vin.next()
                    gt, gk = gin.next()
                    S.dma('sp', vt[:, 0:nb], g.ZC[c * 128:(c + 1) * 128, t0:t0 + nb], W=[vk])
                    S.dma('sp', gt[:, 0:nb], g.ZC[256 + c * 128:256 + (c + 1) * 128, t0:t0 + nb], W=[gk])
                    sg, sgk = sgr.next()
                    S.op('act', lambda e, sg=sg, gt=gt, nb=nb: e.activation(out=sg[:, 0:nb], in_=gt[:, 0:nb], func=AF.Sigmoid),
                         R=[gk], W=[sgk])
                    o0 = 15 + t0 - s0
                    S.op('dve', lambda e, c=c, UP=UP, vt=vt, sg=sg, nb=nb, o0=o0: e.tensor_tensor(
                        out=UP[c][:, o0:o0 + nb], in0=vt[:, 0:nb], in1=sg[:, 0:nb], op=ALU.mult),
                        R=[vk, sgk], W=[ukey])
            iters = []
            for bi_, (t0, nb) in enumerate(seg_blocks(s0, n)):
                st1, st2 = [], []
                S.rec = st1
                o0 = t0 - s0
                ybt = []
                y2t = []
                for c in range(2):
                    ps, pk = g.PS[2 * (bi_ % 2) + c], 'ps%d' % (2 * (bi_ % 2) + c)

                    def fconv(e, ps=ps, c=c, UP=UP, o0=o0, nb=nb):
                        for k in range(31):
                            ins = e.matmul(ps[:, 0:nb], lhsT=DG[:, (c * 31 + k) * 128:(c * 31 + k + 1) * 128],
                                           rhs=UP[c][:, o0 + k:o0 + k + nb], start=(k == 0), stop=(k == 30))
                        return ins
                    S.op('pe', fconv, R=['DG', ukey], W=[pk])
                    a, ak = yb[c].next()
                    b, bk = y2[c].next()
                    cb = pfc(g, 'conv_b', l * 2 + c)
                    S.op('act', lambda e, a=a, ps=ps, cb=cb, nb=nb: e.activation(
                        out=a[:, 0:nb], in_=ps[:, 0:nb], func=AF.Identity, bias=cb, scale=1.0), R=[pk, 'pft'], W=[ak])
                    S.op('act', lambda e, b=b, ps=ps, cb=cb, nb=nb: e.activation(
                        out=b[:, 0:nb], in_=ps[:, 0:nb], func=AF.Square, bias=cb, scale=1.0), R=[pk, 'pft'], W=[bk])
                    ybt.append((a, ak))
                    y2t.append((b, bk))
                S.rec = st2
                p1, p1k = g.PS[4 + 2 * (bi_ % 2)], 'ps%d' % (4 + 2 * (bi_ % 2))
                p2, p2k = g.PS[5 + 2 * (bi_ % 2)], 'ps%d' % (5 + 2 * (bi_ % 2))

                def fst(e, p1=p1, p2=p2, ybt=ybt, y2t=y2t, nb=nb):
                    for c in range(2):
                        e.matmul(p1[:, 0:nb], lhsT=g.onesf[:], rhs=ybt[c][0][:, 0:nb], start=(c == 0), stop=(c == 1))
                    for c in range(2):
                        ins = e.matmul(p2[:, 0:nb], lhsT=g.onesf[:], rhs=y2t[c][0][:, 0:nb], start=(c == 0), stop=(c == 1))
                    return ins
                S.op('pe', fst, R=['onesf', ybt[0][1], ybt[1][1], y2t[0][1], y2t[1][1]], W=[p1k, p2k])
                mt, mk = mean.next()
                qt, qk = msq.next()
                vt_, vk_ = var.next()
                S.op('act', lambda e, mt=mt, p1=p1, nb=nb: e.activation(out=mt[:, 0:nb], in_=p1[:, 0:nb], func=AF.Copy, scale=1.0 / 256),
                     R=[p1k], W=[mk])
                S.op('pool', lambda e, mt=mt, qt=qt, nb=nb: e.tensor_tensor(out=qt[:, 0:nb], in0=mt[:, 0:nb], in1=mt[:, 0:nb], op=ALU.mult),
                     R=[mk], W=[qk])
                S.op('dve', lambda e, vt_=vt_, p2=p2, qt=qt, nb=nb: e.scalar_tensor_tensor(
                    out=vt_[:, 0:nb], in0=p2[:, 0:nb], scalar=1.0 / 256, in1=qt[:, 0:nb], op0=ALU.mult, op1=ALU.subtract),
                    R=[p2k, qk], W=[vk_])
                S.op('act', lambda e, vt_=vt_, nb=nb: e.activation(out=vt_[:, 0:nb], in_=vt_[:, 0:nb], func=AF.Ln, bias=epsc[:], scale=1.0),
                     R=[vk_, 'epsc2'], W=[vk_])
                S.op('act', lambda e, vt_=vt_, nb=nb: e.activation(out=vt_[:, 0:nb], in_=vt_[:, 0:nb], func=AF.Exp, scale=-0.5),
                     R=[vk_], W=[vk_])
                os_, osk = ost.next()
                for c in range(2):
                    a, ak = ybt[c]
                    x1, x1k = t1.next()
                    x2, x2k = t2.next()
                    S.op('dve', lambda e, x1=x1, a=a, mt=mt, nb=nb: e.tensor_tensor(out=x1[:, 0:nb], in0=a[:, 0:nb], in1=mt[:, 0:nb], op=ALU.subtract),
                         R=[ak, mk], W=[x1k])
                    S.op('pool', lambda e, x2=x2, x1=x1, vt_=vt_, nb=nb: e.tensor_tensor(out=x2[:, 0:nb], in0=x1[:, 0:nb], in1=vt_[:, 0:nb], op=ALU.mult),
                         R=[x1k, vk_], W=[x2k])
                    lg = pfc(g, 'conv_ln_g', l * 2 + c)
                    lb = pfc(g, 'conv_ln_b', l * 2 + c)
                    S.op('act', lambda e, os_=os_, x2=x2, c=c, lg=lg, lb=lb, nb=nb: e.activation(
                        out=os_[:, c * 512:c * 512 + nb], in_=x2[:, 0:nb], func=AF.Silu, scale=lg, bias=lb),
                        R=[x2k, 'pft'], W=[osk])
                S.dma('sp', g.MIX[0:256, t0:t0 + nb].rearrange("(c p) n -> p c n", p=128),
                      os_[:].rearrange("p (c n) -> p c n", c=2)[:, :, 0:nb], R=[osk])
                S.rec = None
                iters.append([st1, st2])
            S.pipeline(iters)
        S.barrier()
        S.flush()


def phase_lru(g, l):
    nc, S = g.nc, g.S
    last = (l == DEPTH - 1)
    with ExitStack() as st:
        sbt = lambda name, shape, dt=F32: st.enter_context(nc.sbuf_tensor(name + Ring.suffix, list(shape), dt))
        XPl = [sbt('XPl%d' % c, [128, SEQ + 6]) for c in range(2)]
        XPc = [sbt('XPc%d' % c, [128, CTX + 6]) for c in range(2)]
        HF = [sbt('HF%d' % c, [128, T]) for c in range(2)]
        LW = sbt('LW', [128, 2 * 2 * 2 * 128])
        DG4 = sbt('DG4', [128, 2 * 2 * 4 * 128])
        nsp = sbt('nsp', [128, 8])
        onec = sbt('onec', [128, 1])
        hinit = sbt('hinit', [128, 8])
        S.op('dve', lambda e: e.memset(onec[:], 1.0), W=['onec'])
        S.dma('sp', LW[:], g.lruW[:, l * 1024:(l + 1) * 1024], W=['LW'])
        lo = PF['lru_lam'][0] + l * 4
        tmpa = sbt('lrutmp', [128, 4])
        S.op('act', lambda e: e.activation(out=tmpa[:], in_=g.pft[:, lo:lo + 4], func=AF.Exp, scale=-1.0), R=['pft'], W=['lrutmp'])
        S.op('act', lambda e: e.activation(out=tmpa[:], in_=tmpa[:], func=AF.Ln, bias=onec[:], scale=1.0), R=['lrutmp', 'onec'], W=['lrutmp'])
        S.op('dve', lambda e: e.tensor_scalar(out=nsp[:, 0:4], in0=tmpa[:], scalar1=-8.0, scalar2=None, op0=ALU.mult), R=['lrutmp'], W=['nsp'])
        S.op('dve', lambda e: e.tensor_scalar(out=nsp[:, 4:8], in0=tmpa[:], scalar1=-16.0, scalar2=None, op0=ALU.mult), R=['lrutmp'], W=['nsp'])
        wo = PF['lru_cw'][0] + l * 16
        for d in range(2):
            for c in range(2):
                for j in range(4):
                    idx = (d * 2 + c) * 4 + j
                    S.op('dve', lambda e, idx=idx: e.tensor_scalar(
                        out=DG4[:, idx * 128:(idx + 1) * 128], in0=g.idf[:], scalar1=g.pft[:, wo + idx:wo + idx + 1],
                        scalar2=None, op0=ALU.mult), R=['idf', 'pft'], W=['DG4'])
        for c in range(2):
            for XP, n, s0, key in [(XPc, CTX, 0, 'XPc'), (XPl, SEQ, CTX, 'XPl')]:
                S.op('pool', lambda e, XP=XP, c=c: e.memset(XP[c][:, 0:3], 0.0), W=[key])
                S.op('pool', lambda e, XP=XP, c=c, n=n: e.memset(XP[c][:, 3 + n:6 + n], 0.0), W=[key])
                S.dma('sp', XP[c][:, 3:3 + n], g.ZL[c * 128:(c + 1) * 128, s0:s0 + n], W=[key])
        W5c = []
        ostc = []
        for cc in range(2):
            W5 = {}
            for nm in ['xc', 'r', 'i', 'a', 'om', 'ix', 'u', 'hb', 'gt', 'g2', 'g3', 'sg', 'gl', 'ysum']:
                W5[nm] = Ring(nc, st, 'l%d_' % cc + nm, [128, 512], F32, 2 if nm in ('xc', 'i', 'a', 'om') else 1)
            W5c.append(W5)
            ostc.append(Ring(nc, st, 'lost%d' % cc, [128, 512], BF16, 2))
        for d in range(2):
            merged = None
            for c in range(2):
                W5 = W5c[c]
                ost = ostc[c]
                order = []
                for (s0, n) in SEGS:
                    bl = seg_blocks(s0, n)
                    if d == 1:
                        bl = bl[::-1]
                    order += [(s0, t0, nb) for (t0, nb) in bl]
                dc = d * 2 + c
                prev = None
                iters = []
                for bi_, (s0, t0, nb) in enumerate(order):
                    st0, st1 = [], []
                    S.rec = st0
                    XP = XPc if s0 == 0 else XPl
                    xkey = 'XPc' if s0 == 0 else 'XPl'
                    o0 = t0 - s0
                    pbase = 3 * c
                    ps, pk = g.PS[pbase], 'ps%d' % pbase

                    def fc4(e, ps=ps, XP=XP, o0=o0, nb=nb, d=d, c=c, dc=dc):
                        for j in range(4):
                            off = o0 + j if d == 0 else o0 + 6 - j
                            ins = e.matmul(ps[:, 0:nb], lhsT=DG4[:, (dc * 4 + j) * 128:(dc * 4 + j + 1) * 128],
                                           rhs=XP[c][:, off:off + nb], start=(j == 0), stop=(j == 3))
                        return ins
                    S.op('pe', fc4, R=['DG4', xkey], W=[pk])
                    xc, xck = W5['xc'].next()
                    cb = pfc(g, 'lru_cb', l * 4 + dc)
                    S.op('act', lambda e, xc=xc, ps=ps, cb=cb, nb=nb: e.activation(
                        out=xc[:, 0:nb], in_=ps[:, 0:nb], func=AF.Identity, bias=cb, scale=1.0), R=[pk, 'pft'], W=[xck])
                    gts = []
                    for gi, (nm, bn) in enumerate([('r', 'lru_ba'), ('i', 'lru_bx')]):
                        pg_, pgk = g.PS[pbase + 1 + gi], 'ps%d' % (pbase + 1 + gi)
                        lwo = ((d * 2 + gi) * 2 + c) * 128
                        S.op('pe', lambda e, pg_=pg_, lwo=lwo, xc=xc, nb=nb: e.matmul(
                            pg_[:, 0:nb], lhsT=LW[:, lwo:lwo + 128], rhs=xc[:, 0:nb], start=True, stop=True),
                            R=['LW', xck], W=[pgk])
                        gt_, gtk = W5[nm].next()
                        bb = pfc(g, bn, l * 4 + dc)
                        S.op('act', lambda e, gt_=gt_, pg_=pg_, bb=bb, nb=nb: e.activation(
                            out=gt_[:, 0:nb], in_=pg_[:, 0:nb], func=AF.Sigmoid, bias=bb, scale=1.0), R=[pgk, 'pft'], W=[gtk])
                        gts.append((gt_, gtk))
                    (r_, rk), (i_, ik) = gts
                    a_, ak = W5['a'].next()
                    om, omk = W5['om'].next()
                    S.op('act', lambda e, a_=a_, r_=r_, dc=dc, nb=nb: e.activation(
                        out=a_[:, 0:nb], in_=r_[:, 0:nb], func=AF.Exp, scale=nsp[:, dc:dc + 1]), R=[rk, 'nsp'], W=[ak])
                    S.op('act', lambda e, om=om, r_=r_, dc=dc, nb=nb: e.activation(
                        out=om[:, 0:nb], in_=r_[:, 0:nb], func=AF.Exp, scale=nsp[:, 4 + dc:5 + dc]), R=[rk, 'nsp'], W=[omk])
                    S.op('act', lambda e, om=om, nb=nb: e.activation(
                        out=om[:, 0:nb], in_=om[:, 0:nb], func=AF.Sqrt, scale=-1.0, bias=onec[:]), R=[omk, 'onec'], W=[omk])
                    S.rec = st1
                    ix, ixk = W5['ix'].next()
                    u_, uk = W5['u'].next()
                    S.op('pool', lambda e, ix=ix, i_=i_, xc=xc, nb=nb: e.tensor_tensor(
                        out=ix[:, 0:nb], in0=i_[:, 0:nb], in1=xc[:, 0:nb], op=ALU.mult), R=[ik, xck], W=[ixk])
                    S.op('pool', lambda e, u_=u_, ix=ix, om=om, nb=nb: e.tensor_tensor(
                        out=u_[:, 0:nb], in0=ix[:, 0:nb], in1=om[:, 0:nb], op=ALU.mult), R=[ixk, omk], W=[uk])
                    if d == 0:
                        dst = HF[c][:, t0:t0 + nb]
                        dkey = 'HF%d' % c
                        init = 0.0 if prev is None else HF[c][:, prev:prev + 1]
                        S.op('dve', lambda e, dst=dst, a_=a_, u_=u_, init=init, nb=nb: e.tensor_tensor_scan(
                            out=dst, data0=a_[:, 0:nb], data1=u_[:, 0:nb], initial=init, op0=ALU.mult, op1=ALU.add),
                            R=[ak, uk, dkey], W=[dkey])
                        prev = t0 + nb - 1
                    else:
                        hb, hbk = W5['hb'].next()
                        hi = hinit[:, dc:dc + 1]
                        init = 0.0 if prev is None else hi
                        S.op('dve', lambda e, hb=hb, a_=a_, u_=u_, init=init, nb=nb: e.tensor_tensor_scan(
                            out=hb[:, nb - 1::-1] if False else hb[:, 0:nb][:, ::-1], data0=a_[:, 0:nb][:, ::-1],
                            data1=u_[:, 0:nb][:, ::-1], initial=init, op0=ALU.mult, op1=ALU.add),
                            R=[ak, uk, 'hinit%d' % dc], W=[hbk])
                        S.op('dve', lambda e, hb=hb, hi=hi: e.tensor_copy(out=hi, in_=hb[:, 0:1]), R=[hbk], W=['hinit%d' % dc])
                        prev = 1
                        if last and s0 == 0:
                            S.rec = None
                            iters.append([st0, st1])
                            continue
                        gt, gk = W5['gt'].next()
                        S.dma('sp', gt[:, 0:nb], g.ZL[256 + c * 128:256 + (c + 1) * 128, t0:t0 + nb], W=[gk])
                        g2, g2k = W5['g2'].next()
                        g3, g3k = W5['g3'].next()
                        sg, sgk = W5['sg'].next()
                        gl, glk = W5['gl'].next()
                        ys, ysk = W5['ysum'].next()
                        S.op('pool', lambda e, g2=g2, gt=gt, nb=nb: e.tensor_tensor(out=g2[:, 0:nb], in0=gt[:, 0:nb], in1=gt[:, 0:nb], op=ALU.mult),
                             R=[gk], W=[g2k])
                        S.op('pool', lambda e, g2=g2, nb=nb: e.tensor_scalar(out=g2[:, 0:nb], in0=g2[:, 0:nb], scalar1=0.044715, scalar2=1.0,
                                                                             op0=ALU.mult, op1=ALU.add), R=[g2k], W=[g2k])
                        S.op('pool', lambda e, g3=g3, g2=g2, gt=gt, nb=nb: e.tensor_tensor(out=g3[:, 0:nb], in0=g2[:, 0:nb], in1=gt[:, 0:nb], op=ALU.mult),
                             R=[g2k, gk], W=[g3k])
                        S.op('act', lambda e, sg=sg, g3=g3, nb=nb: e.activation(out=sg[:, 0:nb], in_=g3[:, 0:nb], func=AF.Sigmoid, scale=1.5957691216057308),
                             R=[g3k], W=[sgk])
                        S.op('pool', lambda e, gl=gl, sg=sg, gt=gt, nb=nb: e.tensor_tensor(out=gl[:, 0:nb], in0=sg[:, 0:nb], in1=gt[:, 0:nb], op=ALU.mult),
                             R=[sgk, gk], W=[glk])
                        S.op('dve', lambda e, ys=ys, hb=hb, c=c, t0=t0, nb=nb: e.tensor_tensor(
                            out=ys[:, 0:nb], in0=hb[:, 0:nb], in1=HF[c][:, t0:t0 + nb], op=ALU.add), R=[hbk, 'HF%d' % c], W=[ysk])
                        os_, osk = ost.next()
                        S.op('pool', lambda e, os_=os_, ys=ys, gl=gl, nb=nb: e.tensor_tensor(
                            out=os_[:, 0:nb], in0=ys[:, 0:nb], in1=gl[:, 0:nb], op=ALU.mult), R=[ysk, glk], W=[osk])
                        S.dma('sp', g.MIX[768 + c * 128:768 + (c + 1) * 128, t0:t0 + nb], os_[:, 0:nb], R=[osk])
                    S.rec = None
                    iters.append([st0, st1])
                if merged is None:
                    merged = iters
                else:
                    merged = [merged[i_] + iters[i_] for i_ in range(len(iters))]
            S.pipeline(merged)
        S.barrier()
        S.flush()


def phase_mlstm(g, l):
    nc, S = g.nc, g.S
    last = (l == DEPTH - 1)
    QS_SCALE = 128 ** -0.5
    with ExitStack() as st:
        sbt = lambda name, shape, dt=F32: st.enter_context(nc.sbuf_tensor(name + Ring.suffix, list(shape), dt))
        TMP = sbt('gTMP', [64, T])
        GI = sbt('gGI', [64, T])
        GF = sbt('gGF', [64, T])
        ones64 = sbt('ones64', [64, 128])
        onec = sbt('onec2', [64, 1])
        mcar = sbt('mcar', [64, NT + 1])
        SEL = sbt('SEL', [64, 8 * 128])
        C32 = sbt('C32', [128, 8 * 130])
        CB = sbt('CB', [128, 8 * 130], BF16)
        S.op('dve', lambda e: e.memset(ones64[:], 1.0), W=['ones64'])
        S.op('dve', lambda e: e.memset(onec[:], 1.0), W=['onec2'])
        S.op('dve', lambda e: e.memset(mcar[:], 0.0), W=['mcar'])
        S.op('dve', lambda e: e.memset(C32[:], 0.0), W=['C32%d' % u for u in range(8)])
        S.op('dve', lambda e: e.memset(CB[:], 0.0), W=['CB%d' % u for u in range(8)])
        for d in range(2):
            for h in range(4):
                p = 32 * d + h
                u = d * 4 + h
                S.op('dve', lambda e, p=p, u=u: e.tensor_scalar(
                    out=SEL[:, u * 128:(u + 1) * 128], in0=ones64[:], scalar1=g.idf[0:64, p:p + 1], scalar2=None, op0=ALU.mult),
                    R=['ones64', 'idf'], W=['SEL'])
        SELb = sbt('SELb', [64, 8 * 128], BF16)
        S.op('dve', lambda e: e.tensor_copy(out=SELb[:], in_=SEL[:]), R=['SEL'], W=['SELb'])
        gbo = PF['gb'][0] + l * 2
        for (dst, dkey, r0, bcol) in [(GI, 'gGI', 0, gbo), (GF, 'gGF', 8, gbo + 1)]:
            S.op('pool', lambda e: e.memset(TMP[:], 0.0), W=['gTMP'])
            S.dma('sp', TMP[0:4, :], g.ZG[r0:r0 + 4, :], W=['gTMP'])
            S.dma('sp', TMP[32:36, :], g.ZG[r0 + 4:r0 + 8, :], W=['gTMP'])
            bias = g.pft[0:64, bcol:bcol + 1]
            S.op('dve', lambda e, dst=dst, bias=bias: e.tensor_scalar(
                out=dst[0:32, :], in0=TMP[0:32, :], scalar1=bias[0:32, :], scalar2=None, op0=ALU.add),
                R=['gTMP', 'pft'], W=[dkey])
            S.op('dve', lambda e, dst=dst, bias=bias: e.tensor_scalar(
                out=dst[32:64, 0:CTX], in0=TMP[32:64, 0:CTX][:, ::-1], scalar1=bias[32:64, :], scalar2=None, op0=ALU.add),
                R=['gTMP', 'pft'], W=[dkey])
            S.op('dve', lambda e, dst=dst, bias=bias: e.tensor_scalar(
                out=dst[32:64, CTX:T], in0=TMP[32:64, CTX:T][:, ::-1], scalar1=bias[32:64, :], scalar2=None, op0=ALU.add),
                R=['gTMP', 'pft'], W=[dkey])
        S.op('act', lambda e: e.activation(out=GF[:], in_=GF[:], func=AF.Exp, scale=-1.0), R=['gGF'], W=['gGF'])
        S.op('act', lambda e: e.activation(out=GF[:], in_=GF[:], func=AF.Ln, bias=onec[:], scale=1.0), R=['gGF', 'onec2'], W=['gGF'])

        BN = Ring(nc, st, 'mBN', [64, 128], F32, 2)
        AA = Ring(nc, st, 'mAA', [64, 128], F32, 2)
        MX = Ring(nc, st, 'mMX', [64, 128], F32, 2)
        NML = Ring(nc, st, 'mNML', [64, 1], F32, 2)
        TE = Ring(nc, st, 'mTE', [64, 128], F32, 2)
        RB = Ring(nc, st, 'mRB', [64, 258], F32, 3)
        CL = Ring(nc, st, 'mCL', [64, 3 * 128], F32, 3)
        RBH = Ring(nc, st, 'mRBH', [64, 2 * 258], BF16, 3)
        COL = Ring(nc, st, 'mCOL', [128, 192], F32, 4)
        QK = Ring(nc, st, 'mQK', [128, 8 * 128], BF16, 5)
        KV = Ring(nc, st, 'mKV', [128, 1024], BF16, 7)
        VXs = [sbt('mVX%d' % i, [128, 4 * 130], BF16) for i in range(6)]
        for i in range(6):
            S.op('dve', lambda e, i=i: e.memset(VXs[i][:], 0.0), W=['mVX%d' % i])
            S.op('dve', lambda e, i=i: e.memset(VXs[i][:].rearrange("p (h n) -> p h n", h=4)[:, :, 128:129], 1.0), W=['mVX%d' % i])
        vxi = [0]
        EE = Ring(nc, st, 'mEE', [128, 128], F32, 4)
        EM = Ring(nc, st, 'mEM', [128, 128], F32, 4)
        PT = Ring(nc, st, 'mPT', [128, 128], BF16, 16)
        QS = Ring(nc, st, 'mQS', [128, 128], BF16, 16)
        KW = Ring(nc, st, 'mKW', [128, 128], BF16, 4)
        DN = Ring(nc, st, 'mDN', [128, 1], F32, 8)
        RC = Ring(nc, st, 'mRC', [128, 1], F32, 8)
        DC = Ring(nc, st, 'mDC', [128, 1], F32, 16)
        HO = Ring(nc, st, 'mHO', [128, 512], F32, 4)
        WS = Ring(nc, st, 'mWS', [128, 12288], BF16, 2)

        uc = [0]
        iters = []
        for i in range(NT):
            st0, st1, st2 = [], [], []
            S.rec = st0
            ld = {}
            for d in range(2):
                j = i if d == 0 else ((1 - i) if i < 2 else (35 - i))
                qk, qkk = QK.next()
                kv, kvk = KV.next()
                S.dma('sp', qk[:].rearrange("p (c n) -> p c n", c=8),
                      g.ZQK[:, 128 * j:128 * j + 128].rearrange("(c p) n -> p c n", p=128), W=[qkk])
                S.dma('sp', kv[:], g.ZKV[128 * j:128 * j + 128, :], W=[kvk])
                ld[d] = (j, qk, qkk, kv, kvk)
            cs = slice(128 * i, 128 * i + 128)
            bn, bnk = BN.next()
            aa, aak = AA.next()
            mx, mxk = MX.next()
            nml, nmlk = NML.next()
            te, tek = TE.next()
            rb, rbk = RB.next()
            cl, clk = CL.next()
            S.op('dve', lambda e, bn=bn, cs=cs: e.tensor_tensor_scan(
                out=bn[:], data0=ones64[:], data1=GF[:, cs], initial=0.0, op0=ALU.mult, op1=ALU.add),
                R=['ones64', 'gGF'], W=[bnk])
            S.op('dve', lambda e, aa=aa, bn=bn, cs=cs: e.tensor_tensor(out=aa[:], in0=GI[:, cs], in1=bn[:], op=ALU.add),
                 R=['gGI', bnk], W=[aak])
            S.op('dve', lambda e, mx=mx, aa=aa, i=i: e.tensor_tensor_scan(
                out=mx[:], data0=aa[:], data1=aa[:], initial=mcar[:, i:i + 1], op0=ALU.max, op1=ALU.max),
                R=[aak, 'mcar'], W=[mxk])
            S.op('dve', lambda e, mx=mx, bn=bn, i=i: e.tensor_tensor(
                out=mcar[:, i + 1:i + 2], in0=mx[:, 127:128], in1=bn[:, 127:128], op=ALU.subtract),
                R=[mxk, bnk, 'mcar'], W=['mcar'])
            S.op('dve', lambda e, nml=nml, mx=mx: e.tensor_scalar(
                out=nml[:], in0=mx[:, 127:128], scalar1=-1.0, scalar2=None, op0=ALU.mult), R=[mxk], W=[nmlk])
            S.op('pool', lambda e, te=te, bn=bn, mx=mx: e.tensor_tensor(out=te[:], in0=bn[:], in1=mx[:], op=ALU.subtract),
                 R=[bnk, mxk], W=[tek])
            for (p0, p1, rv) in [(0, 32, False), (32, 64, True)]:
                def o(ap, rv=rv):
                    return ap[:, ::-1] if rv else ap
                S.op('dve', lambda e, p0=p0, p1=p1, o=o, rb=rb, mx=mx: e.tensor_scalar(
                    out=o(rb[p0:p1, 0:128]), in0=mx[p0:p1, :], scalar1=-1.0, scalar2=None, op0=ALU.mult),
                    R=[mxk], W=[rbk])
                S.op('act', lambda e, p0=p0, p1=p1, o=o, rb=rb, mx=mx, i=i: e.activation(
                    out=o(rb[p0:p1, 128:256]), in_=mx[p0:p1, :], func=AF.Exp, scale=-1.0, bias=mcar[p0:p1, i:i + 1]),
                    R=[mxk, 'mcar'], W=[rbk])
                S.op('pool', lambda e, p0=p0, p1=p1, o=o, cl=cl, aa=aa: e.tensor_copy(out=o(cl[p0:p1, 0:128]), in_=aa[p0:p1, :]),
                     R=[aak], W=[clk])
                S.op('act', lambda e, p0=p0, p1=p1, o=o, cl=cl, aa=aa, nml=nml: e.activation(
                    out=o(cl[p0:p1, 128:256]), in_=aa[p0:p1, :], func=AF.Exp, scale=1.0, bias=nml[p0:p1, :]),
                    R=[aak, nmlk], W=[clk])
                S.op('act', lambda e, p0=p0, p1=p1, o=o, cl=cl, te=te: e.activation(
                    out=o(cl[p0:p1, 256:384]), in_=te[p0:p1, :], func=AF.Exp), R=[tek], W=[clk])
            S.op('act', lambda e, rb=rb, nml=nml, i=i: e.activation(
                out=rb[:, 256:257], in_=mcar[:, i:i + 1], func=AF.Exp, scale=1.0, bias=nml[:]), R=['mcar', nmlk], W=[rbk])
            S.op('act', lambda e, rb=rb, nml=nml, i=i: e.activation(
                out=rb[:, 257:258], in_=mcar[:, i:i + 1], func=AF.Exp, scale=1.0, bias=nml[:]), R=['mcar', nmlk], W=[rbk])
            rbh, rbhk = RBH.next()
            S.op('pool', lambda e, rbh=rbh, rb=rb: e.tensor_copy(out=rbh[:, 0:258], in_=rb[:, 0:258]), R=[rbk], W=[rbhk])
            S.op('pool', lambda e, rbh=rbh, rb=rb: e.tensor_tensor(out=rbh[:, 258:516], in0=rb[:, 0:258], in1=rbh[:, 0:258], op=ALU.subtract),
                 R=[rbk, rbhk], W=[rbhk])
            pT = g.PS[7][:, 0:192]
            pTk = ('cT', 0)

            def ftr(e, pT=pT, cl=cl):
                for q in range(3):
                    ins = e.transpose(out=pT[:, q * 64:(q + 1) * 64], in_=cl[:, q * 128:(q + 1) * 128], identity=g.idf[0:64, 0:64])
                return ins
            S.op('pe', ftr, R=[clk, 'idf'], W=[pTk])
            col, colk = COL.next()
            S.op('act', lambda e, col=col, pT=pT: e.copy(out=col[:], in_=pT[:, 0:192]), R=[pTk], W=[colk])
            for d in range(2):
                j, qk, qkk, kv, kvk = ld[d]
                S.rec = st1
                vx = VXs[vxi[0]]
                vxk = 'mVX%d' % vxi[0]
                vxi[0] = (vxi[0] + 1) % len(VXs)
                S.op('pool', lambda e, vx=vx, kv=kv: e.tensor_copy(
                    out=vx[:].rearrange("p (h n) -> p h n", h=4)[:, :, 0:128],
                    in_=kv[:, 512:1024].rearrange("p (h n) -> p h n", h=4)), R=[kvk], W=[vxk])
                need_out = not (last and j < 2)
                ho, hok = HO.next()
                tails = []
                for h in range(4):
                    u = d * 4 + h
                    pc = 32 * d + h
                    ui = uc[0] % 3
                    uc[0] += 1
                    ps_s, sk = g.PS[ui][:, 0:128], ('cP', ui)
                    ps_b, bk = g.PS[ui][:, 128:386], ('cP', ui)
                    ps_n, nk = g.PS[3 + h][:, 0:130], ('cQ', h)
                    ps_c, ck = g.PS[3 + h][:, 130:260], ('cQ', h)
                    S.rec = st1
                    S.op('pe', lambda e, ps_s=ps_s, qk=qk, h=h: e.matmul(
                        ps_s[:, 0:128], lhsT=qk[:, (4 + h) * 128:(5 + h) * 128], rhs=qk[:, h * 128:(h + 1) * 128], start=True, stop=True),
                        R=[qkk], W=[sk])
                    def fb(e, ps_b=ps_b, rbh=rbh, u=u):
                        e.matmul(ps_b[:, 0:258], lhsT=SELb[:, u * 128:(u + 1) * 128], rhs=rbh[:, 0:258], start=True, stop=False)
                        return e.matmul(ps_b[:, 0:258], lhsT=SELb[:, u * 128:(u + 1) * 128], rhs=rbh[:, 258:516], start=False, stop=True)
                    S.op('pe', fb, R=[rbhk, 'SELb'], W=[bk])
                    ee, eek = EE.next()
                    em, emk = EM.next()
                    S.op('act', lambda e, ee=ee, ps_b=ps_b, col=col, pc=pc: e.activation(
                        out=ee[:], in_=ps_b[:, 0:128], func=AF.Exp, scale=1.0, bias=col[:, pc:pc + 1]), R=[bk, colk], W=[eek])
                    dc, dck = DC.next()
                    S.op('act', lambda e, dc=dc, ps_b=ps_b: e.copy(out=dc[:], in_=ps_b[:, 256:257]), R=[bk], W=[dck])
                    if d == 0:
                        S.op('pool', lambda e, em=em, ee=ee: e.affine_select(
                            out=em[:], in_=ee[:], pattern=[[1, 128]], compare_op=ALU.is_ge, fill=0.0, base=0, channel_multiplier=-1),
                            R=[eek], W=[emk])
                    else:
                        S.op('pool', lambda e, em=em, ee=ee: e.affine_select(
                            out=em[:], in_=ee[:], pattern=[[-1, 128]], compare_op=ALU.is_ge, fill=0.0, base=0, channel_multiplier=1),
                            R=[eek], W=[emk])
                    pt, ptk = PT.next()
                    S.op('dve', lambda e, pt=pt, ps_s=ps_s, em=em: e.scalar_tensor_tensor(
                        out=pt[:], in0=ps_s[:, 0:128], scalar=QS_SCALE, in1=em[:], op0=ALU.mult, op1=ALU.mult), R=[sk, emk], W=[ptk])
                    qs, qsk = QS.next()
                    S.op('dve', lambda e, qs=qs, qk=qk, h=h, ps_b=ps_b: e.scalar_tensor_tensor(
                        out=qs[:], in0=qk[:, h * 128:(h + 1) * 128], scalar=QS_SCALE, in1=ps_b[:, 128:256], op0=ALU.mult, op1=ALU.mult),
                        R=[qkk, bk], W=[qsk])
                    S.rec = st2
                    kw, kwk = KW.next()
                    S.op('act', lambda e, kw=kw, kv=kv, h=h, col=col, pc=pc: e.activation(
                        out=kw[:], in_=kv[:, h * 128:(h + 1) * 128], func=AF.Copy, scale=col[:, 64 + pc:65 + pc]), R=[kvk, colk], W=[kwk])

                    def fnum(e, ps_n=ps_n, pt=pt, qs=qs, vx=vx, h=h, u=u):
                        e.matmul(ps_n[:, 0:130], lhsT=pt[:], rhs=vx[:, h * 130:(h + 1) * 130], start=True, stop=False)
                        return e.matmul(ps_n[:, 0:130], lhsT=qs[:], rhs=CB[:, u * 130:(u + 1) * 130], start=False, stop=True)
                    S.op('pe', fnum, R=[ptk, qsk, vxk, 'CB%d' % u], W=[nk])
                    S.op('pe', lambda e, ps_c=ps_c, kw=kw, vx=vx, h=h: e.matmul(
                        ps_c[:, 0:130], lhsT=kw[:], rhs=vx[:, h * 130:(h + 1) * 130], start=True, stop=True), R=[kwk, vxk], W=[ck])
                    if need_out:
                        dn, dnk = DN.next()
                        rc, rck = RC.next()
                        S.op('act', lambda e, dn=dn, ps_n=ps_n: e.activation(out=dn[:], in_=ps_n[:, 128:129], func=AF.Abs),
                             R=[nk], W=[dnk])
                    S.op('dve', lambda e, u=u, dc=dc, ps_c=ps_c: e.scalar_tensor_tensor(
                        out=C32[:, u * 130:(u + 1) * 130], in0=C32[:, u * 130:(u + 1) * 130], scalar=dc[:, 0:1], in1=ps_c[:, 0:130],
                        op0=ALU.mult, op1=ALU.add), R=['C32%d' % u, dck, ck], W=['C32%d' % u])
                    if need_out:
                        S.op('dve', lambda e, dn=dn, col=col, pc=pc: e.tensor_tensor(
                            out=dn[:], in0=dn[:], in1=col[:, 128 + pc:129 + pc], op=ALU.max), R=[dnk, colk], W=[dnk])
                        S.op('dve', lambda e, dn=dn, rc=rc: e.reciprocal(out=rc[:], in_=dn[:]), R=[dnk], W=[rck])
                        tails.append((h, ps_n, nk, rc, rck))
                    S.op('pool', lambda e, u=u: e.tensor_copy(out=CB[:, u * 130:(u + 1) * 130], in_=C32[:, u * 130:(u + 1) * 130]),
                         R=['C32%d' % u], W=['CB%d' % u])
                S.rec = st2
                for (h, ps_n, nk, rc, rck) in tails:
                    S.op('act', lambda e, ho=ho, h=h, ps_n=ps_n, rc=rc: e.activation(
                        out=ho[:, h * 128:(h + 1) * 128], in_=ps_n[:, 0:128], func=AF.Copy, scale=rc[:, 0:1]), R=[nk, rck], W=[hok])
                if need_out:
                    S.dma('sp', g.HM[d, 128 * j:128 * j + 128, :], ho[:], R=[hok])
            S.rec = None
            st3 = []
            if i < NE:
                S.rec = st3
                e_ = i
                W, wk = WS.next()
                S.dma('pool', W[:, 0:4096].rearrange("p (k n) -> p k n", k=8),
                      g.w_gate[l, e_].rearrange("(k p) n -> p k n", p=128), W=[(wk, 'g')])
                S.dma('pool', W[:, 4096:8192].rearrange("p (k n) -> p k n", k=8),
                      g.w_up[l, e_].rearrange("(k p) n -> p k n", p=128), W=[(wk, 'u')])
                S.dma('pool', W[:, 8192:12288].rearrange("p (k n) -> p k n", k=4),
                      g.w_down[l, e_].rearrange("(k p) n -> p k n", p=128), W=[(wk, 'd')])
                S.dma('sp', g.WB[e_ * 128:(e_ + 1) * 128, :], W[:], R=[(wk, 'g'), (wk, 'u'), (wk, 'd')])
                S.rec = None
            iters.append([st0, st1, st2, st3])
        S.pipeline(iters)
        S.barrier()
        S.flush()


def bcast_gate(g, gi, s):
    o = (gi * 2 + s) * 1024
    return g.gateb[:, o:o + 1024]


def phase_out(g, l):
    nc, S = g.nc, g.S
    last = (l == DEPTH - 1)
    with ExitStack() as st:
        sbt = lambda name, shape, dt=F32: st.enter_context(nc.sbuf_tensor(name + Ring.suffix, list(shape), dt))
        wout = sbt('wout', [128, 8 * 1024], BF16)
        wr32 = sbt('wr32', [128, 160])
        g.epsc = sbt('epsc3', [128, 1])
        S.op('dve', lambda e: e.memset(g.epsc[:], EPS), W=['epsc'])
        for k in range(8):
            S.dma('pool', wout[:, k * 1024:(k + 1) * 1024], g.w_out[l, k * 128:(k + 1) * 128, :], W=['wout'])
        S.dma('sp', wr32[:], g.wr[:, l * 160:(l + 1) * 160], W=['wr32'])
        H0 = Ring(nc, st, 'oH0', [128, 512], F32, 3)
        H1 = Ring(nc, st, 'oH1', [128, 512], F32, 3)
        OO = Ring(nc, st, 'oOO', [128, 512], F32, 3)
        HSm = Ring(nc, st, 'oHS', [128, 512], F32, 2)
        XN = Ring(nc, st, 'oXN', [128, 512], F32, 2)
        MMb = Ring(nc, st, 'oMM', [128, 512], BF16, 3)
        ST6 = Ring(nc, st, 'oST6', [128, 24], F32, 2)
        MV = Ring(nc, st, 'oMV', [128, 8], F32, 2)
        SD = Ring(nc, st, 'oSD', [128, 4], F32, 2)
        RS = Ring(nc, st, 'oRS', [128, 4], F32, 2)
        MT = Ring(nc, st, 'oMT', [128, 1024], BF16, 4)
        HT = Ring(nc, st, 'oHT', [128, 1024], F32, 4)
        TM = Ring(nc, st, 'oTM', [128, 1024], F32, 2)
        HN = Ring(nc, st, 'oHN', [128, 1024], F32, 3)
        HN2 = Ring(nc, st, 'oHN2', [128, 1024], F32, 3)
        V32 = Ring(nc, st, 'oV32', [128, 1024], F32, 2)
        V16 = Ring(nc, st, 'oV16', [128, 1024], BF16, 3)
        junk = sbt('ojunk', [128, 1024], BF16)
        SSQ = Ring(nc, st, 'oSSQ', [128, 1], F32, 3)
        RSTD = Ring(nc, st, 'oRSTD', [128, 1], F32, 3)
        RT = Ring(nc, st, 'oRT', [128, 64], F32, 3)
        CWt = Ring(nc, st, 'oCW', [128, 16], F32, 3)
        VTKr = Ring(nc, st, 'oVTK', [128, 1032], BF16, 3)
        bro = PF['brb'][0] + l * 20
        ngo = PF['mnorm_g'][0] + l * 4
        iters = []
        jstart = 2 if last else 0
        for j in range(jstart, NT):
            st0, st1, st2, st3, st4 = [], [], [], [], []
            S.rec = st0
            s = 1 if j < 2 else 0
            rows = slice(128 * j, 128 * j + 128)
            h0, h0k = H0.next()
            h1, h1k = H1.next()
            oo, ook = OO.next()
            S.dma('sp', h0[:], g.HM[0, rows, :], W=[h0k])
            S.dma('sp', h1[:], g.HM[1, rows, :], W=[h1k])
            S.dma('sp', oo[:], g.ZO[rows, :], W=[ook])
            mt, mtk = MT.next()
            S.dma('sp', mt[:, 0:256].rearrange("p (c n) -> p c n", c=2),
                  g.MIX[0:256, rows].rearrange("(c p) n -> p c n", p=128), W=[mtk])
            S.dma('sp', mt[:, 768:1024].rearrange("p (c n) -> p c n", c=2),
                  g.MIX[768:1024, rows].rearrange("(c p) n -> p c n", p=128), W=[mtk])
            ht, htk = HT.next()
            load_h_tile(g, l, j, ht, htk, l == 0)
            S.rec = st1
            hs, hsk = HSm.next()
            S.op('pool', lambda e, hs=hs, h0=h0, h1=h1: e.tensor_tensor(out=hs[:], in0=h0[:], in1=h1[:], op=ALU.add), R=[h0k, h1k], W=[hsk])
            s6, s6k = ST6.next()
            mv, mvk = MV.next()
            for hh in range(4):
                S.op('dve', lambda e, s6=s6, hs=hs, hh=hh: e.bn_stats(out=s6[:, hh * 6:(hh + 1) * 6], in_=hs[:, hh * 128:(hh + 1) * 128]),
                     R=[hsk], W=[s6k])
                S.op('dve', lambda e, s6=s6, mv=mv, hh=hh: e.bn_aggr(out=mv[:, hh * 2:(hh + 1) * 2], in_=s6[:, hh * 6:(hh + 1) * 6]),
                     R=[s6k], W=[mvk])
            sd, sdk = SD.next()
            rs, rsk = RS.next()
            S.op('act', lambda e, sd=sd, mv=mv: e.activation(out=sd[:], in_=mv[:, 1:8:2], func=AF.Ln, bias=g.epsc[:], scale=1.0),
                 R=[mvk, 'epsc'], W=[sdk])
            S.op('act', lambda e, rs=rs, sd=sd: e.activation(out=rs[:], in_=sd[:], func=AF.Exp, scale=-0.5), R=[sdk], W=[rsk])
            xn, xnk = XN.next()
            for hh in range(4):
                S.op('dve', lambda e, xn=xn, hs=hs, mv=mv, rs=rs, hh=hh: e.tensor_scalar(
                    out=xn[:, hh * 128:(hh + 1) * 128], in0=hs[:, hh * 128:(hh + 1) * 128], scalar1=mv[:, 2 * hh:2 * hh + 1],
                    scalar2=rs[:, hh:hh + 1], op0=ALU.subtract, op1=ALU.mult), R=[hsk, mvk, rsk], W=[xnk])
            S.op('act', lambda e, oo=oo: e.activation(out=oo[:], in_=oo[:], func=AF.Sigmoid), R=[ook], W=[ook])
            mm, mmk = MMb.next()
            S.op('pool', lambda e, mm=mm, xn=xn, oo=oo: e.tensor_tensor(out=mm[:], in0=xn[:], in1=oo[:], op=ALU.mult), R=[xnk, ook], W=[mmk])
            ps, pk = g.PS[j % 2], 'ps%d' % (j % 2)
            psb = ps[:].bitcast(BF16)

            def ftr(e, mm=mm, psb=psb):
                for hh in range(4):
                    ins = e.transpose(out=psb[:, hh * 128:(hh + 1) * 128], in_=mm[:, hh * 128:(hh + 1) * 128], identity=g.idb[:])
                return ins
            S.op('pe', ftr, R=[mmk, 'idb'], W=[pk])
            for hh in range(4):
                S.op('act', lambda e, mt=mt, psb=psb, hh=hh: e.activation(
                    out=mt[:, (2 + hh) * 128:(3 + hh) * 128], in_=psb[:, hh * 128:(hh + 1) * 128], func=AF.Copy,
                    scale=g.pft[:, ngo + hh:ngo + hh + 1]), R=[pk, 'pft'], W=[mtk])
            S.rec = st2
            tm, tmk = TM.next()
            hn, hnk = HN.next()
            g1b = bcast_gate(g, 0, s)
            for half in range(2):
                py, pyk = g.PS[2 + half], 'ps%d' % (2 + half)

                def fy(e, py=py, mt=mt, half=half):
                    for k in range(8):
                        ins = e.matmul(py[:, :], lhsT=mt[:, k * 128:(k + 1) * 128],
                                       rhs=wout[:, k * 1024 + half * 512:k * 1024 + (half + 1) * 512], start=(k == 0), stop=(k == 7))
                    return ins
                S.op('pe', fy, R=[mtk, 'wout'], W=[pyk])
                hsl = slice(half * 512, (half + 1) * 512)
                S.op('dve', lambda e, tm=tm, py=py, hsl=hsl, g1b=g1b: e.tensor_tensor(out=tm[:, hsl], in0=py[:, :], in1=g1b[:, hsl], op=ALU.mult),
                     R=[pyk, 'gateb'], W=[tmk])
            S.op('pool', lambda e, hn=hn, tm=tm, ht=ht: e.tensor_tensor(out=hn[:], in0=tm[:], in1=ht[:], op=ALU.add), R=[tmk, htk], W=[hnk])
            for (rs_, p0, npart) in tile_rows(l, j):
                S.dma('sp', g.Hs[rs_, :], hn[p0:p0 + npart, :], R=[hnk])
            sq, sqk = SSQ.next()
            rstd, rstdk = RSTD.next()
            rms_rstd(g, hn, hnk, junk, 'ojunk', sq, sqk, rstd, rstdk)
            h2, h2k = HN2.next()
            S.op('dve', lambda e, h2=h2, hn=hn, rstd=rstd: e.tensor_scalar(out=h2[:], in0=hn[:], scalar1=rstd[:, 0:1], scalar2=None, op0=ALU.mult),
                 R=[hnk, rstdk], W=[h2k])
            S.rec = st3
            v32, v32k = V32.next()
            v16, v16k = V16.next()
            for half in range(2):
                pt_, ptk_ = g.PS[4 + half], 'ps%d' % (4 + half)

                def ft2(e, pt_=pt_, h2=h2, half=half):
                    for kk in range(4):
                        k = half * 4 + kk
                        ins = e.transpose(out=pt_[:, kk * 128:(kk + 1) * 128], in_=h2[:, k * 128:(k + 1) * 128], identity=g.idf[:])
                    return ins
                S.op('pe', ft2, R=[h2k, 'idf'], W=[ptk_])
                for kk in range(4):
                    k = half * 4 + kk
                    col = 2 * k + s
                    sh = 3 * 16 + col
                    if half == 0:
                        S.op('act', lambda e, v32=v32, pt_=pt_, k=k, kk=kk, col=col, sh=sh: e.activation(
                            out=v32[:, k * 128:(k + 1) * 128], in_=pt_[:, kk * 128:(kk + 1) * 128], func=AF.Identity,
                            scale=g.gs2[:, col:col + 1], bias=g.modT[:, sh:sh + 1]), R=[ptk_, 'gs', 'modT'], W=[v32k])
                    else:
                        S.op('dve', lambda e, v32=v32, pt_=pt_, k=k, kk=kk, col=col, sh=sh: e.tensor_scalar(
                            out=v32[:, k * 128:(k + 1) * 128], in0=pt_[:, kk * 128:(kk + 1) * 128],
                            scalar1=g.gs2[:, col:col + 1], scalar2=g.modT[:, sh:sh + 1], op0=ALU.mult, op1=ALU.add),
                            R=[ptk_, 'gs', 'modT'], W=[v32k])
            S.op('pool', lambda e, v16=v16, v32=v32: e.tensor_copy(out=v16[:], in_=v32[:]), R=[v32k], W=[v16k])
            pv, pvk = g.PS[7], 'ps7'
            pvb = pv[:].bitcast(BF16)

            def ftv(e, v16=v16, pvb=pvb):
                for k in range(8):
                    ins = e.transpose(out=pvb[:, k * 128:(k + 1) * 128], in_=v16[:, k * 128:(k + 1) * 128], identity=g.idb[:])
                return ins
            S.op('pe', ftv, R=[v16k, 'idb'], W=[pvk])
            vtk, vtkk = VTKr.next()
            S.op('act', lambda e, vtk=vtk, pvb=pvb: e.copy(out=vtk[:, 0:1024], in_=pvb[:, 0:1024]), R=[pvk], W=[vtkk])
            pl, plk = g.PS[6], 'ps6'

            def frt(e, pl=pl, v32=v32):
                for k in range(8):
                    ins = e.matmul(pl[:, 0:20], lhsT=v32[:, k * 128:(k + 1) * 128], rhs=wr32[:, k * 20:(k + 1) * 20],
                                   start=(k == 0), stop=(k == 7))
                return ins
            S.op('pe', frt, R=[v32k, 'wr32'], W=[plk])
            rt, rtk = RT.next()
            cw, cwk = CWt.next()

            def route(rt, rtk, cw, cwk, pl, plk, vtk, vtkk):
                LG = rt[:, 0:20]; GMAX = rt[:, 20:21]; OHG = rt[:, 21:25]; NGM = rt[:, 25:26]; EG = rt[:, 26:30]
                SUMG = rt[:, 30:31]; PG = rt[:, 31:32]; ES = rt[:, 32:36]; M1 = rt[:, 36:37]; OH1 = rt[:, 37:41]
                ES2 = rt[:, 41:45]; M2 = rt[:, 45:46]; OH2 = rt[:, 46:50]; DD = rt[:, 50:51]; W1 = rt[:, 51:52]; W2 = rt[:, 52:53]
                CW4 = rt[:, 53:57]

                first = [True]

                def R_(fn, eng='dve'):
                    if first[0]:
                        S.op(eng, fn, R=[rtk, plk, 'pft'], W=[rtk])
                        first[0] = False
                    else:
                        S.op(eng, fn, R=[rtk, 'pft'], W=[rtk])
                R_(lambda e: e.tensor_tensor(out=LG, in0=pl[:, 0:20], in1=g.pft[:, bro:bro + 20], op=ALU.add))
                S.rec = st4
                R_(lambda e: e.tensor_reduce(out=GMAX, in_=LG[:, 0:4], axis=AX.X, op=ALU.max))
                R_(lambda e: e.tensor_scalar(out=OHG, in0=LG[:, 0:4], scalar1=GMAX, scalar2=None, op0=ALU.is_equal))
                R_(lambda e: e.tensor_scalar(out=NGM, in0=GMAX, scalar1=-1.0, scalar2=None, op0=ALU.mult))
                R_(lambda e: e.activation(out=EG, in_=LG[:, 0:4], func=AF.Exp, bias=NGM, scale=1.0, accum_out=SUMG), 'act')
                R_(lambda e: e.reciprocal(out=PG, in_=SUMG))
                R_(lambda e: e.tensor_scalar(out=ES, in0=LG[:, 4:8], scalar1=OHG[:, 0:1], scalar2=None, op0=ALU.mult))
                for gg in range(1, 4):
                    R_(lambda e, gg=gg: e.scalar_tensor_tensor(out=ES, in0=LG[:, 4 + 4 * gg:8 + 4 * gg], scalar=OHG[:, gg:gg + 1], in1=ES,
                                                               op0=ALU.mult, op1=ALU.add))
                R_(lambda e: e.tensor_reduce(out=M1, in_=ES, axis=AX.X, op=ALU.max))
                R_(lambda e: e.tensor_scalar(out=OH1, in0=ES, scalar1=M1, scalar2=None, op0=ALU.is_equal))
                R_(lambda e: e.scalar_tensor_tensor(out=ES2, in0=OH1, scalar=-1e30, in1=ES, op0=ALU.mult, op1=ALU.add))
                R_(lambda e: e.tensor_reduce(out=M2, in_=ES2, axis=AX.X, op=ALU.max))
                R_(lambda e: e.tensor_scalar(out=OH2, in0=ES2, scalar1=M2, scalar2=None, op0=ALU.is_equal))
                R_(lambda e: e.tensor_tensor(out=DD, in0=M1, in1=M2, op=ALU.subtract))
                R_(lambda e: e.activation(out=W2, in_=DD, func=AF.Exp, scale=-1.0), 'act')
                R_(lambda e: e.tensor_scalar(out=W1, in0=W2, scalar1=1.0, scalar2=None, op0=ALU.add))
                R_(lambda e: e.reciprocal(out=W1, in_=W1))
                R_(lambda e: e.tensor_tensor(out=W2, in0=W2, in1=W1, op=ALU.mult))
                R_(lambda e: e.tensor_tensor(out=W1, in0=W1, in1=PG, op=ALU.mult))
                R_(lambda e: e.tensor_tensor(out=W2, in0=W2, in1=PG, op=ALU.mult))
                R_(lambda e: e.tensor_scalar(out=CW4, in0=OH1, scalar1=W1, scalar2=None, op0=ALU.mult))
                R_(lambda e: e.scalar_tensor_tensor(out=CW4, in0=OH2, scalar=W2, in1=CW4, op0=ALU.mult, op1=ALU.add))
                S.op('dve', lambda e: e.tensor_copy(out=vtk[:, 1024:1032].bitcast(F32), in_=CW4), R=[rtk], W=[vtkk])
                S.op('dve', lambda e: e.tensor_copy(out=cw[:, 0:4], in_=OHG), R=[rtk], W=[cwk])

            route(rt, rtk, cw, cwk, pl, plk, vtk, vtkk)
            S.dma('sp', g.VTK[rows, :], vtk[:], R=[vtkk])
            S.dma('sp', g.GOH[rows, :], cw[:, 0:4], R=[cwk])
            if g.RTD is not None:
                S.dma('sp', g.RTD[rows, :], rt[:], R=[rtk])
            S.rec = None
            iters.append([st0, st1, st2, st3, st4])
        S.pipeline(iters)
        S.barrier()
        S.flush()


def phase_moe_sparse(g, l):
    nc, S = g.nc, g.S
    last = (l == DEPTH - 1)
    jstart = 2 if last else 0
    with ExitStack() as st:
        sbt = lambda name, shape, dt=F32: st.enter_context(nc.sbuf_tensor(name + Ring.suffix, list(shape), dt))
        posI = sbt('sPOSI', [128, NT], I32)
        widxI = sbt('sWIDX', [128, SL * 4], I32)
        g.epsc = sbt('epsc4', [128, 1])
        S.op('dve', lambda e: e.memset(g.epsc[:], EPS), W=['epsc'])
        with ExitStack() as st1:
            sb1 = lambda name, shape, dt=F32: st1.enter_context(nc.sbuf_tensor(name + Ring.suffix, list(shape), dt))
            N4 = NT * 4
            tri = sb1('sTRI', [128, 128])
            cst = sb1('sCST', [128, 32])
            goh = sb1('sGOH', [128, N4])
            cum = sb1('sCUM', [128, N4])
            totb = sb1('sTOT', [128, N4])
            incl = sb1('sINC', [128, N4])
            onesr = sb1('sONE', [128, NT])
            ng = sb1('sNG', [128, 4])
            cmp9 = sb1('sCMP', [128, 16])
            ns = sb1('sNS', [128, 4])
            gst = sb1('sGST', [128, 4])
            posf = sb1('sPOSF', [128, NT])
            gidf = sb1('sGID', [128, SL])
            gtmp = sb1('sGTMP', [128, SL])
            widxf = sb1('sWIDXF', [128, SL * 4])
            S.dma('sp', tri[:], g.tri[:, :], W=['sTRI'])
            S.dma('sp', cst[:], g.cst[:, :], W=['sCST'])
            S.dma('sp', goh[:].rearrange("p (j q) -> p j q", q=4), g.GOH[:, :].rearrange("(j p) q -> p j q", p=128), W=['sGOH'])
            if last:
                S.op('dve', lambda e: e.memset(goh[:, 0:8], 0.0), R=['sGOH'], W=['sGOH'])
            S.op('dve', lambda e: e.memset(onesr[:], 1.0), W=['sONE'])
            p1, p1k = g.PS[0], 'ps0'
            p2, p2k = g.PS[1], 'ps1'
            S.op('pe', lambda e: e.matmul(p1[:, 0:N4], lhsT=tri[:], rhs=goh[:], start=True, stop=True), R=['sTRI', 'sGOH'], W=[p1k])
            S.op('pe', lambda e: e.matmul(p2[:, 0:N4], lhsT=g.onesf[:], rhs=goh[:], start=True, stop=True), R=['onesf', 'sGOH'], W=[p2k])
            S.op('act', lambda e: e.copy(out=cum[:], in_=p1[:, 0:N4]), R=[p1k], W=['sCUM'])
            S.op('act', lambda e: e.copy(out=totb[:], in_=p2[:, 0:N4]), R=[p2k], W=['sTOT'])
            for q in range(4):
                S.op('dve', lambda e, q=q: e.tensor_tensor_scan(out=incl[:, q:N4:4], data0=onesr[:], data1=totb[:, q:N4:4], initial=0.0,
                                                                op0=ALU.mult, op1=ALU.add), R=['sONE', 'sTOT'], W=['sINC'])
            S.op('dve', lambda e: e.tensor_copy(out=ng[:], in_=incl[:, N4 - 4:N4]), R=['sINC'], W=['sNG'])
            for q in range(4):
                S.op('dve', lambda e, q=q: e.tensor_scalar(out=cmp9[:, 0:9], in0=cst[:, 0:9], scalar1=ng[:, q:q + 1], scalar2=None, op0=ALU.is_lt),
                     R=['sCST', 'sNG'], W=['sCMP'])
                S.op('dve', lambda e, q=q: e.tensor_reduce(out=ns[:, q:q + 1], in_=cmp9[:, 0:9], axis=AX.X, op=ALU.add), R=['sCMP'], W=['sNS'])
            S.op('dve', lambda e: e.tensor_scalar(out=ns[:], in0=ns[:], scalar1=512.0, scalar2=None, op0=ALU.mult), R=['sNS'], W=['sNS'])
            S.op('dve', lambda e: e.memset(gst[:, 0:1], 0.0), W=['sGST'])
            for q in range(1, 4):
                S.op('dve', lambda e, q=q: e.tensor_tensor(out=gst[:, q:q + 1], in0=gst[:, q - 1:q], in1=ns[:, q - 1:q], op=ALU.add),
                     R=['sGST', 'sNS'], W=['sGST'])
            S.op('dve', lambda e: e.tensor_tensor(out=incl[:], in0=incl[:], in1=totb[:], op=ALU.subtract), R=['sINC', 'sTOT'], W=['sINC'])
            S.op('dve', lambda e: e.tensor_tensor(out=cum[:], in0=cum[:], in1=incl[:], op=ALU.add), R=['sCUM', 'sINC'], W=['sCUM'])
            for q in range(4):
                S.op('dve', lambda e, q=q: e.tensor_scalar(out=cum[:, q:N4:4], in0=cum[:, q:N4:4], scalar1=gst[:, q:q + 1], scalar2=-1.0,
                                                           op0=ALU.add, op1=ALU.add), R=['sCUM', 'sGST'], W=['sCUM'])
            S.op('dve', lambda e: e.tensor_tensor(out=cum[:], in0=cum[:], in1=goh[:], op=ALU.mult), R=['sCUM', 'sGOH'], W=['sCUM'])
            S.op('dve', lambda e: e.tensor_reduce(out=posf[:], in_=cum[:].rearrange("p (j q) -> p j q", q=4), axis=AX.X, op=ALU.add),
                 R=['sCUM'], W=['sPOSF'])
            S.op('dve', lambda e: e.tensor_copy(out=posI[:], in_=posf[:]), R=['sPOSF'], W=['sPOSI'])
            S.op('dve', lambda e: e.memset(gidf[:], 0.0), W=['sGID'])
            for q in range(1, 4):
                S.op('dve', lambda e, q=q: e.tensor_scalar(out=gtmp[:], in0=cst[:, 9:9 + SL], scalar1=gst[:, q:q + 1], scalar2=None, op0=ALU.is_ge),
                     R=['sCST', 'sGST'], W=['sGTMP'])
                S.op('dve', lambda e: e.tensor_tensor(out=gidf[:], in0=gidf[:], in1=gtmp[:], op=ALU.add), R=['sGID', 'sGTMP'], W=['sGID'])
            for q in range(4):
                S.op('dve', lambda e, q=q: e.tensor_scalar(out=widxf[:, q:SL * 4:4], in0=gidf[:], scalar1=512.0, scalar2=cst[:, 22 + q:23 + q],
                                                           op0=ALU.mult, op1=ALU.add), R=['sGID', 'sCST'], W=['sWIDXF'])
            S.op('dve', lambda e: e.tensor_copy(out=widxI[:], in_=widxf[:]), R=['sWIDXF'], W=['sWIDX'])
            VL = Ring(nc, st1, 'sVL', [128, 1032], BF16, 8)
            for j in range(jstart, NT):
                vl, vlk = VL.next()
                S.dma('sp', vl[:], g.VTK[128 * j:128 * j + 128, :], W=[vlk])
                S.dma_fn('pool', lambda e, vl=vl, j=j: e.indirect_dma_start(
                    out=g.VS[:, :], out_offset=bass.IndirectOffsetOnAxis(ap=posI[:, j:j + 1], axis=0), in_=vl[:, :], in_offset=None,
                    bounds_check=S.reg(NCAP - 1), oob_is_err=False), R=[vlk, 'sPOSI'], W=[])
            S.barrier()
            S.flush()
        with ExitStack() as st2:
            VSr = Ring(nc, st2, 'sVS', [128, 1032], BF16, 8)
            VTr = Ring(nc, st2, 'sVT', [128, 8 * 512], BF16, 3)
            CWr = Ring(nc, st2, 'sCW', [128, 16], F32, 3)
            WW = Ring(nc, st2, 'sW', [128, 12288], BF16, 3)
            SG = Ring(nc, st2, 'sSG', [128, 512], F32, 3)
            AT = Ring(nc, st2, 'sAT', [128, 4 * 512], BF16, 3)
            YA = Ring(nc, st2, 'sYA', [128, 4 * 1024], F32, 2)
            iters = []
            cnt = [0, 0, 0, 0]
            for sl in range(SL - 1 if last else SL):
                s0, s1, s2 = [], [], []
                S.rec = s0
                vts = []
                for ti in range(4):
                    vs, vsk = VSr.next()
                    r0 = sl * 512 + ti * 128
                    S.dma('sp', vs[:], g.VS[r0:r0 + 128, :], W=[vsk])
                    vts.append((vs, vsk))
                S.rec = s1
                vT, vtk_ = VTr.next()
                cwr, cwrk = CWr.next()
                for ti in range(4):
                    vs, vsk = vts[ti]
                    pb_ = 0
                    pt_, ptk_ = g.PS[pb_], 'ps%d' % pb_
                    ptb = pt_[:].bitcast(BF16)

                    def ftr(e, vs=vs, ptb=ptb):
                        for k in range(8):
                            ins = e.transpose(out=ptb[:, k * 128:(k + 1) * 128], in_=vs[:, k * 128:(k + 1) * 128], identity=g.idb[:])
                        return ins
                    S.op('pe', ftr, R=[vsk, 'idb'], W=[ptk_])
                    eng = 'act' if ti % 2 == 0 else 'dve'
                    if eng == 'act':
                        S.op('act', lambda e, vT=vT, ptb=ptb, ti=ti: e.copy(
                            out=vT[:].rearrange("p (k n) -> p k n", k=8)[:, :, ti * 128:(ti + 1) * 128],
                            in_=ptb[:, 0:1024].rearrange("p (k n) -> p k n", k=8)), R=[ptk_], W=[vtk_])
                    else:
                        S.op('dve', lambda e, vT=vT, ptb=ptb, ti=ti: e.tensor_copy(
                            out=vT[:].rearrange("p (k n) -> p k n", k=8)[:, :, ti * 128:(ti + 1) * 128],
                            in_=ptb[:, 0:1024].rearrange("p (k n) -> p k n", k=8)), R=[ptk_], W=[vtk_])
                    S.op('act', lambda e, cwr=cwr, vs=vs, ti=ti: e.copy(out=cwr[:, ti * 4:(ti + 1) * 4], in_=vs[:, 1024:1032].bitcast(F32)),
                         R=[vsk], W=[cwrk])
                S.rec = s2
                ya, yak = YA.next()
                for q in range(4):
                    W, wk = WW.next()
                    S.dma_fn('pool', lambda e, W=W, sl=sl, q=q: e.indirect_dma_start(
                        out=W[:, :], out_offset=None, in_=g.WB[:, :],
                        in_offset=bass.IndirectOffsetOnAxis(ap=widxI[:, sl * 4 + q:sl * 4 + q + 1], axis=0),
                        bounds_check=S.reg(NE * 128 - 1), oob_is_err=False), R=['sWIDX'], W=[wk])
                    at, atk = AT.next()
                    for c in range(4):
                        pa, pak = g.PS[1 + cnt[1] % 2], 'ps%d' % (1 + cnt[1] % 2)
                        pb, pbk = g.PS[3 + cnt[1] % 2], 'ps%d' % (3 + cnt[1] % 2)
                        cnt[1] += 1

                        def fgu(e, pa=pa, pb=pb, W=W, c=c, vT=vT):
                            for k in range(8):
                                e.matmul(pa[:, :], lhsT=W[:, k * 512 + c * 128:k * 512 + (c + 1) * 128],
                                         rhs=vT[:, k * 512:(k + 1) * 512], start=(k == 0), stop=(k == 7))
                            for k in range(8):
                                ins = e.matmul(pb[:, :], lhsT=W[:, 4096 + k * 512 + c * 128:4096 + k * 512 + (c + 1) * 128],
                                               rhs=vT[:, k * 512:(k + 1) * 512], start=(k == 0), stop=(k == 7))
                            return ins
                        S.op('pe', fgu, R=[wk, vtk_], W=[pak, pbk])
                        sg, sgk = SG.next()
                        S.op('act', lambda e, sg=sg, pa=pa: e.activation(out=sg[:], in_=pa[:, :], func=AF.Silu), R=[pak], W=[sgk])
                        S.op('dve', lambda e, at=at, sg=sg, pb=pb, c=c: e.tensor_tensor(
                            out=at[:, c * 512:(c + 1) * 512], in0=pb[:, :], in1=sg[:], op=ALU.mult), R=[pbk, sgk], W=[atk])
                    for ts in range(4):
                        for half in range(2):
                            pd, pdk = g.PS[5 + cnt[2] % 3], 'ps%d' % (5 + cnt[2] % 3)
                            cnt[2] += 1

                            def fd(e, pd=pd, at=at, W=W, ts=ts, half=half):
                                for kc in range(4):
                                    ins = e.matmul(pd[:, :], lhsT=at[:, kc * 512 + ts * 128:kc * 512 + (ts + 1) * 128],
                                                   rhs=W[:, 8192 + kc * 1024 + half * 512:8192 + kc * 1024 + (half + 1) * 512],
                                                   start=(kc == 0), stop=(kc == 3))
                                return ins
                            S.op('pe', fd, R=[atk, wk], W=[pdk])
                            yv = ya[:, ts * 1024 + half * 512:ts * 1024 + (half + 1) * 512]
                            cwc = cwr[:, ts * 4 + q:ts * 4 + q + 1]
                            if q == 0:
                                S.op('act', lambda e, yv=yv, pd=pd, cwc=cwc: e.activation(out=yv, in_=pd[:, :], func=AF.Copy, scale=cwc),
                                     R=[pdk, cwrk], W=[yak])
                            else:
                                S.op('dve', lambda e, yv=yv, pd=pd, cwc=cwc: e.scalar_tensor_tensor(out=yv, in0=pd[:, :], scalar=cwc, in1=yv, op0=ALU.mult, op1=ALU.add),
                                     R=[pdk, cwrk, yak], W=[yak])
                S.dma('sp', g.YS[sl * 512:(sl + 1) * 512, :].rearrange("(t p) n -> p t n", p=128),
                      ya[:].rearrange("p (t n) -> p t n", t=4), R=[yak])
                S.rec = None
                iters.append([s0, s1, s2])
            S.pipeline(iters)
            S.barrier()
            S.flush()
        with ExitStack() as st3:
            YG = Ring(nc, st3, 'sYG', [128, 1024], F32, 8)
            HT = Ring(nc, st3, 'sHT', [128, 1024], F32, 8)
            TM = Ring(nc, st3, 'sTM', [128, 1024], F32, 6)
            HN = Ring(nc, st3, 'sHN', [128, 1024], F32, 6)
            junk = st3.enter_context(nc.sbuf_tensor('sjunk' + Ring.suffix, [128, 1024], BF16))
            SSQ = Ring(nc, st3, 'sSSQ', [128, 1], F32, 4)
            RSTD = Ring(nc, st3, 'sRSTD', [128, 1], F32, 4)
            iters = []
            for j in range(jstart, NT):
                e0, e1, e2 = [], [], []
                S.rec = e0
                s = 1 if j < 2 else 0
                yg, ygk = YG.next()
                S.dma_fn('pool', lambda e, yg=yg, j=j: e.indirect_dma_start(
                    out=yg[:, :], out_offset=None, in_=g.YS[:, :],
                    in_offset=bass.IndirectOffsetOnAxis(ap=posI[:, j:j + 1], axis=0),
                    bounds_check=S.reg(NCAP - 1), oob_is_err=False), R=['sPOSI'], W=[ygk])
                ht, htk = HT.next()
                load_h_tile(g, l, j, ht, htk, False)
                S.rec = e1
                tm, tmk = TM.next()
                hn, hnk = HN.next()
                g2b = bcast_gate(g, 1, s)
                S.op('dve', lambda e, tm=tm, yg=yg, g2b=g2b: e.tensor_tensor(out=tm[:], in0=yg[:], in1=g2b, op=ALU.mult),
                     R=[ygk, 'gateb'], W=[tmk])
                S.op('dve', lambda e, hn=hn, tm=tm, ht=ht: e.tensor_tensor(out=hn[:], in0=tm[:], in1=ht[:], op=ALU.add), R=[tmk, htk], W=[hnk])
                if not last:
                    for (rs_, p0, npart) in tile_rows(l, j):
                        S.dma('sp', g.Hs[rs_, :], hn[p0:p0 + npart, :], R=[hnk])
                else:
                    S.rec = e2
                    sq, sqk = SSQ.next()
                    rstd, rstdk = RSTD.next()
                    rms_rstd(g, hn, hnk, junk, 'sjunk', sq, sqk, rstd, rstdk)
                    S.op('dve', lambda e, tm=tm, hn=hn, rstd=rstd: e.scalar_tensor_tensor(
                        out=tm[:], in0=hn[:], scalar=rstd[:, 0:1], in1=g.fgb[:], op0=ALU.mult, op1=ALU.mult), R=[hnk, rstdk, 'fgb'], W=[tmk])
                    for (rs_, p0, npart) in tile_rows(l, j):
                        ro = slice(rs_.start - CTX, rs_.stop - CTX, rs_.step)
                        S.dma('sp', g.out[ro, :], tm[p0:p0 + npart, :], R=[tmk])
                S.rec = None
                iters.append([e0, e1, e2])
            S.pipeline(iters)
            S.barrier()
            S.flush()


def col128(v):
    v = np.asarray(v, np.float32)
    return np.ascontiguousarray(v.reshape(-1, 128).T)


def make_pf(inp, b):
    pf = np.zeros((128, NPF), np.float32)

    def put(name, arr):
        o, n = PF[name]
        arr = np.asarray(arr, np.float32).reshape(128, -1)
        assert arr.shape[1] == n, (name, arr.shape, n)
        pf[:, o:o + n] = arr
    c2 = np.stack([col128(inp['c'][b]), col128(inp['c_ctx'])], axis=-1)
    put('c2', c2)
    put('b_mod', np.stack([col128(inp['b_mod'][l]) for l in range(DEPTH)], axis=1))
    put('g1', np.stack([col128(inp['norm1_g'][l]) for l in range(DEPTH)], axis=1))
    put('g2', np.stack([col128(inp['norm2_g'][l]) for l in range(DEPTH)], axis=1))
    put('fg', col128(inp['final_g']))
    cw = np.zeros((128, DEPTH, 2, 31), np.float32)
    for l in range(DEPTH):
        for c in range(2):
            cw[:, l, c, :] = inp['conv_w'][l][:, c * 128:(c + 1) * 128].T
    put('conv_w', cw)
    for nm, key in [('conv_b', 'conv_b'), ('conv_ln_g', 'conv_ln_g'), ('conv_ln_b', 'conv_ln_b')]:
        put(nm, np.stack([col128(inp[key][l]) for l in range(DEPTH)], axis=1))
    put('mnorm_g', np.stack([col128(inp['mlstm_norm_g'][l]) for l in range(DEPTH)], axis=1))
    lcw = np.zeros((128, DEPTH, 2, 2, 4), np.float32)
    for l in range(DEPTH):
        for d in range(2):
            for c in range(2):
                lcw[:, l, d, c, :] = inp['lru_conv_w'][l, d][:, c * 128:(c + 1) * 128].T
    put('lru_cw', lcw)
    for nm, key in [('lru_cb', 'lru_conv_b'), ('lru_ba', 'lru_b_a'), ('lru_bx', 'lru_b_x'), ('lru_lam', 'lru_lambda')]:
        a = np.zeros((128, DEPTH, 2, 2), np.float32)
        for l in range(DEPTH):
            for d in range(2):
                a[:, l, d, :] = col128(inp[key][l, d])
        put(nm, a)
    brb = np.zeros((128, DEPTH, 20), np.float32)
    for l in range(DEPTH):
        brb[:, l, 0:4] = inp['b_rg'][l][None, :]
        brb[:, l, 4:20] = inp['b_re'][l][None, :]
    put('brb', brb)
    gb = np.zeros((128, DEPTH, 2), np.float32)
    for l in range(DEPTH):
        for d in range(2):
            gb[32 * d:32 * d + 4, l, 0] = inp['mlstm_b_i'][l, d]
            gb[32 * d:32 * d + 4, l, 1] = inp['mlstm_b_f'][l, d]
    put('gb', gb)
    return pf


def make_shared(inp):
    sh = {}
    sh['ident'] = np.eye(128, dtype=np.float32)
    sh['tri'] = np.triu(np.ones((128, 128), np.float32))
    cst = np.zeros((128, 32), np.float32)
    cst[:, 0:9] = 512.0 * np.arange(9)[None, :]
    cst[:, 9:9 + SL] = 512.0 * np.arange(SL)[None, :]
    for e in range(4):
        cst[:, 22 + e] = e * 128 + np.arange(128)
    sh['cst'] = cst
    for k in ['w_mod', 'w_in', 'w_out', 'w_gate', 'w_up', 'w_down']:
        sh[k] = np.ascontiguousarray(np.asarray(inp[k], np.float32))
    lw = np.zeros((128, DEPTH, 2, 2, 2, 128), np.float32)
    for l in range(DEPTH):
        for d in range(2):
            for gi, key in enumerate(['lru_w_a', 'lru_w_x']):
                for c in range(2):
                    for a in range(2):
                        lw[64 * a:64 * a + 64, l, d, gi, c, 64 * a:64 * a + 64] = inp[key][l, d, 2 * c + a]
    sh['lruW'] = lw.reshape(128, -1)
    wr = np.zeros((128, DEPTH, 8, 20), np.float32)
    for l in range(DEPTH):
        wcat = np.concatenate([inp['w_rg'][l], inp['w_re'][l]], axis=1)
        wr[:, l] = wcat.reshape(8, 128, 20).transpose(1, 0, 2)
    sh['wr'] = wr.reshape(128, -1)
    return sh


def kernel(**inp):
    inp = {k: np.asarray(v) for k, v in inp.items()}
    nc = build()
    sh = make_shared(inp)
    in_maps = []
    for b in range(8):
        m = dict(sh)
        m['x'] = np.ascontiguousarray(inp['x'][b], np.float32)
        m['ctx'] = np.ascontiguousarray(inp['ctx'][b], np.float32)
        m['pf'] = make_pf(inp, b)
        in_maps.append(m)
    res = run_bass_kernel_spmd(nc, in_maps, core_ids=list(range(8)))
    return np.stack([np.asarray(r['out'], np.float32) for r in res.results], axis=0)
```

```python
import numpy as np
from contextlib import ExitStack
import concourse.bass as bass
import concourse.mybir as mybir
from concourse.bass_utils import run_bass_kernel_spmd

F32 = mybir.dt.float32
BF16 = mybir.dt.bfloat16
AF = mybir.ActivationFunctionType
ALU = mybir.AluOpType
AX = mybir.AxisListType

D = 1024
SEQ = 4096
CTX = 256
T = SEQ + CTX
NT = T // 128
DEPTH = 2
GRID_W = 64
EPS = 1e-6
IN_COLS = 3088
NE = 16
DE = 512
SL = 12
NCAP = SL * 512
I32 = mybir.dt.int32
SAME_ENG_SYNC = True
SAME_ENG_DIST = 0

PF = {}
_off = 0
def _pf(name, n):
    global _off
    PF[name] = (_off, n)
    _off += n
_pf('c2', 16)
_pf('b_mod', 2 * 48)
_pf('g1', 2 * 8)
_pf('g2', 2 * 8)
_pf('fg', 8)
_pf('conv_w', 2 * 2 * 31)
_pf('conv_b', 2 * 2)
_pf('conv_ln_g', 2 * 2)
_pf('conv_ln_b', 2 * 2)
_pf('mnorm_g', 2 * 4)
_pf('lru_cw', 2 * 2 * 2 * 4)
_pf('lru_cb', 2 * 2 * 2)
_pf('lru_ba', 2 * 2 * 2)
_pf('lru_bx', 2 * 2 * 2)
_pf('lru_lam', 2 * 2 * 2)
_pf('brb', 2 * 20)
_pf('gb', 2 * 2)
NPF = _off


class Sched:
    ENG = ['pe', 'act', 'dve', 'pool', 'sp']

    def __init__(self, nc, stack, nds=14):
        self.nc = nc
        self.e = {}
        for n in self.ENG:
            self.e[n] = dict(sem=stack.enter_context(nc.semaphore('sem_' + n)), cnt=0, ops=[], seen={})
        self.dsq = {}
        for q, cnt in [('sp', 12), ('pool', 8), ('act', 4), ('dve', 2), ('pe', 2)]:
            self.dsq[q] = dict(i=0, lst=[dict(sem=stack.enter_context(nc.semaphore('ds_%s%d' % (q, i))), n=0, name='ds_%s%d' % (q, i))
                                        for i in range(cnt)])
        self.ds = [d for q in self.dsq.values() for d in q['lst']]
        self.lastw = {}
        self.rd = {}
        self.nops = 0
        self.rec = None

    def reg(self, val):
        if val not in self.regs:
            self.regs[val] = self.cur_e.to_reg(val)
        return self.regs[val]

    def replay(self, lst):
        assert self.rec is None
        for (kind, en, a, R, W, kw) in lst:
            if kind == 'op':
                self.op(en, a, R, W)
            else:
                self.dma(en, a[0], a[1], R, W, **kw)

    def pipeline(self, iters):
        n = len(iters)
        ns = max(len(x) for x in iters)
        for t in range(n + ns - 1):
            act = []
            for st in range(ns - 1, -1, -1):
                it = t - st
                if 0 <= it < n and st < len(iters[it]) and iters[it][st]:
                    act.append(iters[it][st])
            tot = sum(len(x) for x in act)
            pos = [0] * len(act)
            for k in range(tot):
                best, bi = None, -1
                for i, lst in enumerate(act):
                    if pos[i] < len(lst):
                        f = (pos[i] + 0.5) / len(lst)
                        if best is None or f < best:
                            best, bi = f, i
                self.replay(act[bi][pos[bi]:pos[bi] + 1])
                pos[bi] += 1

    @staticmethod
    def _excl(k):
        return (isinstance(k, tuple) and k and k[0] in ('cP', 'cQ', 'cT')) or (isinstance(k, str) and len(k) == 3 and k.startswith('ps'))

    def _deps(self, R, W):
        if any(self._excl(r) for r in R):
            W = tuple(W) + tuple(r for r in R if self._excl(r))
        deps = []
        for r in R:
            if r in self.lastw:
                deps.append(self.lastw[r])
        for w in W:
            if w in self.lastw:
                deps.append(self.lastw[w])
            deps.extend(self.rd.get(w, ()))
        return deps

    def _commit(self, R, W, tok):
        if any(self._excl(r) for r in R):
            W = tuple(W) + tuple(r for r in R if self._excl(r))
            R = tuple(r for r in R if not self._excl(r))
        for r in R:
            self.rd.setdefault(r, []).append(tok)
        for w in W:
            self.lastw[w] = tok
            self.rd[w] = []

    def _waits(self, en, deps):
        E = self.e[en]
        out = []
        for (sname, sem, val, src) in deps:
            if src == en and en == 'pe':
                continue
            if src == en and (not SAME_ENG_SYNC) and en in ('act', 'dve'):
                continue
            if src == en and en in ('act', 'dve') and SAME_ENG_DIST > 0 and (E['cnt'] + 1 - val) >= SAME_ENG_DIST:
                continue
            if E['seen'].get(sname, 0) >= val:
                continue
            E['seen'][sname] = val
            out.append((sem, val))
        return out

    def op(self, en, fn, R=(), W=()):
        if self.rec is not None:
            self.rec.append(('op', en, fn, tuple(R), tuple(W), None))
            return
        E = self.e[en]
        waits = self._waits(en, self._deps(R, W))
        E['cnt'] += 1
        tok = ('sem_' + en, E['sem'], E['cnt'], en)
        E['ops'].append((waits, fn, (E['sem'], 1)))
        self._commit(R, W, tok)
        self.nops += 1

    def dma_fn(self, q, fn, R=(), W=()):
        self.dma(q, None, None, R, W, _fn=fn)

    def dma(self, q, out, in_, R=(), W=(), **kw):
        if self.rec is not None:
            self.rec.append(('dma', q, (out, in_), tuple(R), tuple(W), kw))
            return
        E = self.e[q]
        dq = self.dsq[q]
        d = dq['lst'][dq['i']]
        dq['i'] = (dq['i'] + 1) % len(dq['lst'])
        deps = self._deps(R, W)
        if d['n'] > 0:
            deps.append((d['name'], d['sem'], 16 * d['n'], 'dma'))
        waits = self._waits(q, deps)
        d['n'] += 1
        tok = (d['name'], d['sem'], 16 * d['n'], 'dma')
        if '_fn' in kw:
            E['ops'].append((waits, kw['_fn'], (d['sem'], 16)))
        else:
            E['ops'].append((waits, (lambda e: e.dma_start(out=out, in_=in_, **kw)), (d['sem'], 16)))
        self._commit(R, W, tok)
        self.nops += 1

    def barrier(self):
        toks = [('sem_' + n, self.e[n]['sem'], self.e[n]['cnt'], n) for n in self.ENG if self.e[n]['cnt'] > 0]
        toks += [(d['name'], d['sem'], 16 * d['n'], 'dma') for d in self.ds if d['n'] > 0]
        for n in self.ENG:
            E = self.e[n]
            waits = []
            for (sname, sem, val, src) in toks:
                if src == n:
                    continue
                if E['seen'].get(sname, 0) >= val:
                    continue
                E['seen'][sname] = val
                waits.append((sem, val))
            E['ops'].append((waits, None, None))
        self.lastw = {}
        self.rd = {}

    def flush(self):
        nc = self.nc
        with nc.Block() as block:
            for n, dec in [('sp', block.sync), ('act', block.scalar), ('dve', block.vector),
                           ('pool', block.gpsimd), ('pe', block.tensor)]:
                ops = self.e[n]['ops']

                def body(e, ops=ops, n=n):
                    self.regs = {}
                    self.cur_e = e
                    for waits, fn, inc in ops:
                        for sem, val in waits:
                            e.wait_ge(sem, val)
                        if fn is not None:
                            ins = fn(e)
                            ins.then_inc(inc[0], inc[1])
                    for r in self.regs.values():
                        e.free_register(r)
                    self.regs = {}
                dec(body)
                self.e[n]['ops'] = []


class Ring:
    suffix = ''

    def __init__(self, nc, stack, name, shape, dtype, n):
        self.t = [stack.enter_context(nc.sbuf_tensor('%s%d%s' % (name, i, Ring.suffix), shape, dtype)) for i in range(n)]
        self.k = [('%s%d' % (name, i)) for i in range(n)]
        self.i = 0

    def next(self):
        i = self.i
        self.i = (i + 1) % len(self.t)
        return self.t[i], self.k[i]


def tile_rows(l, j):
    if j < 2:
        return [(slice(128 * j, 128 * j + 128), 0, 128)]
    jl = j - 2
    if l % 2 == 0:
        return [(slice(CTX + 128 * jl, CTX + 128 * jl + 128), 0, 128)]
    return [(slice(CTX + 2 * jl + wi, CTX + SEQ, GRID_W), 64 * wi, 64) for wi in range(2)]


class K:
    pass


def build(debug=None, stop_after=None):
    nc = bass.Bass("TRN2", target_bir_lowering=False)
    g = K()
    g.nc = nc
    g.debug = debug or ()
    g.stop_after = stop_after

    def din(name, shape, dt=F32):
        return nc.dram_tensor(name, list(shape), dt, kind="ExternalInput").ap()

    def dscr(name, shape, dt=F32):
        kind = "ExternalOutput" if name in g.debug else "Internal"
        return nc.dram_tensor(name, list(shape), dt, kind=kind).ap()

    g.x = din('x', [SEQ, D])
    g.ctx = din('ctx', [CTX, D])
    g.pf = din('pf', [128, NPF])
    g.ident = din('ident', [128, 128])
    g.w_mod = din('w_mod', [DEPTH, D, 6 * D])
    g.w_in = din('w_in', [DEPTH, D, IN_COLS])
    g.lruW = din('lruW', [128, DEPTH * 2 * 2 * 2 * 128])
    g.w_out = din('w_out', [DEPTH, D, D])
    g.wr = din('wr', [128, DEPTH * 8 * 20])
    g.w_gate = din('w_gate', [DEPTH, NE, D, DE])
    g.w_up = din('w_up', [DEPTH, NE, D, DE])
    g.w_down = din('w_down', [DEPTH, NE, DE, D])
    g.tri = din('tri', [128, 128])
    g.cst = din('cst', [128, 32])
    g.out = nc.dram_tensor('out', [SEQ, D], F32, kind="ExternalOutput").ap()

    g.Hs = dscr('Hs', [T, D])
    g.ZC = dscr('ZC', [512, T])
    g.ZQK = dscr('ZQK', [1024, T], BF16)
    g.ZG = dscr('ZG', [16, T])
    g.ZL = dscr('ZL', [512, T])
    g.ZKV = dscr('ZKV', [T, 1024], BF16)
    g.ZO = dscr('ZO', [T, 512])
    g.HM = dscr('HM', [2, T, 512])
    g.MIX = dscr('MIX', [1024, T], BF16)
    g.VT = dscr('VT', [1024, T], BF16)
    g.CW = dscr('CW', [T, 16])
    g.WB = dscr('WB', [NE * 128, 12288], BF16)
    g.VTK = dscr('VTK', [T, 1032], BF16)
    g.GOH = dscr('GOH', [T, 4])
    g.VS = dscr('VS', [NCAP, 1032], BF16)
    g.YS = dscr('YS', [NCAP, 1024])
    g.RTD = dscr('RTD', [T, 64]) if 'RTD' in g.debug else None

    with ExitStack() as gs:
        S = Sched(nc, gs)
        g.S = S
        sb = lambda name, shape, dt=F32: gs.enter_context(nc.sbuf_tensor(name, list(shape), dt))
        g.PS = [gs.enter_context(nc.psum_tensor('ps%d' % i, [128, 512], F32)) for i in range(8)]
        g.psi = 0
        g.pft = sb('pft', [128, NPF])
        g.idf = sb('idf', [128, 128])
        g.idb = sb('idb', [128, 128], BF16)
        g.onesf = sb('onesf', [128, 128])
        g.onesb = sb('onesb', [128, 128], BF16)
        g.sc = sb('sc', [128, 16])
        g.modT = sb('modT', [128, 96])
        g.gs1 = sb('gs1', [128, 16])
        g.gs2 = sb('gs2', [128, 16])
        g.gateb = sb('gateb', [128, 4 * 1024])
        g.fgb = sb('fgb', [128, 1024])

        S.dma('sp', g.pft[:], g.pf[:, :], W=['pft'])
        S.dma('sp', g.idf[:], g.ident[:, :], W=['idf'])
        S.op('dve', lambda e: e.tensor_copy(out=g.idb[:], in_=g.idf[:]), R=['idf'], W=['idb'])
        S.op('dve', lambda e: e.memset(g.onesf[:], 1.0), W=['onesf'])
        S.op('dve', lambda e: e.memset(g.onesb[:], 1.0), W=['onesb'])
        c0 = PF['c2'][0]
        S.op('act', lambda e: e.activation(out=g.sc[:], in_=g.pft[:, c0:c0 + 16], func=AF.Silu),
             R=['pft'], W=['sc'])

        for l in range(DEPTH):
            last = (l == DEPTH - 1)
            Ring.suffix = '_L%d' % l
            with ExitStack() as stA:
                g.win = stA.enter_context(nc.sbuf_tensor('win' + Ring.suffix, [128, 8 * IN_COLS], BF16))
                for k in range(8):
                    S.dma('pool', g.win[:, k * IN_COLS:(k + 1) * IN_COLS], g.w_in[l, k * 128:(k + 1) * 128, :], W=['win'])
                phase_mod(g, l)
                phase_proj(g, l)
            if g.stop_after == ('A', l):
                break
            phase_convlru(g, l)
            if g.stop_after == ('B', l):
                break
            phase_mlstm(g, l)
            if g.stop_after == ('C', l):
                break
            phase_out(g, l)
            if g.stop_after == ('D', l):
                break
            phase_moe_sparse(g, l)
            if g.stop_after == ('E', l):
                break
        S.barrier()
        S.flush()
    return nc


def psum(g):
    i = g.psi
    g.psi = (i + 1) % 8
    return g.PS[i], 'ps%d' % i


def pfc(g, name, idx=0, n=1):
    o = PF[name][0] + idx
    return g.pft[:, o:o + n]


def phase_mod(g, l):
    nc, S = g.nc, g.S
    with ExitStack() as st:
        wm = Ring(nc, st, 'wm', [128, 8 * 512], BF16, 3)
        scb = st.enter_context(nc.sbuf_tensor('scb' + Ring.suffix, [128, 16], BF16))
        S.op('dve', lambda e: e.tensor_copy(out=scb[:], in_=g.sc[:]), R=['sc'], W=['scb'])
        dg = Ring(nc, st, 'dg', [128, 128], F32, 2)
        tmp = st.enter_context(nc.sbuf_tensor('modtmp' + Ring.suffix, [128, 16], F32))
        ps, pk = psum(g)
        for cb in range(12):
            wt, wk = wm.next()
            S.dma('pool', wt[:].rearrange("p (k n) -> p k n", k=8),
                  g.w_mod[l, :, cb * 512:(cb + 1) * 512].rearrange("(k p) n -> p k n", p=128), W=[wk])
            for j in range(4):
                m = cb * 4 + j

                def f(e, wt=wt, j=j, m=m):
                    for k in range(8):
                        ins = e.matmul(ps[:, 2 * m:2 * m + 2], lhsT=wt[:, k * 512 + j * 128:k * 512 + (j + 1) * 128],
                                       rhs=scb[:, 2 * k:2 * k + 2], start=(k == 0), stop=(k == 7))
                    return ins
                S.op('pe', f, R=[wk, 'scb'], W=[pk])
        bo = PF['b_mod'][0] + l * 48
        for s in range(2):
            S.op('dve', lambda e, s=s: e.tensor_tensor(out=g.modT[:, s:96:2], in0=ps[:, s:96:2],
                                                       in1=g.pft[:, bo:bo + 48], op=ALU.add),
                 R=[pk, 'pft'], W=['modT'])
        for (gsT, gname, sec) in [(g.gs1, 'g1', 1), (g.gs2, 'g2', 4)]:
            go = PF[gname][0] + l * 8
            S.op('dve', lambda e, sec=sec: e.tensor_scalar(out=tmp[:], in0=g.modT[:, sec * 16:sec * 16 + 16],
                                                           scalar1=1.0, scalar2=None, op0=ALU.add),
                 R=['modT'], W=['modtmp'])
            for s in range(2):
                S.op('dve', lambda e, s=s, gsT=gsT, go=go: e.tensor_tensor(out=gsT[:, s:16:2], in0=tmp[:, s:16:2],
                                                                           in1=g.pft[:, go:go + 8], op=ALU.mult),
                     R=['modtmp', 'pft'], W=['gs'])
        for gi, sec in enumerate([2, 5]):
            for s in range(2):
                pa, pak = psum(g)
                pb, pbk = psum(g)
                for k in range(8):
                    dt_, dk = dg.next()
                    col = sec * 16 + 2 * k + s
                    S.op('dve', lambda e, dt_=dt_, col=col: e.tensor_scalar(
                        out=dt_[:], in0=g.idf[:], scalar1=g.modT[:, col:col + 1], scalar2=None, op0=ALU.mult),
                        R=['idf', 'modT'], W=[dk])
                    pp, ppk = (pa, pak) if k < 4 else (pb, pbk)
                    S.op('pe', lambda e, pp=pp, dt_=dt_, k=k: e.matmul(
                        pp[:, (k % 4) * 128:(k % 4 + 1) * 128], lhsT=g.onesf[:], rhs=dt_[:], start=True, stop=True),
                        R=[dk, 'onesf'], W=[ppk])
                o = (gi * 2 + s) * 1024
                S.op('act', lambda e, o=o, pa=pa: e.copy(out=g.gateb[:, o:o + 512], in_=pa[:]), R=[pak], W=['gateb'])
                S.op('act', lambda e, o=o, pb=pb: e.copy(out=g.gateb[:, o + 512:o + 1024], in_=pb[:]), R=[pbk], W=['gateb'])
        if l == DEPTH - 1:
            fo = PF['fg'][0]
            pa, pak = psum(g)
            pb, pbk = psum(g)
            for k in range(8):
                dt_, dk = dg.next()
                S.op('dve', lambda e, dt_=dt_, k=k: e.tensor_scalar(
                    out=dt_[:], in0=g.idf[:], scalar1=g.pft[:, fo + k:fo + k + 1], scalar2=None, op0=ALU.mult),
                    R=['idf', 'pft'], W=[dk])
                pp, ppk = (pa, pak) if k < 4 else (pb, pbk)
                S.op('pe', lambda e, pp=pp, dt_=dt_, k=k: e.matmul(
                    pp[:, (k % 4) * 128:(k % 4 + 1) * 128], lhsT=g.onesf[:], rhs=dt_[:], start=True, stop=True),
                    R=[dk, 'onesf'], W=[ppk])
            S.op('act', lambda e, pa=pa: e.copy(out=g.fgb[:, 0:512], in_=pa[:]), R=[pak], W=['fgb'])
            S.op('act', lambda e, pb=pb: e.copy(out=g.fgb[:, 512:1024], in_=pb[:]), R=[pbk], W=['fgb'])
        S.barrier()
        S.flush()


def load_h_tile(g, l, j, dst, dk, src_is_input, q='sp'):
    S = g.S
    for (rs, p0, npart) in tile_rows(l, j):
        if src_is_input:
            if j < 2:
                src = g.ctx[rs, :]
            else:
                src = g.x[slice(rs.start - CTX, rs.stop - CTX, rs.step), :]
        else:
            src = g.Hs[rs, :]
        S.dma(q, dst[p0:p0 + npart, :], src, W=[dk])


def rms_rstd(g, ht, hk, junk, jk, ssq, sk, rstd, rk):
    S = g.S
    S.op('act', lambda e: e.activation(out=junk[:], in_=ht[:], func=AF.Square, accum_out=ssq[:]),
         R=[hk], W=[jk, sk])
    S.op('act', lambda e: e.activation(out=ssq[:], in_=ssq[:], func=AF.Ln, scale=1.0 / D, bias=g.epsc[:]),
         R=[sk], W=[sk])
    S.op('act', lambda e: e.activation(out=rstd[:], in_=ssq[:], func=AF.Exp, scale=-0.5), R=[sk], W=[rk])


def phase_proj(g, l):
    nc, S = g.nc, g.S
    with ExitStack() as st:
        sbt = lambda name, shape, dt=F32: st.enter_context(nc.sbuf_tensor(name + Ring.suffix, list(shape), dt))
        win = g.win
        g.epsc = sbt('epsc', [128, 1])
        S.op('dve', lambda e: e.memset(g.epsc[:], EPS), W=['epsc'])
        hT = Ring(nc, st, 'hT', [128, 1024], F32, 5)
        hn = Ring(nc, st, 'hn', [128, 1024], BF16, 3)
        junk = sbt('junk', [128, 1024], BF16)
        ssq = Ring(nc, st, 'ssq', [128, 1], F32, 4)
        rstd = Ring(nc, st, 'rstd', [128, 1], F32, 4)
        uT = Ring(nc, st, 'uT', [128, 8 * 512], BF16, 3)
        stC = Ring(nc, st, 'stC', [128, 4 * 512], F32, 2)
        stL = Ring(nc, st, 'stL', [128, 4 * 512], F32, 2)
        stQ = Ring(nc, st, 'stQ', [128, 8 * 512], BF16, 2)
        stG = Ring(nc, st, 'stG', [16, 512], F32, 2)
        stKV = Ring(nc, st, 'stKV', [128, 1024], BF16, 3)
        stO = Ring(nc, st, 'stO', [128, 512], F32, 3)
        evi = [0]

        def evac(out, in_, R, W):
            evi[0] += 1
            if evi[0] % 2 == 0:
                S.op('act', lambda e: e.copy(out=out, in_=in_), R=R, W=W)
            else:
                S.op('dve', lambda e: e.tensor_copy(out=out, in_=in_), R=R, W=W)

        blocks = [(0, 2)] + [(2 + 4 * i, 4) for i in range(8)]
        iters = []
        pji = [0]
        tci = [0]
        for (j0, ntile) in blocks:
            st1, st2 = [], []
            S.rec = st1
            NB = ntile * 128
            t0 = j0 * 128
            ut, uk = uT.next()
            s = 1 if j0 == 0 else 0
            for ti in range(ntile):
                j = j0 + ti
                ht, hk = hT.next()
                load_h_tile(g, l, j, ht, hk, l == 0)
                sq, sk = ssq.next()
                rs_, rk = rstd.next()
                rms_rstd(g, ht, hk, junk, 'junk', sq, sk, rs_, rk)
                hb, hbk = hn.next()
                S.op('dve', lambda e, hb=hb, ht=ht, rs_=rs_: e.tensor_scalar(
                    out=hb[:], in0=ht[:], scalar1=rs_[:, 0:1], scalar2=None, op0=ALU.mult),
                    R=[hk, rk], W=[hbk])
                tb = 2 * (tci[0] % 2)
                tci[0] += 1
                psA, pkA = g.PS[tb], 'ps%d' % tb
                psB, pkB = g.PS[tb + 1], 'ps%d' % (tb + 1)
                psbA = psA[:].bitcast(BF16)
                psbB = psB[:].bitcast(BF16)

                def ftr(e, hb=hb, psbA=psbA, psbB=psbB):
                    for k in range(8):
                        dst = psbA if k < 4 else psbB
                        ins = e.transpose(out=dst[:, (k % 4) * 128:(k % 4 + 1) * 128], in_=hb[:, k * 128:(k + 1) * 128],
                                          identity=g.idb[:])
                    return ins
                S.op('pe', ftr, R=[hbk, 'idb'], W=[pkA, pkB])
                for k in range(8):
                    col = 2 * k + s
                    sh = 0 * 16 + col
                    eng = 'act' if k < 4 else 'dve'
                    psb = psbA if k < 4 else psbB
                    pk = pkA if k < 4 else pkB
                    if eng == 'act':
                        S.op('act', lambda e, k=k, ti=ti, ut=ut, psb=psb, col=col, sh=sh: e.activation(
                            out=ut[:, k * 512 + ti * 128:k * 512 + (ti + 1) * 128], in_=psb[:, (k % 4) * 128:(k % 4 + 1) * 128],
                            func=AF.Identity, scale=g.gs1[:, col:col + 1], bias=g.modT[:, sh:sh + 1]),
                            R=[pk, 'gs', 'modT'], W=[uk])
                    else:
                        S.op('dve', lambda e, k=k, ti=ti, ut=ut, psb=psb, col=col, sh=sh: e.tensor_scalar(
                            out=ut[:, k * 512 + ti * 128:k * 512 + (ti + 1) * 128], in0=psb[:, (k % 4) * 128:(k % 4 + 1) * 128],
                            scalar1=g.gs1[:, col:col + 1], scalar2=g.modT[:, sh:sh + 1], op0=ALU.mult, op1=ALU.add),
                            R=[pk, 'gs', 'modT'], W=[uk])
            S.rec = st2
            groups = [
                ('C', stC, g.ZC, [(0 + 128 * i, 128) for i in range(4)], F32),
                ('Q', stQ, g.ZQK, [(512 + 128 * i, 128) for i in range(8)], BF16),
                ('G', stG, g.ZG, [(2560, 16)], F32),
                ('L', stL, g.ZL, [(2576 + 128 * i, 128) for i in range(4)], F32),
            ]
            for (gname, ring, dst, chunks, dt_) in groups:
                stt, stk = ring.next()
                for ci, (c0, M) in enumerate(chunks):
                    ps, pk = g.PS[4 + pji[0] % 4], 'ps%d' % (4 + pji[0] % 4)
                    pji[0] += 1

                    def fmm(e, ps=ps, c0=c0, M=M, ut=ut, NB=NB):
                        for k in range(8):
                            ins = e.matmul(ps[0:M, 0:NB], lhsT=win[:, k * IN_COLS + c0:k * IN_COLS + c0 + M],
                                           rhs=ut[:, k * 512:k * 512 + NB], start=(k == 0), stop=(k == 7))
                        return ins
                    S.op('pe', fmm, R=['win', uk], W=[pk])
                    evac(stt[0:M, ci * 512:ci * 512 + NB], ps[0:M, 0:NB], [pk], [stk])
                nch = len(chunks)
                M = chunks[0][1]
                if nch == 1:
                    S.dma('pool', dst[0:M, t0:t0 + NB], stt[0:M, 0:NB], R=[stk])
                else:
                    S.dma('pool', dst[:, t0:t0 + NB].rearrange("(c p) n -> p c n", p=128),
                          stt[:].rearrange("p (c n) -> p c n", c=nch)[:, :, 0:NB], R=[stk])
            for ti in range(ntile):
                r0 = t0 + ti * 128
                kv, kvk = stKV.next()
                ot, ok = stO.next()
                for ci, c0 in enumerate([1024, 1536, 2048]):
                    ps, pk = g.PS[4 + pji[0] % 4], 'ps%d' % (4 + pji[0] % 4)
                    pji[0] += 1

                    def fmm(e, ps=ps, c0=c0, ut=ut, ti=ti):
                        for k in range(8):
                            ins = e.matmul(ps[:, :], lhsT=ut[:, k * 512 + ti * 128:k * 512 + (ti + 1) * 128],
                                           rhs=win[:, k * IN_COLS + c0:k * IN_COLS + c0 + 512],
                                           start=(k == 0), stop=(k == 7))
                        return ins
                    S.op('pe', fmm, R=['win', uk], W=[pk])
                    if ci < 2:
                        evac(kv[:, ci * 512:(ci + 1) * 512], ps[:, :], [pk], [kvk])
                    else:
                        evac(ot[:, :], ps[:, :], [pk], [ok])
                S.dma('pool', g.ZKV[r0:r0 + 128, :], kv[:], R=[kvk])
                S.dma('pool', g.ZO[r0:r0 + 128, :], ot[:], R=[ok])
            S.rec = None
            iters.append([st1, st2])
        S.pipeline(iters)
        S.barrier()
        S.flush()


def phase_convlru(g, l):
    phase_conv(g, l)
    phase_lru(g, l)


SEGS = [(0, CTX), (CTX, SEQ)]


def seg_blocks(s0, n):
    return [(s0 + o, min(512, n - o)) for o in range(0, n, 512)]


def phase_conv(g, l):
    nc, S = g.nc, g.S
    last = (l == DEPTH - 1)
    with ExitStack() as st:
        sbt = lambda name, shape, dt=F32: st.enter_context(nc.sbuf_tensor(name + Ring.suffix, list(shape), dt))
        DG = sbt('DG', [128, 2 * 31 * 128], BF16)
        UPl = [sbt('UPl%d' % c, [128, SEQ + 30], BF16) for c in range(2)]
        UPc = [sbt('UPc%d' % c, [128, CTX + 30], BF16) for c in range(2)]
        epsc = sbt('epsc2', [128, 1])
        S.op('dve', lambda e: e.memset(epsc[:], EPS), W=['epsc2'])
        vin = Ring(nc, st, 'vin', [128, 512], F32, 3)
        gin = Ring(nc, st, 'gin', [128, 512], F32, 3)
        sgr = Ring(nc, st, 'sgr', [128, 512], F32, 2)
        yb = [Ring(nc, st, 'yb%d' % c, [128, 512], F32, 3) for c in range(2)]
        y2 = [Ring(nc, st, 'y2%d' % c, [128, 512], F32, 3) for c in range(2)]
        mean = Ring(nc, st, 'mean', [128, 512], F32, 2)
        msq = Ring(nc, st, 'msq', [128, 512], F32, 2)
        var = Ring(nc, st, 'var', [128, 512], F32, 2)
        t1 = Ring(nc, st, 't1', [128, 512], F32, 3)
        t2 = Ring(nc, st, 't2', [128, 512], F32, 3)
        ost = Ring(nc, st, 'ost', [128, 2 * 512], BF16, 2)
        cwo = PF['conv_w'][0] + l * 62
        for c in range(2):
            for k in range(31):
                S.op('dve', lambda e, c=c, k=k: e.tensor_scalar(
                    out=DG[:, (c * 31 + k) * 128:(c * 31 + k + 1) * 128], in0=g.idf[:],
                    scalar1=g.pft[:, cwo + c * 31 + k:cwo + c * 31 + k + 1], scalar2=None, op0=ALU.mult),
                    R=['idf', 'pft'], W=['DG'])
        segs = [SEGS[1]] if last else SEGS
        for (s0, n) in segs:
            UP = UPc if s0 == 0 else UPl
            ukey = 'UPc' if s0 == 0 else 'UPl'
            for c in range(2):
                S.op('pool', lambda e, c=c, UP=UP: e.memset(UP[c][:, 0:15], 0.0), W=[ukey])
                S.op('pool', lambda e, c=c, UP=UP, n=n: e.memset(UP[c][:, 15 + n:30 + n], 0.0), W=[ukey])
            for (t0, nb) in seg_blocks(s0, n):
                for c in range(2):
                    vt, vk = vin.next()
                    gt, gk = gin.next()
                    S.dma('sp', vt[:, 0:nb], g.ZC[c * 128:(c + 1) * 128, t0:t0 + nb], W=[vk])
                    S.dma('sp', gt[:, 0:nb], g.ZC[256 + c * 128:256 + (c + 1) * 128, t0:t0 + nb], W=[gk])
                    sg, sgk = sgr.next()
                    S.op('act', lambda e, sg=sg, gt=gt, nb=nb: e.activation(out=sg[:, 0:nb], in_=gt[:, 0:nb], func=AF.Sigmoid),
                         R=[gk], W=[sgk])
                    o0 = 15 + t0 - s0
                    S.op('dve', lambda e, c=c, UP=UP, vt=vt, sg=sg, nb=nb, o0=o0: e.tensor_tensor(
                        out=UP[c][:, o0:o0 + nb], in0=vt[:, 0:nb], in1=sg[:, 0:nb], op=ALU.mult),
                        R=[vk, sgk], W=[ukey])
            iters = []
            for bi_, (t0, nb) in enumerate(seg_blocks(s0, n)):
                st1, st2 = [], []
                S.rec = st1
                o0 = t0 - s0
                ybt = []
                y2t = []
                for c in range(2):
                    ps, pk = g.PS[2 * (bi_ % 2) + c], 'ps%d' % (2 * (bi_ % 2) + c)

                    def fconv(e, ps=ps, c=c, UP=UP, o0=o0, nb=nb):
                        for k in range(31):
                            ins = e.matmul(ps[:, 0:nb], lhsT=DG[:, (c * 31 + k) * 128:(c * 31 + k + 1) * 128],
                                           rhs=UP[c][:, o0 + k:o0 + k + nb], start=(k == 0), stop=(k == 30))
                        return ins
                    S.op('pe', fconv, R=['DG', ukey], W=[pk])
                    a, ak = yb[c].next()
                    b, bk = y2[c].next()
                    cb = pfc(g, 'conv_b', l * 2 + c)
                    S.op('act', lambda e, a=a, ps=ps, cb=cb, nb=nb: e.activation(
                        out=a[:, 0:nb], in_=ps[:, 0:nb], func=AF.Identity, bias=cb, scale=1.0), R=[pk, 'pft'], W=[ak])
                    S.op('act', lambda e, b=b, ps=ps, cb=cb, nb=nb: e.activation(
                        out=b[:, 0:nb], in_=ps[:, 0:nb], func=AF.Square, bias=cb, scale=1.0), R=[pk, 'pft'], W=[bk])
                    ybt.append((a, ak))
                    y2t.append((b, bk))
                S.rec = st2
                p1, p1k = g.PS[4 + 2 * (bi_ % 2)], 'ps%d' % (4 + 2 * (bi_ % 2))
                p2, p2k = g.PS[5 + 2 * (bi_ % 2)], 'ps%d' % (5 + 2 * (bi_ % 2))

                def fst(e, p1=p1, p2=p2, ybt=ybt, y2t=y2t, nb=nb):
                    for c in range(2):
                        e.matmul(p1[:, 0:nb], lhsT=g.onesf[:], rhs=ybt[c][0][:, 0:nb], start=(c == 0), stop=(c == 1))
                    for c in range(2):
                        ins = e.matmul(p2[:, 0:nb], lhsT=g.onesf[:], rhs=y2t[c][0][:, 0:nb], start=(c == 0), stop=(c == 1))
                    return ins
                S.op('pe', fst, R=['onesf', ybt[0][1], ybt[1][1], y2t[0][1], y2t[1][1]], W=[p1k, p2k])
                mt, mk = mean.next()
                qt, qk = msq.next()
                vt_, vk_ = var.next()
                S.op('act', lambda e, mt=mt, p1=p1, nb=nb: e.activation(out=mt[:, 0:nb], in_=p1[:, 0:nb], func=AF.Copy, scale=1.0 / 256),
                     R=[p1k], W=[mk])
                S.op('pool', lambda e, mt=mt, qt=qt, nb=nb: e.tensor_tensor(out=qt[:, 0:nb], in0=mt[:, 0:nb], in1=mt[:, 0:nb], op=ALU.mult),
                     R=[mk], W=[qk])
                S.op('dve', lambda e, vt_=vt_, p2=p2, qt=qt, nb=nb: e.scalar_tensor_tensor(
                    out=vt_[:, 0:nb], in0=p2[:, 0:nb], scalar=1.0 / 256, in1=qt[:, 0:nb], op0=ALU.mult, op1=ALU.subtract),
                    R=[p2k, qk], W=[vk_])
                S.op('act', lambda e, vt_=vt_, nb=nb: e.activation(out=vt_[:, 0:nb], in_=vt_[:, 0:nb], func=AF.Ln, bias=epsc[:], scale=1.0),
                     R=[vk_, 'epsc2'], W=[vk_])
                S.op('act', lambda e, vt_=vt_, nb=nb: e.activation(out=vt_[:, 0:nb], in_=vt_[:, 0:nb], func=AF.Exp, scale=-0.5),
                     R=[vk_], W=[vk_])
                os_, osk = ost.next()
                for c in range(2):
                    a, ak = ybt[c]
                    x1, x1k = t1.next()
                    x2, x2k = t2.next()
                    S.op('dve', lambda e, x1=x1, a=a, mt=mt, nb=nb: e.tensor_tensor(out=x1[:, 0:nb], in0=a[:, 0:nb], in1=mt[:, 0:nb], op=ALU.subtract),
                         R=[ak, mk], W=[x1k])
                    S.op('pool', lambda e, x2=x2, x1=x1, vt_=vt_, nb=nb: e.tensor_tensor(out=x2[:, 0:nb], in0=x1[:, 0:nb], in1=vt_[:, 0:nb], op=ALU.mult),
                         R=[x1k, vk_], W=[x2k])
                    lg = pfc(g, 'conv_ln_g', l * 2 + c)
                    lb = pfc(g, 'conv_ln_b', l * 2 + c)
                    S.op('act', lambda e, os_=os_, x2=x2, c=c, lg=lg, lb=lb, nb=nb: e.activation(
                        out=os_[:, c * 512:c * 512 + nb], in_=x2[:, 0:nb], func=AF.Silu, scale=lg, bias=lb),
                        R=[x2k, 'pft'], W=[osk])
                S.dma('sp', g.MIX[0:256, t0:t0 + nb].rearrange("(c p) n -> p c n", p=128),
                      os_[:].rearrange("p (c n) -> p c n", c=2)[:, :, 0:nb], R=[osk])
                S.rec = None
                iters.append([st1, st2])
            S.pipeline(iters)
        S.barrier()
        S.flush()


def phase_lru(g, l):
    nc, S = g.nc, g.S
    last = (l == DEPTH - 1)
    with ExitStack() as st:
        sbt = lambda name, shape, dt=F32: st.enter_context(nc.sbuf_tensor(name + Ring.suffix, list(shape), dt))
        XPl = [sbt('XPl%d' % c, [128, SEQ + 6]) for c in range(2)]
        XPc = [sbt('XPc%d' % c, [128, CTX + 6]) for c in range(2)]
        HF = [sbt('HF%d' % c, [128, T]) for c in range(2)]
        LW = sbt('LW', [128, 2 * 2 * 2 * 128])
        DG4 = sbt('DG4', [128, 2 * 2 * 4 * 128])
        nsp = sbt('nsp', [128, 8])
        onec = sbt('onec', [128, 1])
        hinit = sbt('hinit', [128, 8])
        S.op('dve', lambda e: e.memset(onec[:], 1.0), W=['onec'])
        S.dma('sp', LW[:], g.lruW[:, l * 1024:(l + 1) * 1024], W=['LW'])
        lo = PF['lru_lam'][0] + l * 4
        tmpa = sbt('lrutmp', [128, 4])
        S.op('act', lambda e: e.activation(out=tmpa[:], in_=g.pft[:, lo:lo + 4], func=AF.Exp, scale=-1.0), R=['pft'], W=['lrutmp'])
        S.op('act', lambda e: e.activation(out=tmpa[:], in_=tmpa[:], func=AF.Ln, bias=onec[:], scale=1.0), R=['lrutmp', 'onec'], W=['lrutmp'])
        S.op('dve', lambda e: e.tensor_scalar(out=nsp[:, 0:4], in0=tmpa[:], scalar1=-8.0, scalar2=None, op0=ALU.mult), R=['lrutmp'], W=['nsp'])
        S.op('dve', lambda e: e.tensor_scalar(out=nsp[:, 4:8], in0=tmpa[:], scalar1=-16.0, scalar2=None, op0=ALU.mult), R=['lrutmp'], W=['nsp'])
        wo = PF['lru_cw'][0] + l * 16
        for d in range(2):
            for c in range(2):
                for j in range(4):
                    idx = (d * 2 + c) * 4 + j
                    S.op('dve', lambda e, idx=idx: e.tensor_scalar(
                        out=DG4[:, idx * 128:(idx + 1) * 128], in0=g.idf[:], scalar1=g.pft[:, wo + idx:wo + idx + 1],
                        scalar2=None, op0=ALU.mult), R=['idf', 'pft'], W=['DG4'])
        for c in range(2):
            for XP, n, s0, key in [(XPc, CTX, 0, 'XPc'), (XPl, SEQ, CTX, 'XPl')]:
                S.op('pool', lambda e, XP=XP, c=c: e.memset(XP[c][:, 0:3], 0.0), W=[key])
                S.op('pool', lambda e, XP=XP, c=c, n=n: e.memset(XP[c][:, 3 + n:6 + n], 0.0), W=[key])
                S.dma('sp', XP[c][:, 3:3 + n], g.ZL[c * 128:(c + 1) * 128, s0:s0 + n], W=[key])
        W5c = []
        ostc = []
        for cc in range(2):
            W5 = {}
            for nm in ['xc', 'r', 'i', 'a', 'om', 'ix', 'u', 'hb', 'gt', 'g2', 'g3', 'sg', 'gl', 'ysum']:
                W5[nm] = Ring(nc, st, 'l%d_' % cc + nm, [128, 512], F32, 2 if nm in ('xc', 'i', 'a', 'om') else 1)
            W5c.append(W5)
            ostc.append(Ring(nc, st, 'lost%d' % cc, [128, 512], BF16, 2))
        for d in range(2):
            merged = None
            for c in range(2):
                W5 = W5c[c]
                ost = ostc[c]
                order = []
                for (s0, n) in SEGS:
                    bl = seg_blocks(s0, n)
                    if d == 1:
                        bl = bl[::-1]
                    order += [(s0, t0, nb) for (t0, nb) in bl]
                dc = d * 2 + c
                prev = None
                iters = []
                for bi_, (s0, t0, nb) in enumerate(order):
                    st0, st1 = [], []
                    S.rec = st0
                    XP = XPc if s0 == 0 else XPl
                    xkey = 'XPc' if s0 == 0 else 'XPl'
                    o0 = t0 - s0
                    pbase = 3 * c
                    ps, pk = g.PS[pbase], 'ps%d' % pbase

                    def fc4(e, ps=ps, XP=XP, o0=o0, nb=nb, d=d, c=c, dc=dc):
                        for j in range(4):
                            off = o0 + j if d == 0 else o0 + 6 - j
                            ins = e.matmul(ps[:, 0:nb], lhsT=DG4[:, (dc * 4 + j) * 128:(dc * 4 + j + 1) * 128],
                                           rhs=XP[c][:, off:off + nb], start=(j == 0), stop=(j == 3))
                        return ins
                    S.op('pe', fc4, R=['DG4', xkey], W=[pk])
                    xc, xck = W5['xc'].next()
                    cb = pfc(g, 'lru_cb', l * 4 + dc)
                    S.op('act', lambda e, xc=xc, ps=ps, cb=cb, nb=nb: e.activation(
                        out=xc[:, 0:nb], in_=ps[:, 0:nb], func=AF.Identity, bias=cb, scale=1.0), R=[pk, 'pft'], W=[xck])
                    gts = []
                    for gi, (nm, bn) in enumerate([('r', 'lru_ba'), ('i', 'lru_bx')]):
                        pg_, pgk = g.PS[pbase + 1 + gi], 'ps%d' % (pbase + 1 + gi)
                        lwo = ((d * 2 + gi) * 2 + c) * 128
                        S.op('pe', lambda e, pg_=pg_, lwo=lwo, xc=xc, nb=nb: e.matmul(
                            pg_[:, 0:nb], lhsT=LW[:, lwo:lwo + 128], rhs=xc[:, 0:nb], start=True, stop=True),
                            R=['LW', xck], W=[pgk])
                        gt_, gtk = W5[nm].next()
                        bb = pfc(g, bn, l * 4 + dc)
                        S.op('act', lambda e, gt_=gt_, pg_=pg_, bb=bb, nb=nb: e.activation(
                            out=gt_[:, 0:nb], in_=pg_[:, 0:nb], func=AF.Sigmoid, bias=bb, scale=1.0), R=[pgk, 'pft'], W=[gtk])
                        gts.append((gt_, gtk))
                    (r_, rk), (i_, ik) = gts
                    a_, ak = W5['a'].next()
                    om, omk = W5['om'].next()
                    S.op('act', lambda e, a_=a_, r_=r_, dc=dc, nb=nb: e.activation(
                        out=a_[:, 0:nb], in_=r_[:, 0:nb], func=AF.Exp, scale=nsp[:, dc:dc + 1]), R=[rk, 'nsp'], W=[ak])
                    S.op('act', lambda e, om=om, r_=r_, dc=dc, nb=nb: e.activation(
                        out=om[:, 0:nb], in_=r_[:, 0:nb], func=AF.Exp, scale=nsp[:, 4 + dc:5 + dc]), R=[rk, 'nsp'], W=[omk])
                    S.op('act', lambda e, om=om, nb=nb: e.activation(
                        out=om[:, 0:nb], in_=om[:, 0:nb], func=AF.Sqrt, scale=-1.0, bias=onec[:]), R=[omk, 'onec'], W=[omk])
                    S.rec = st1
                    ix, ixk = W5['ix'].next()
                    u_, uk = W5['u'].next()
                    S.op('pool', lambda e, ix=ix, i_=i_, xc=xc, nb=nb: e.tensor_tensor(
                        out=ix[:, 0:nb], in0=i_[:, 0:nb], in1=xc[:, 0:nb], op=ALU.mult), R=[ik, xck], W=[ixk])
                    S.op('pool', lambda e, u_=u_, ix=ix, om=om, nb=nb: e.tensor_tensor(
                        out=u_[:, 0:nb], in0=ix[:, 0:nb], in1=om[:, 0:nb], op=ALU.mult), R=[ixk, omk], W=[uk])
                    if d == 0:
                        dst = HF[c][:, t0:t0 + nb]
                        dkey = 'HF%d' % c
                        init = 0.0 if prev is None else HF[c][:, prev:prev + 1]
                        S.op('dve', lambda e, dst=dst, a_=a_, u_=u_, init=init, nb=nb: e.tensor_tensor_scan(
                            out=dst, data0=a_[:, 0:nb], data1=u_[:, 0:nb], initial=init, op0=ALU.mult, op1=ALU.add),
                            R=[ak, uk, dkey], W=[dkey])
                        prev = t0 + nb - 1
                    else:
                        hb, hbk = W5['hb'].next()
                        hi = hinit[:, dc:dc + 1]
                        init = 0.0 if prev is None else hi
                        S.op('dve', lambda e, hb=hb, a_=a_, u_=u_, init=init, nb=nb: e.tensor_tensor_scan(
                            out=hb[:, nb - 1::-1] if False else hb[:, 0:nb][:, ::-1], data0=a_[:, 0:nb][:, ::-1],
                            data1=u_[:, 0:nb][:, ::-1], initial=init, op0=ALU.mult, op1=ALU.add),
                            R=[ak, uk, 'hinit%d' % dc], W=[hbk])
                        S.op('dve', lambda e, hb=hb, hi=hi: e.tensor_copy(out=hi, in_=hb[:, 0:1]), R=[hbk], W=['hinit%d' % dc])
                        prev = 1
                        if last and s0 == 0:
                            S.rec = None
                            iters.append([st0, st1])
                            continue
                        gt, gk = W5['gt'].next()
                        S.dma('sp', gt[:, 0:nb], g.ZL[256 + c * 128:256 + (c + 1) * 128, t0:t0 + nb], W=[gk])
                        g2, g2k = W5['g2'].next()
                        g3, g3k = W5['g3'].next()
                        sg, sgk = W5['sg'].next()
                        gl, glk = W5['gl'].next()
                        ys, ysk = W5['ysum'].next()
                        S.op('pool', lambda e, g2=g2, gt=gt, nb=nb: e.tensor_tensor(out=g2[:, 0:nb], in0=gt[:, 0:nb], in1=gt[:, 0:nb], op=ALU.mult),
                             R=[gk], W=[g2k])
                        S.op('pool', lambda e, g2=g2, nb=nb: e.tensor_scalar(out=g2[:, 0:nb], in0=g2[:, 0:nb], scalar1=0.044715, scalar2=1.0,
                                                                             op0=ALU.mult, op1=ALU.add), R=[g2k], W=[g2k])
                        S.op('pool', lambda e, g3=g3, g2=g2, gt=gt, nb=nb: e.tensor_tensor(out=g3[:, 0:nb], in0=g2[:, 0:nb], in1=gt[:, 0:nb], op=ALU.mult),
                             R=[g2k, gk], W=[g3k])
                        S.op('act', lambda e, sg=sg, g3=g3, nb=nb: e.activation(out=sg[:, 0:nb], in_=g3[:, 0:nb], func=AF.Sigmoid, scale=1.5957691216057308),
                             R=[g3k], W=[sgk])
                        S.op('pool', lambda e, gl=gl, sg=sg, gt=gt, nb=nb: e.tensor_tensor(out=gl[:, 0:nb], in0=sg[:, 0:nb], in1=gt[:, 0:nb], op=ALU.mult),
                             R=[sgk, gk], W=[glk])
                        S.op('dve', lambda e, ys=ys, hb=hb, c=c, t0=t0, nb=nb: e.tensor_tensor(
                            out=ys[:, 0:nb], in0=hb[:, 0:nb], in1=HF[c][:, t0:t0 + nb], op=ALU.add), R=[hbk, 'HF%d' % c], W=[ysk])
                        os_, osk = ost.next()
                        S.op('pool', lambda e, os_=os_, ys=ys, gl=gl, nb=nb: e.tensor_tensor(
                            out=os_[:, 0:nb], in0=ys[:, 0:nb], in1=gl[:, 0:nb], op=ALU.mult), R=[ysk, glk], W=[osk])
                        S.dma('sp', g.MIX[768 + c * 128:768 + (c + 1) * 128, t0:t0 + nb], os_[:, 0:nb], R=[osk])
                    S.rec = None
                    iters.append([st0, st1])
                if merged is None:
                    merged = iters
                else:
                    merged = [merged[i_] + iters[i_] for i_ in range(len(iters))]
            S.pipeline(merged)
        S.barrier()
        S.flush()


def phase_mlstm(g, l):
    nc, S = g.nc, g.S
    last = (l == DEPTH - 1)
    QS_SCALE = 128 ** -0.5
    with ExitStack() as st:
        sbt = lambda name, shape, dt=F32: st.enter_context(nc.sbuf_tensor(name + Ring.suffix, list(shape), dt))
        TMP = sbt('gTMP', [64, T])
        GI = sbt('gGI', [64, T])
        GF = sbt('gGF', [64, T])
        ones64 = sbt('ones64', [64, 128])
        onec = sbt('onec2', [64, 1])
        mcar = sbt('mcar', [64, NT + 1])
        SEL = sbt('SEL', [64, 8 * 128])
        C32 = sbt('C32', [128, 8 * 130])
        CB = sbt('CB', [128, 8 * 130], BF16)
        S.op('dve', lambda e: e.memset(ones64[:], 1.0), W=['ones64'])
        S.op('dve', lambda e: e.memset(onec[:], 1.0), W=['onec2'])
        S.op('dve', lambda e: e.memset(mcar[:], 0.0), W=['mcar'])
        S.op('dve', lambda e: e.memset(C32[:], 0.0), W=['C32%d' % u for u in range(8)])
        S.op('dve', lambda e: e.memset(CB[:], 0.0), W=['CB%d' % u for u in range(8)])
        for d in range(2):
            for h in range(4):
                p = 32 * d + h
                u = d * 4 + h
                S.op('dve', lambda e, p=p, u=u: e.tensor_scalar(
                    out=SEL[:, u * 128:(u + 1) * 128], in0=ones64[:], scalar1=g.idf[0:64, p:p + 1], scalar2=None, op0=ALU.mult),
                    R=['ones64', 'idf'], W=['SEL'])
        SELb = sbt('SELb', [64, 8 * 128], BF16)
        S.op('dve', lambda e: e.tensor_copy(out=SELb[:], in_=SEL[:]), R=['SEL'], W=['SELb'])
        gbo = PF['gb'][0] + l * 2
        for (dst, dkey, r0, bcol) in [(GI, 'gGI', 0, gbo), (GF, 'gGF', 8, gbo + 1)]:
            S.op('pool', lambda e: e.memset(TMP[:], 0.0), W=['gTMP'])
            S.dma('sp', TMP[0:4, :], g.ZG[r0:r0 + 4, :], W=['gTMP'])
            S.dma('sp', TMP[32:36, :], g.ZG[r0 + 4:r0 + 8, :], W=['gTMP'])
            bias = g.pft[0:64, bcol:bcol + 1]
            S.op('dve', lambda e, dst=dst, bias=bias: e.tensor_scalar(
                out=dst[0:32, :], in0=TMP[0:32, :], scalar1=bias[0:32, :], scalar2=None, op0=ALU.add),
                R=['gTMP', 'pft'], W=[dkey])
            S.op('dve', lambda e, dst=dst, bias=bias: e.tensor_scalar(
                out=dst[32:64, 0:CTX], in0=TMP[32:64, 0:CTX][:, ::-1], scalar1=bias[32:64, :], scalar2=None, op0=ALU.add),
                R=['gTMP', 'pft'], W=[dkey])
            S.op('dve', lambda e, dst=dst, bias=bias: e.tensor_scalar(
                out=dst[32:64, CTX:T], in0=TMP[32:64, CTX:T][:, ::-1], scalar1=bias[32:64, :], scalar2=None, op0=ALU.add),
                R=['gTMP', 'pft'], W=[dkey])
        S.op('act', lambda e: e.activation(out=GF[:], in_=GF[:], func=AF.Exp, scale=-1.0), R=['gGF'], W=['gGF'])
        S.op('act', lambda e: e.activation(out=GF[:], in_=GF[:], func=AF.Ln, bias=onec[:], scale=1.0), R=['gGF', 'onec2'], W=['gGF'])

        BN = Ring(nc, st, 'mBN', [64, 128], F32, 2)
        AA = Ring(nc, st, 'mAA', [64, 128], F32, 2)
        MX = Ring(nc, st, 'mMX', [64, 128], F32, 2)
        NML = Ring(nc, st, 'mNML', [64, 1], F32, 2)
        TE = Ring(nc, st, 'mTE', [64, 128], F32, 2)
        RB = Ring(nc, st, 'mRB', [64, 258], F32, 3)
        CL = Ring(nc, st, 'mCL', [64, 3 * 128], F32, 3)
        RBH = Ring(nc, st, 'mRBH', [64, 2 * 258], BF16, 3)
        COL = Ring(nc, st, 'mCOL', [128, 192], F32, 4)
        QK = Ring(nc, st, 'mQK', [128, 8 * 128], BF16, 5)
        KV = Ring(nc, st, 'mKV', [128, 1024], BF16, 7)
        VXs = [sbt('mVX%d' % i, [128, 4 * 130], BF16) for i in range(6)]
        for i in range(6):
            S.op('dve', lambda e, i=i: e.memset(VXs[i][:], 0.0), W=['mVX%d' % i])
            S.op('dve', lambda e, i=i: e.memset(VXs[i][:].rearrange("p (h n) -> p h n", h=4)[:, :, 128:129], 1.0), W=['mVX%d' % i])
        vxi = [0]
        EE = Ring(nc, st, 'mEE', [128, 128], F32, 4)
        EM = Ring(nc, st, 'mEM', [128, 128], F32, 4)
        PT = Ring(nc, st, 'mPT', [128, 128], BF16, 16)
        QS = Ring(nc, st, 'mQS', [128, 128], BF16, 16)
        KW = Ring(nc, st, 'mKW', [128, 128], BF16, 4)
        DN = Ring(nc, st, 'mDN', [128, 1], F32, 8)
        RC = Ring(nc, st, 'mRC', [128, 1], F32, 8)
        DC = Ring(nc, st, 'mDC', [128, 1], F32, 16)
        HO = Ring(nc, st, 'mHO', [128, 512], F32, 4)
        WS = Ring(nc, st, 'mWS', [128, 12288], BF16, 2)

        uc = [0]
        iters = []
        for i in range(NT):
            st0, st1, st2 = [], [], []
            S.rec = st0
            ld = {}
            for d in range(2):
                j = i if d == 0 else ((1 - i) if i < 2 else (35 - i))
                qk, qkk = QK.next()
                kv, kvk = KV.next()
                S.dma('sp', qk[:].rearrange("p (c n) -> p c n", c=8),
                      g.ZQK[:, 128 * j:128 * j + 128].rearrange("(c p) n -> p c n", p=128), W=[qkk])
                S.dma('sp', kv[:], g.ZKV[128 * j:128 * j + 128, :], W=[kvk])
                ld[d] = (j, qk, qkk, kv, kvk)
            cs = slice(128 * i, 128 * i + 128)
            bn, bnk = BN.next()
            aa, aak = AA.next()
            mx, mxk = MX.next()
            nml, nmlk = NML.next()
            te, tek = TE.next()
            rb, rbk = RB.next()
            cl, clk = CL.next()
            S.op('dve', lambda e, bn=bn, cs=cs: e.tensor_tensor_scan(
                out=bn[:], data0=ones64[:], data1=GF[:, cs], initial=0.0, op0=ALU.mult, op1=ALU.add),
                R=['ones64', 'gGF'], W=[bnk])
            S.op('dve', lambda e, aa=aa, bn=bn, cs=cs: e.tensor_tensor(out=aa[:], in0=GI[:, cs], in1=bn[:], op=ALU.add),
                 R=['gGI', bnk], W=[aak])
            S.op('dve', lambda e, mx=mx, aa=aa, i=i: e.tensor_tensor_scan(
                out=mx[:], data0=aa[:], data1=aa[:], initial=mcar[:, i:i + 1], op0=ALU.max, op1=ALU.max),
                R=[aak, 'mcar'], W=[mxk])
            S.op('dve', lambda e, mx=mx, bn=bn, i=i: e.tensor_tensor(
                out=mcar[:, i + 1:i + 2], in0=mx[:, 127:128], in1=bn[:, 127:128], op=ALU.subtract),
                R=[mxk, bnk, 'mcar'], W=['mcar'])
            S.op('dve', lambda e, nml=nml, mx=mx: e.tensor_scalar(
                out=nml[:], in0=mx[:, 127:128], scalar1=-1.0, scalar2=None, op0=ALU.mult), R=[mxk], W=[nmlk])
            S.op('pool', lambda e, te=te, bn=bn, mx=mx: e.tensor_tensor(out=te[:], in0=bn[:], in1=mx[:], op=ALU.subtract),
                 R=[bnk, mxk], W=[tek])
            for (p0, p1, rv) in [(0, 32, False), (32, 64, True)]:
                def o(ap, rv=rv):
                    return ap[:, ::-1] if rv else ap
                S.op('dve', lambda e, p0=p0, p1=p1, o=o, rb=rb, mx=mx: e.tensor_scalar(
                    out=o(rb[p0:p1, 0:128]), in0=mx[p0:p1, :], scalar1=-1.0, scalar2=None, op0=ALU.mult),
                    R=[mxk], W=[rbk])
                S.op('act', lambda e, p0=p0, p1=p1, o=o, rb=rb, mx=mx, i=i: e.activation(
                    out=o(rb[p0:p1, 128:256]), in_=mx[p0:p1, :], func=AF.Exp, scale=-1.0, bias=mcar[p0:p1, i:i + 1]),
                    R=[mxk, 'mcar'], W=[rbk])
                S.op('pool', lambda e, p0=p0, p1=p1, o=o, cl=cl, aa=aa: e.tensor_copy(out=o(cl[p0:p1, 0:128]), in_=aa[p0:p1, :]),
                     R=[aak], W=[clk])
                S.op('act', lambda e, p0=p0, p1=p1, o=o, cl=cl, aa=aa, nml=nml: e.activation(
                    out=o(cl[p0:p1, 128:256]), in_=aa[p0:p1, :], func=AF.Exp, scale=1.0, bias=nml[p0:p1, :]),
                    R=[aak, nmlk], W=[clk])
                S.op('act', lambda e, p0=p0, p1=p1, o=o, cl=cl, te=te: e.activation(
                    out=o(cl[p0:p1, 256:384]), in_=te[p0:p1, :], func=AF.Exp), R=[tek], W=[clk])
            S.op('act', lambda e, rb=rb, nml=nml, i=i: e.activation(
                out=rb[:, 256:257], in_=mcar[:, i:i + 1], func=AF.Exp, scale=1.0, bias=nml[:]), R=['mcar', nmlk], W=[rbk])
            S.op('act', lambda e, rb=rb, nml=nml, i=i: e.activation(
                out=rb[:, 257:258], in_=mcar[:, i:i + 1], func=AF.Exp, scale=1.0, bias=nml[:]), R=['mcar', nmlk], W=[rbk])
            rbh, rbhk = RBH.next()
            S.op('pool', lambda e, rbh=rbh, rb=rb: e.tensor_copy(out=rbh[:, 0:258], in_=rb[:, 0:258]), R=[rbk], W=[rbhk])
            S.op('pool', lambda e, rbh=rbh, rb=rb: e.tensor_tensor(out=rbh[:, 258:516], in0=rb[:, 0:258], in1=rbh[:, 0:258], op=ALU.subtract),
                 R=[rbk, rbhk], W=[rbhk])
            pT = g.PS[7][:, 0:192]
            pTk = ('cT', 0)

            def ftr(e, pT=pT, cl=cl):
                for q in range(3):
                    ins = e.transpose(out=pT[:, q * 64:(q + 1) * 64], in_=cl[:, q * 128:(q + 1) * 128], identity=g.idf[0:64, 0:64])
                return ins
            S.op('pe', ftr, R=[clk, 'idf'], W=[pTk])
            col, colk = COL.next()
            S.op('act', lambda e, col=col, pT=pT: e.copy(out=col[:], in_=pT[:, 0:192]), R=[pTk], W=[colk])
            for d in range(2):
                j, qk, qkk, kv, kvk = ld[d]
                S.rec = st1
                vx = VXs[vxi[0]]
                vxk = 'mVX%d' % vxi[0]
                vxi[0] = (vxi[0] + 1) % len(VXs)
                S.op('pool', lambda e, vx=vx, kv=kv: e.tensor_copy(
                    out=vx[:].rearrange("p (h n) -> p h n", h=4)[:, :, 0:128],
                    in_=kv[:, 512:1024].rearrange("p (h n) -> p h n", h=4)), R=[kvk], W=[vxk])
                need_out = not (last and j < 2)
                ho, hok = HO.next()
                tails = []
                for h in range(4):
                    u = d * 4 + h
                    pc = 32 * d + h
                    ui = uc[0] % 3
                    uc[0] += 1
                    ps_s, sk = g.PS[ui][:, 0:128], ('cP', ui)
                    ps_b, bk = g.PS[ui][:, 128:386], ('cP', ui)
                    ps_n, nk = g.PS[3 + h][:, 0:130], ('cQ', h)
                    ps_c, ck = g.PS[3 + h][:, 130:260], ('cQ', h)
                    S.rec = st1
                    S.op('pe', lambda e, ps_s=ps_s, qk=qk, h=h: e.matmul(
                        ps_s[:, 0:128], lhsT=qk[:, (4 + h) * 128:(5 + h) * 128], rhs=qk[:, h * 128:(h + 1) * 128], start=True, stop=True),
                        R=[qkk], W=[sk])
                    def fb(e, ps_b=ps_b, rbh=rbh, u=u):
                        e.matmul(ps_b[:, 0:258], lhsT=SELb[:, u * 128:(u + 1) * 128], rhs=rbh[:, 0:258], start=True, stop=False)
                        return e.matmul(ps_b[:, 0:258], lhsT=SELb[:, u * 128:(u + 1) * 128], rhs=rbh[:, 258:516], start=False, stop=True)
                    S.op('pe', fb, R=[rbhk, 'SELb'], W=[bk])
                    ee, eek = EE.next()
                    em, emk = EM.next()
                    S.op('act', lambda e, ee=ee, ps_b=ps_b, col=col, pc=pc: e.activation(
                        out=ee[:], in_=ps_b[:, 0:128], func=AF.Exp, scale=1.0, bias=col[:, pc:pc + 1]), R=[bk, colk], W=[eek])
                    dc, dck = DC.next()
                    S.op('act', lambda e, dc=dc, ps_b=ps_b: e.copy(out=dc[:], in_=ps_b[:, 256:257]), R=[bk], W=[dck])
                    if d == 0:
                        S.op('pool', lambda e, em=em, ee=ee: e.affine_select(
                            out=em[:], in_=ee[:], pattern=[[1, 128]], compare_op=ALU.is_ge, fill=0.0, base=0, channel_multiplier=-1),
                            R=[eek], W=[emk])
                    else:
                        S.op('pool', lambda e, em=em, ee=ee: e.affine_select(
                            out=em[:], in_=ee[:], pattern=[[-1, 128]], compare_op=ALU.is_ge, fill=0.0, base=0, channel_multiplier=1),
                            R=[eek], W=[emk])
                    pt, ptk = PT.next()
                    S.op('dve', lambda e, pt=pt, ps_s=ps_s, em=em: e.scalar_tensor_tensor(
                        out=pt[:], in0=ps_s[:, 0:128], scalar=QS_SCALE, in1=em[:], op0=ALU.mult, op1=ALU.mult), R=[sk, emk], W=[ptk])
                    qs, qsk = QS.next()
                    S.op('dve', lambda e, qs=qs, qk=qk, h=h, ps_b=ps_b: e.scalar_tensor_tensor(
                        out=qs[:], in0=qk[:, h * 128:(h + 1) * 128], scalar=QS_SCALE, in1=ps_b[:, 128:256], op0=ALU.mult, op1=ALU.mult),
                        R=[qkk, bk], W=[qsk])
                    S.rec = st2
                    kw, kwk = KW.next()
                    S.op('act', lambda e, kw=kw, kv=kv, h=h, col=col, pc=pc: e.activation(
                        out=kw[:], in_=kv[:, h * 128:(h + 1) * 128], func=AF.Copy, scale=col[:, 64 + pc:65 + pc]), R=[kvk, colk], W=[kwk])

                    def fnum(e, ps_n=ps_n, pt=pt, qs=qs, vx=vx, h=h, u=u):
                        e.matmul(ps_n[:, 0:130], lhsT=pt[:], rhs=vx[:, h * 130:(h + 1) * 130], start=True, stop=False)
                        return e.matmul(ps_n[:, 0:130], lhsT=qs[:], rhs=CB[:, u * 130:(u + 1) * 130], start=False, stop=True)
                    S.op('pe', fnum, R=[ptk, qsk, vxk, 'CB%d' % u], W=[nk])
                    S.op('pe', lambda e, ps_c=ps_c, kw=kw, vx=vx, h=h: e.matmul(
                        ps_c[:, 0:130], lhsT=kw[:], rhs=vx[:, h * 130:(h + 1) * 130], start=True, stop=True), R=[kwk, vxk], W=[ck])
                    if need_out:
                        dn, dnk = DN.next()
                        rc, rck = RC.next()
                        S.op('act', lambda e, dn=dn, ps_n=ps_n: e.activation(out=dn[:], in_=ps_n[:, 128:129], func=AF.Abs),
                             R=[nk], W=[dnk])
                    S.op('dve', lambda e, u=u, dc=dc, ps_c=ps_c: e.scalar_tensor_tensor(
                        out=C32[:, u * 130:(u + 1) * 130], in0=C32[:, u * 130:(u + 1) * 130], scalar=dc[:, 0:1], in1=ps_c[:, 0:130],
                        op0=ALU.mult, op1=ALU.add), R=['C32%d' % u, dck, ck], W=['C32%d' % u])
                    if need_out:
                        S.op('dve', lambda e, dn=dn, col=col, pc=pc: e.tensor_tensor(
                            out=dn[:], in0=dn[:], in1=col[:, 128 + pc:129 + pc], op=ALU.max), R=[dnk, colk], W=[dnk])
                        S.op('dve', lambda e, dn=dn, rc=rc: e.reciprocal(out=rc[:], in_=dn[:]), R=[dnk], W=[rck])
                        tails.append((h, ps_n, nk, rc, rck))
                    S.op('pool', lambda e, u=u: e.tensor_copy(out=CB[:, u * 130:(u + 1) * 130], in_=C32[:, u * 130:(u + 1) * 130]),
                         R=['C32%d' % u], W=['CB%d' % u])
                S.rec = st2
                for (h, ps_n, nk, rc, rck) in tails:
                    S.op('act', lambda e, ho=ho, h=h, ps_n=ps_n, rc=rc: e.activation(
                        out=ho[:, h * 128:(h + 1) * 128], in_=ps_n[:, 0:128], func=AF.Copy, scale=rc[:, 0:1]), R=[nk, rck], W=[hok])
                if need_out:
                    S.dma('sp', g.HM[d, 128 * j:128 * j + 128, :], ho[:], R=[hok])
            S.rec = None
            st3 = []
            if i < NE:
                S.rec = st3
                e_ = i
                W, wk = WS.next()
                S.dma('pool', W[:, 0:4096].rearrange("p (k n) -> p k n", k=8),
                      g.w_gate[l, e_].rearrange("(k p) n -> p k n", p=128), W=[(wk, 'g')])
                S.dma('pool', W[:, 4096:8192].rearrange("p (k n) -> p k n", k=8),
                      g.w_up[l, e_].rearrange("(k p) n -> p k n", p=128), W=[(wk, 'u')])
                S.dma('pool', W[:, 8192:12288].rearrange("p (k n) -> p k n", k=4),
                      g.w_down[l, e_].rearrange("(k p) n -> p k n", p=128), W=[(wk, 'd')])
                S.dma('sp', g.WB[e_ * 128:(e_ + 1) * 128, :], W[:], R=[(wk, 'g'), (wk, 'u'), (wk, 'd')])
                S.rec = None
            iters.append([st0, st1, st2, st3])
        S.pipeline(iters)
        S.barrier()
        S.flush()


def bcast_gate(g, gi, s):
    o = (gi * 2 + s) * 1024
    return g.gateb[:, o:o + 1024]


def phase_out(g, l):
    nc, S = g.nc, g.S
    last = (l == DEPTH - 1)
    with ExitStack() as st:
        sbt = lambda name, shape, dt=F32: st.enter_context(nc.sbuf_tensor(name + Ring.suffix, list(shape), dt))
        wout = sbt('wout', [128, 8 * 1024], BF16)
        wr32 = sbt('wr32', [128, 160])
        g.epsc = sbt('epsc3', [128, 1])
        S.op('dve', lambda e: e.memset(g.epsc[:], EPS), W=['epsc'])
        for k in range(8):
            S.dma('pool', wout[:, k * 1024:(k + 1) * 1024], g.w_out[l, k * 128:(k + 1) * 128, :], W=['wout'])
        S.dma('sp', wr32[:], g.wr[:, l * 160:(l + 1) * 160], W=['wr32'])
        H0 = Ring(nc, st, 'oH0', [128, 512], F32, 3)
        H1 = Ring(nc, st, 'oH1', [128, 512], F32, 3)
        OO = Ring(nc, st, 'oOO', [128, 512], F32, 3)
        HSm = Ring(nc, st, 'oHS', [128, 512], F32, 2)
        XN = Ring(nc, st, 'oXN', [128, 512], F32, 2)
        MMb = Ring(nc, st, 'oMM', [128, 512], BF16, 3)
        ST6 = Ring(nc, st, 'oST6', [128, 24], F32, 2)
        MV = Ring(nc, st, 'oMV', [128, 8], F32, 2)
        SD = Ring(nc, st, 'oSD', [128, 4], F32, 2)
        RS = Ring(nc, st, 'oRS', [128, 4], F32, 2)
        MT = Ring(nc, st, 'oMT', [128, 1024], BF16, 4)
        HT = Ring(nc, st, 'oHT', [128, 1024], F32, 4)
        TM = Ring(nc, st, 'oTM', [128, 1024], F32, 2)
        HN = Ring(nc, st, 'oHN', [128, 1024], F32, 3)
        HN2 = Ring(nc, st, 'oHN2', [128, 1024], F32, 3)
        V32 = Ring(nc, st, 'oV32', [128, 1024], F32, 2)
        V16 = Ring(nc, st, 'oV16', [128, 1024], BF16, 3)
        junk = sbt('ojunk', [128, 1024], BF16)
        SSQ = Ring(nc, st, 'oSSQ', [128, 1], F32, 3)
        RSTD = Ring(nc, st, 'oRSTD', [128, 1], F32, 3)
        RT = Ring(nc, st, 'oRT', [128, 64], F32, 3)
        CWt = Ring(nc, st, 'oCW', [128, 16], F32, 3)
        VTKr = Ring(nc, st, 'oVTK', [128, 1032], BF16, 3)
        bro = PF['brb'][0] + l * 20
        ngo = PF['mnorm_g'][0] + l * 4
        iters = []
        jstart = 2 if last else 0
        for j in range(jstart, NT):
            st0, st1, st2, st3, st4 = [], [], [], [], []
            S.rec = st0
            s = 1 if j < 2 else 0
            rows = slice(128 * j, 128 * j + 128)
            h0, h0k = H0.next()
            h1, h1k = H1.next()
            oo, ook = OO.next()
            S.dma('sp', h0[:], g.HM[0, rows, :], W=[h0k])
            S.dma('sp', h1[:], g.HM[1, rows, :], W=[h1k])
            S.dma('sp', oo[:], g.ZO[rows, :], W=[ook])
            mt, mtk = MT.next()
            S.dma('sp', mt[:, 0:256].rearrange("p (c n) -> p c n", c=2),
                  g.MIX[0:256, rows].rearrange("(c p) n -> p c n", p=128), W=[mtk])
            S.dma('sp', mt[:, 768:1024].rearrange("p (c n) -> p c n", c=2),
                  g.MIX[768:1024, rows].rearrange("(c p) n -> p c n", p=128), W=[mtk])
            ht, htk = HT.next()
            load_h_tile(g, l, j, ht, htk, l == 0)
            S.rec = st1
            hs, hsk = HSm.next()
            S.op('pool', lambda e, hs=hs, h0=h0, h1=h1: e.tensor_tensor(out=hs[:], in0=h0[:], in1=h1[:], op=ALU.add), R=[h0k, h1k], W=[hsk])
            s6, s6k = ST6.next()
            mv, mvk = MV.next()
            for hh in range(4):
                S.op('dve', lambda e, s6=s6, hs=hs, hh=hh: e.bn_stats(out=s6[:, hh * 6:(hh + 1) * 6], in_=hs[:, hh * 128:(hh + 1) * 128]),
                     R=[hsk], W=[s6k])
                S.op('dve', lambda e, s6=s6, mv=mv, hh=hh: e.bn_aggr(out=mv[:, hh * 2:(hh + 1) * 2], in_=s6[:, hh * 6:(hh + 1) * 6]),
                     R=[s6k], W=[mvk])
            sd, sdk = SD.next()
            rs, rsk = RS.next()
            S.op('act', lambda e, sd=sd, mv=mv: e.activation(out=sd[:], in_=mv[:, 1:8:2], func=AF.Ln, bias=g.epsc[:], scale=1.0),
                 R=[mvk, 'epsc'], W=[sdk])
            S.op('act', lambda e, rs=rs, sd=sd: e.activation(out=rs[:], in_=sd[:], func=AF.Exp, scale=-0.5), R=[sdk], W=[rsk])
            xn, xnk = XN.next()
            for hh in range(4):
                S.op('dve', lambda e, xn=xn, hs=hs, mv=mv, rs=rs, hh=hh: e.tensor_scalar(
                    out=xn[:, hh * 128:(hh + 1) * 128], in0=hs[:, hh * 128:(hh + 1) * 128], scalar1=mv[:, 2 * hh:2 * hh + 1],
                    scalar2=rs[:, hh:hh + 1], op0=ALU.subtract, op1=ALU.mult), R=[hsk, mvk, rsk], W=[xnk])
            S.op('act', lambda e, oo=oo: e.activation(out=oo[:], in_=oo[:], func=AF.Sigmoid), R=[ook], W=[ook])
            mm, mmk = MMb.next()
            S.op('pool', lambda e, mm=mm, xn=xn, oo=oo: e.tensor_tensor(out=mm[:], in0=xn[:], in1=oo[:], op=ALU.mult), R=[xnk, ook], W=[mmk])
            ps, pk = g.PS[j % 2], 'ps%d' % (j % 2)
            psb = ps[:].bitcast(BF16)

            def ftr(e, mm=mm, psb=psb):
                for hh in range(4):
                    ins = e.transpose(out=psb[:, hh * 128:(hh + 1) * 128], in_=mm[:, hh * 128:(hh + 1) * 128], identity=g.idb[:])
                return ins
            S.op('pe', ftr, R=[mmk, 'idb'], W=[pk])
            for hh in range(4):
                S.op('act', lambda e, mt=mt, psb=psb, hh=hh: e.activation(
                    out=mt[:, (2 + hh) * 128:(3 + hh) * 128], in_=psb[:, hh * 128:(hh + 1) * 128], func=AF.Copy,
                    scale=g.pft[:, ngo + hh:ngo + hh + 1]), R=[pk, 'pft'], W=[mtk])
            S.rec = st2
            tm, tmk = TM.next()
            hn, hnk = HN.next()
            g1b = bcast_gate(g, 0, s)
            for half in range(2):
                py, pyk = g.PS[2 + half], 'ps%d' % (2 + half)

                def fy(e, py=py, mt=mt, half=half):
                    for k in range(8):
                        ins = e.matmul(py[:, :], lhsT=mt[:, k * 128:(k + 1) * 128],
                                       rhs=wout[:, k * 1024 + half * 512:k * 1024 + (half + 1) * 512], start=(k == 0), stop=(k == 7))
                    return ins
                S.op('pe', fy, R=[mtk, 'wout'], W=[pyk])
                hsl = slice(half * 512, (half + 1) * 512)
                S.op('dve', lambda e, tm=tm, py=py, hsl=hsl, g1b=g1b: e.tensor_tensor(out=tm[:, hsl], in0=py[:, :], in1=g1b[:, hsl], op=ALU.mult),
                     R=[pyk, 'gateb'], W=[tmk])
            S.op('pool', lambda e, hn=hn, tm=tm, ht=ht: e.tensor_tensor(out=hn[:], in0=tm[:], in1=ht[:], op=ALU.add), R=[tmk, htk], W=[hnk])
            for (rs_, p0, npart) in tile_rows(l, j):
                S.dma('sp', g.Hs[rs_, :], hn[p0:p0 + npart, :], R=[hnk])
            sq, sqk = SSQ.next()
            rstd, rstdk = RSTD.next()
            rms_rstd(g, hn, hnk, junk, 'ojunk', sq, sqk, rstd, rstdk)
            h2, h2k = HN2.next()
            S.op('dve', lambda e, h2=h2, hn=hn, rstd=rstd: e.tensor_scalar(out=h2[:], in0=hn[:], scalar1=rstd[:, 0:1], scalar2=None, op0=ALU.mult),
                 R=[hnk, rstdk], W=[h2k])
            S.rec = st3
            v32, v32k = V32.next()
            v16, v16k = V16.next()
            for half in range(2):
                pt_, ptk_ = g.PS[4 + half], 'ps%d' % (4 + half)

                def ft2(e, pt_=pt_, h2=h2, half=half):
                    for kk in range(4):
                        k = half * 4 + kk
                        ins = e.transpose(out=pt_[:, kk * 128:(kk + 1) * 128], in_=h2[:, k * 128:(k + 1) * 128], identity=g.idf[:])
                    return ins
                S.op('pe', ft2, R=[h2k, 'idf'], W=[ptk_])
                for kk in range(4):
                    k = half * 4 + kk
                    col = 2 * k + s
                    sh = 3 * 16 + col
                    if half == 0:
                        S.op('act', lambda e, v32=v32, pt_=pt_, k=k, kk=kk, col=col, sh=sh: e.activation(
                            out=v32[:, k * 128:(k + 1) * 128], in_=pt_[:, kk * 128:(kk + 1) * 128], func=AF.Identity,
                            scale=g.gs2[:, col:col + 1], bias=g.modT[:, sh:sh + 1]), R=[ptk_, 'gs', 'modT'], W=[v32k])
                    else:
                        S.op('dve', lambda e, v32=v32, pt_=pt_, k=k, kk=kk, col=col, sh=sh: e.tensor_scalar(
                            out=v32[:, k * 128:(k + 1) * 128], in0=pt_[:, kk * 128:(kk + 1) * 128],
                            scalar1=g.gs2[:, col:col + 1], scalar2=g.modT[:, sh:sh + 1], op0=ALU.mult, op1=ALU.add),
                            R=[ptk_, 'gs', 'modT'], W=[v32k])
            S.op('pool', lambda e, v16=v16, v32=v32: e.tensor_copy(out=v16[:], in_=v32[:]), R=[v32k], W=[v16k])
            pv, pvk = g.PS[7], 'ps7'
            pvb = pv[:].bitcast(BF16)

            def ftv(e, v16=v16, pvb=pvb):
                for k in range(8):
                    ins = e.transpose(out=pvb[:, k * 128:(k + 1) * 128], in_=v16[:, k * 128:(k + 1) * 128], identity=g.idb[:])
                return ins
            S.op('pe', ftv, R=[v16k, 'idb'], W=[pvk])
            vtk, vtkk = VTKr.next()
            S.op('act', lambda e, vtk=vtk, pvb=pvb: e.copy(out=vtk[:, 0:1024], in_=pvb[:, 0:1024]), R=[pvk], W=[vtkk])
            pl, plk = g.PS[6], 'ps6'

            def frt(e, pl=pl, v32=v32):
                for k in range(8):
                    ins = e.matmul(pl[:, 0:20], lhsT=v32[:, k * 128:(k + 1) * 128], rhs=wr32[:, k * 20:(k + 1) * 20],
                                   start=(k == 0), stop=(k == 7))
                return ins
            S.op('pe', frt, R=[v32k, 'wr32'], W=[plk])
            rt, rtk = RT.next()
            cw, cwk = CWt.next()

            def route(rt, rtk, cw, cwk, pl, plk, vtk, vtkk):
                LG = rt[:, 0:20]; GMAX = rt[:, 20:21]; OHG = rt[:, 21:25]; NGM = rt[:, 25:26]; EG = rt[:, 26:30]
                SUMG = rt[:, 30:31]; PG = rt[:, 31:32]; ES = rt[:, 32:36]; M1 = rt[:, 36:37]; OH1 = rt[:, 37:41]
                ES2 = rt[:, 41:45]; M2 = rt[:, 45:46]; OH2 = rt[:, 46:50]; DD = rt[:, 50:51]; W1 = rt[:, 51:52]; W2 = rt[:, 52:53]
                CW4 = rt[:, 53:57]

                first = [True]

                def R_(fn, eng='dve'):
                    if first[0]:
                        S.op(eng, fn, R=[rtk, plk, 'pft'], W=[rtk])
                        first[0] = False
                    else:
                        S.op(eng, fn, R=[rtk, 'pft'], W=[rtk])
                R_(lambda e: e.tensor_tensor(out=LG, in0=pl[:, 0:20], in1=g.pft[:, bro:bro + 20], op=ALU.add))
                S.rec = st4
                R_(lambda e: e.tensor_reduce(out=GMAX, in_=LG[:, 0:4], axis=AX.X, op=ALU.max))
                R_(lambda e: e.tensor_scalar(out=OHG, in0=LG[:, 0:4], scalar1=GMAX, scalar2=None, op0=ALU.is_equal))
                R_(lambda e: e.tensor_scalar(out=NGM, in0=GMAX, scalar1=-1.0, scalar2=None, op0=ALU.mult))
                R_(lambda e: e.activation(out=EG, in_=LG[:, 0:4], func=AF.Exp, bias=NGM, scale=1.0, accum_out=SUMG), 'act')
                R_(lambda e: e.reciprocal(out=PG, in_=SUMG))
                R_(lambda e: e.tensor_scalar(out=ES, in0=LG[:, 4:8], scalar1=OHG[:, 0:1], scalar2=None, op0=ALU.mult))
                for gg in range(1, 4):
                    R_(lambda e, gg=gg: e.scalar_tensor_tensor(out=ES, in0=LG[:, 4 + 4 * gg:8 + 4 * gg], scalar=OHG[:, gg:gg + 1], in1=ES,
                                                               op0=ALU.mult, op1=ALU.add))
                R_(lambda e: e.tensor_reduce(out=M1, in_=ES, axis=AX.X, op=ALU.max))
                R_(lambda e: e.tensor_scalar(out=OH1, in0=ES, scalar1=M1, scalar2=None, op0=ALU.is_equal))
                R_(lambda e: e.scalar_tensor_tensor(out=ES2, in0=OH1, scalar=-1e30, in1=ES, op0=ALU.mult, op1=ALU.add))
                R_(lambda e: e.tensor_reduce(out=M2, in_=ES2, axis=AX.X, op=ALU.max))
                R_(lambda e: e.tensor_scalar(out=OH2, in0=ES2, scalar1=M2, scalar2=None, op0=ALU.is_equal))
                R_(lambda e: e.tensor_tensor(out=DD, in0=M1, in1=M2, op=ALU.subtract))
                R_(lambda e: e.activation(out=W2, in_=DD, func=AF.Exp, scale=-1.0), 'act')
                R_(lambda e: e.tensor_scalar(out=W1, in0=W2, scalar1=1.0, scalar2=None, op0=ALU.add))
                R_(lambda e: e.reciprocal(out=W1, in_=W1))
                R_(lambda e: e.tensor_tensor(out=W2, in0=W2, in1=W1, op=ALU.mult))
                R_(lambda e: e.tensor_tensor(out=W1, in0=W1, in1=PG, op=ALU.mult))
                R_(lambda e: e.tensor_tensor(out=W2, in0=W2, in1=PG, op=ALU.mult))
                R_(lambda e: e.tensor_scalar(out=CW4, in0=OH1, scalar1=W1, scalar2=None, op0=ALU.mult))
                R_(lambda e: e.scalar_tensor_tensor(out=CW4, in0=OH2, scalar=W2, in1=CW4, op0=ALU.mult, op1=ALU.add))
                S.op('dve', lambda e: e.tensor_copy(out=vtk[:, 1024:1032].bitcast(F32), in_=CW4), R=[rtk], W=[vtkk])
                S.op('dve', lambda e: e.tensor_copy(out=cw[:, 0:4], in_=OHG), R=[rtk], W=[cwk])

            route(rt, rtk, cw, cwk, pl, plk, vtk, vtkk)
            S.dma('sp', g.VTK[rows, :], vtk[:], R=[vtkk])
            S.dma('sp', g.GOH[rows, :], cw[:, 0:4], R=[cwk])
            if g.RTD is not None:
                S.dma('sp', g.RTD[rows, :], rt[:], R=[rtk])
            S.rec = None
            iters.append([st0, st1, st2, st3, st4])
        S.pipeline(iters)
        S.barrier()
        S.flush()


def phase_moe_sparse(g, l):
    nc, S = g.nc, g.S
    last = (l == DEPTH - 1)
    jstart = 2 if last else 0
    with ExitStack() as st:
        sbt = lambda name, shape, dt=F32: st.enter_context(nc.sbuf_tensor(name + Ring.suffix, list(shape), dt))
        posI = sbt('sPOSI', [128, NT], I32)
        widxI = sbt('sWIDX', [128, SL * 4], I32)
        g.epsc = sbt('epsc4', [128, 1])
        S.op('dve', lambda e: e.memset(g.epsc[:], EPS), W=['epsc'])
        with ExitStack() as st1:
            sb1 = lambda name, shape, dt=F32: st1.enter_context(nc.sbuf_tensor(name + Ring.suffix, list(shape), dt))
            N4 = NT * 4
            tri = sb1('sTRI', [128, 128])
            cst = sb1('sCST', [128, 32])
            goh = sb1('sGOH', [128, N4])
            cum = sb1('sCUM', [128, N4])
            totb = sb1('sTOT', [128, N4])
            incl = sb1('sINC', [128, N4])
            onesr = sb1('sONE', [128, NT])
            ng = sb1('sNG', [128, 4])
            cmp9 = sb1('sCMP', [128, 16])
            ns = sb1('sNS', [128, 4])
            gst = sb1('sGST', [128, 4])
            posf = sb1('sPOSF', [128, NT])
            gidf = sb1('sGID', [128, SL])
            gtmp = sb1('sGTMP', [128, SL])
            widxf = sb1('sWIDXF', [128, SL * 4])
            S.dma('sp', tri[:], g.tri[:, :], W=['sTRI'])
            S.dma('sp', cst[:], g.cst[:, :], W=['sCST'])
            S.dma('sp', goh[:].rearrange("p (j q) -> p j q", q=4), g.GOH[:, :].rearrange("(j p) q -> p j q", p=128), W=['sGOH'])
            if last:
                S.op('dve', lambda e: e.memset(goh[:, 0:8], 0.0), R=['sGOH'], W=['sGOH'])
            S.op('dve', lambda e: e.memset(onesr[:], 1.0), W=['sONE'])
            p1, p1k = g.PS[0], 'ps0'
            p2, p2k = g.PS[1], 'ps1'
            S.op('pe', lambda e: e.matmul(p1[:, 0:N4], lhsT=tri[:], rhs=goh[:], start=True, stop=True), R=['sTRI', 'sGOH'], W=[p1k])
            S.op('pe', lambda e: e.matmul(p2[:, 0:N4], lhsT=g.onesf[:], rhs=goh[:], start=True, stop=True), R=['onesf', 'sGOH'], W=[p2k])
            S.op('act', lambda e: e.copy(out=cum[:], in_=p1[:, 0:N4]), R=[p1k], W=['sCUM'])
            S.op('act', lambda e: e.copy(out=totb[:], in_=p2[:, 0:N4]), R=[p2k], W=['sTOT'])
            for q in range(4):
                S.op('dve', lambda e, q=q: e.tensor_tensor_scan(out=incl[:, q:N4:4], data0=onesr[:], data1=totb[:, q:N4:4], initial=0.0,
                                                                op0=ALU.mult, op1=ALU.add), R=['sONE', 'sTOT'], W=['sINC'])
            S.op('dve', lambda e: e.tensor_copy(out=ng[:], in_=incl[:, N4 - 4:N4]), R=['sINC'], W=['sNG'])
            for q in range(4):
                S.op('dve', lambda e, q=q: e.tensor_scalar(out=cmp9[:, 0:9], in0=cst[:, 0:9], scalar1=ng[:, q:q + 1], scalar2=None, op0=ALU.is_lt),
                     R=['sCST', 'sNG'], W=['sCMP'])
                S.op('dve', lambda e, q=q: e.tensor_reduce(out=ns[:, q:q + 1], in_=cmp9[:, 0:9], axis=AX.X, op=ALU.add), R=['sCMP'], W=['sNS'])
            S.op('dve', lambda e: e.tensor_scalar(out=ns[:], in0=ns[:], scalar1=512.0, scalar2=None, op0=ALU.mult), R=['sNS'], W=['sNS'])
            S.op('dve', lambda e: e.memset(gst[:, 0:1], 0.0), W=['sGST'])
            for q in range(1, 4):
                S.op('dve', lambda e, q=q: e.tensor_tensor(out=gst[:, q:q + 1], in0=gst[:, q - 1:q], in1=ns[:, q - 1:q], op=ALU.add),
                     R=['sGST', 'sNS'], W=['sGST'])
            S.op('dve', lambda e: e.tensor_tensor(out=incl[:], in0=incl[:], in1=totb[:], op=ALU.subtract), R=['sINC', 'sTOT'], W=['sINC'])
            S.op('dve', lambda e: e.tensor_tensor(out=cum[:], in0=cum[:], in1=incl[:], op=ALU.add), R=['sCUM', 'sINC'], W=['sCUM'])
            for q in range(4):
                S.op('dve', lambda e, q=q: e.tensor_scalar(out=cum[:, q:N4:4], in0=cum[:, q:N4:4], scalar1=gst[:, q:q + 1], scalar2=-1.0,
                                                           op0=ALU.add, op1=ALU.add), R=['sCUM', 'sGST'], W=['sCUM'])
            S.op('dve', lambda e: e.tensor_tensor(out=cum[:], in0=cum[:], in1=goh[:], op=ALU.mult), R=['sCUM', 'sGOH'], W=['sCUM'])
            S.op('dve', lambda e: e.tensor_reduce(out=posf[:], in_=cum[:].rearrange("p (j q) -> p j q", q=4), axis=AX.X, op=ALU.add),
                 R=['sCUM'], W=['sPOSF'])
            S.op('dve', lambda e: e.tensor_copy(out=posI[:], in_=posf[:]), R=['sPOSF'], W=['sPOSI'])
            S.op('dve', lambda e: e.memset(gidf[:], 0.0), W=['sGID'])
            for q in range(1, 4):
                S.op('dve', lambda e, q=q: e.tensor_scalar(out=gtmp[:], in0=cst[:, 9:9 + SL], scalar1=gst[:, q:q + 1], scalar2=None, op0=ALU.is_ge),
                     R=['sCST', 'sGST'], W=['sGTMP'])
                S.op('dve', lambda e: e.tensor_tensor(out=gidf[:], in0=gidf[:], in1=gtmp[:], op=ALU.add), R=['sGID', 'sGTMP'], W=['sGID'])
            for q in range(4):
                S.op('dve', lambda e, q=q: e.tensor_scalar(out=widxf[:, q:SL * 4:4], in0=gidf[:], scalar1=512.0, scalar2=cst[:, 22 + q:23 + q],
                                                           op0=ALU.mult, op1=ALU.add), R=['sGID', 'sCST'], W=['sWIDXF'])
            S.op('dve', lambda e: e.tensor_copy(out=widxI[:], in_=widxf[:]), R=['sWIDXF'], W=['sWIDX'])
            VL = Ring(nc, st1, 'sVL', [128, 1032], BF16, 8)
            for j in range(jstart, NT):
                vl, vlk = VL.next()
                S.dma('sp', vl[:], g.VTK[128 * j:128 * j + 128, :], W=[vlk])
                S.dma_fn('pool', lambda e, vl=vl, j=j: e.indirect_dma_start(
                    out=g.VS[:, :], out_offset=bass.IndirectOffsetOnAxis(ap=posI[:, j:j + 1], axis=0), in_=vl[:, :], in_offset=None,
                    bounds_check=S.reg(NCAP - 1), oob_is_err=False), R=[vlk, 'sPOSI'], W=[])
            S.barrier()
            S.flush()
        with ExitStack() as st2:
            VSr = Ring(nc, st2, 'sVS', [128, 1032], BF16, 8)
            VTr = Ring(nc, st2, 'sVT', [128, 8 * 512], BF16, 4)
            CWr = Ring(nc, st2, 'sCW', [128, 16], F32, 3)
            WW = Ring(nc, st2, 'sW', [128, 12288], BF16, 3)
            SG = Ring(nc, st2, 'sSG', [128, 512], F32, 3)
            AT = Ring(nc, st2, 'sAT', [128, 4 * 512], BF16, 3)
            YA = Ring(nc, st2, 'sYA', [128, 4 * 1024], F32, 2)
            iters = []
            cnt = [0, 0, 0, 0]
            for sl in range(SL - 1 if last else SL):
                s0, s1, s2 = [], [], []
                S.rec = s0
                vts = []
                for ti in range(4):
                    vs, vsk = VSr.next()
                    r0 = sl * 512 + ti * 128
                    S.dma('sp', vs[:], g.VS[r0:r0 + 128, :], W=[vsk])
                    vts.append((vs, vsk))
                S.rec = s1
                vT, vtk_ = VTr.next()
                cwr, cwrk = CWr.next()
                for ti in range(4):
                    vs, vsk = vts[ti]
                    pb_ = 0
                    pt_, ptk_ = g.PS[pb_], 'ps%d' % pb_
                    ptb = pt_[:].bitcast(BF16)

                    def ftr(e, vs=vs, ptb=ptb):
                        for k in range(8):
                            ins = e.transpose(out=ptb[:, k * 128:(k + 1) * 128], in_=vs[:, k * 128:(k + 1) * 128], identity=g.idb[:])
                        return ins
                    S.op('pe', ftr, R=[vsk, 'idb'], W=[ptk_])
                    eng = 'act' if ti % 2 == 0 else 'dve'
                    if eng == 'act':
                        S.op('act', lambda e, vT=vT, ptb=ptb, ti=ti: e.copy(
                            out=vT[:].rearrange("p (k n) -> p k n", k=8)[:, :, ti * 128:(ti + 1) * 128],
                            in_=ptb[:, 0:1024].rearrange("p (k n) -> p k n", k=8)), R=[ptk_], W=[vtk_])
                    else:
                        S.op('dve', lambda e, vT=vT, ptb=ptb, ti=ti: e.tensor_copy(
                            out=vT[:].rearrange("p (k n) -> p k n", k=8)[:, :, ti * 128:(ti + 1) * 128],
                            in_=ptb[:, 0:1024].rearrange("p (k n) -> p k n", k=8)), R=[ptk_], W=[vtk_])
                    S.op('act', lambda e, cwr=cwr, vs=vs, ti=ti: e.copy(out=cwr[:, ti * 4:(ti + 1) * 4], in_=vs[:, 1024:1032].bitcast(F32)),
                         R=[vsk], W=[cwrk])
                S.rec = s2
                ya, yak = YA.next()
                for q in range(4):
                    W, wk = WW.next()
                    S.dma_fn('pool', lambda e, W=W, sl=sl, q=q: e.indirect_dma_start(
                        out=W[:, :], out_offset=None, in_=g.WB[:, :],
                        in_offset=bass.IndirectOffsetOnAxis(ap=widxI[:, sl * 4 + q:sl * 4 + q + 1], axis=0),
                        bounds_check=S.reg(NE * 128 - 1), oob_is_err=False), R=['sWIDX'], W=[wk])
                    at, atk = AT.next()
                    for c in range(4):
                        pa, pak = g.PS[1 + cnt[1] % 2], 'ps%d' % (1 + cnt[1] % 2)
                        pb, pbk = g.PS[3 + cnt[1] % 2], 'ps%d' % (3 + cnt[1] % 2)
                        cnt[1] += 1

                        def fgu(e, pa=pa, pb=pb, W=W, c=c, vT=vT):
                            for k in range(8):
                                e.matmul(pa[:, :], lhsT=W[:, k * 512 + c * 128:k * 512 + (c + 1) * 128],
                                         rhs=vT[:, k * 512:(k + 1) * 512], start=(k == 0), stop=(k == 7))
                            for k in range(8):
                                ins = e.matmul(pb[:, :], lhsT=W[:, 4096 + k * 512 + c * 128:4096 + k * 512 + (c + 1) * 128],
                                               rhs=vT[:, k * 512:(k + 1) * 512], start=(k == 0), stop=(k == 7))
                            return ins
                        S.op('pe', fgu, R=[wk, vtk_], W=[pak, pbk])
                        sg, sgk = SG.next()
                        S.op('act', lambda e, sg=sg, pa=pa: e.activation(out=sg[:], in_=pa[:, :], func=AF.Silu), R=[pak], W=[sgk])
                        S.op('dve', lambda e, at=at, sg=sg, pb=pb, c=c: e.tensor_tensor(
                            out=at[:, c * 512:(c + 1) * 512], in0=pb[:, :], in1=sg[:], op=ALU.mult), R=[pbk, sgk], W=[atk])
                    for ts in range(4):
                        for half in range(2):
                            pd, pdk = g.PS[5 + cnt[2] % 3], 'ps%d' % (5 + cnt[2] % 3)
                            cnt[2] += 1

                            def fd(e, pd=pd, at=at, W=W, ts=ts, half=half):
                                for kc in range(4):
                                    ins = e.matmul(pd[:, :], lhsT=at[:, kc * 512 + ts * 128:kc * 512 + (ts + 1) * 128],
                                                   rhs=W[:, 8192 + kc * 1024 + half * 512:8192 + kc * 1024 + (half + 1) * 512],
                                                   start=(kc == 0), stop=(kc == 3))
                                return ins
                            S.op('pe', fd, R=[atk, wk], W=[pdk])
                            yv = ya[:, ts * 1024 + half * 512:ts * 1024 + (half + 1) * 512]
                            cwc = cwr[:, ts * 4 + q:ts * 4 + q + 1]
                            if q == 0:
                                S.op('act', lambda e, yv=yv, pd=pd, cwc=cwc: e.activation(out=yv, in_=pd[:, :], func=AF.Copy, scale=cwc),
                                     R=[pdk, cwrk], W=[yak])
                            else:
                                S.op('dve', lambda e, yv=yv, pd=pd, cwc=cwc: e.scalar_tensor_tensor(out=yv, in0=pd[:, :], scalar=cwc, in1=yv, op0=ALU.mult, op1=ALU.add),
                                     R=[pdk, cwrk, yak], W=[yak])
                S.dma('sp', g.YS[sl * 512:(sl + 1) * 512, :].rearrange("(t p) n -> p t n", p=128),
                      ya[:].rearrange("p (t n) -> p t n", t=4), R=[yak])
                S.rec = None
                iters.append([s0, s1, s2])
            S.pipeline(iters)
            S.barrier()
            S.flush()
        with ExitStack() as st3:
            YG = Ring(nc, st3, 'sYG', [128, 1024], F32, 5)
            HT = Ring(nc, st3, 'sHT', [128, 1024], F32, 5)
            TM = Ring(nc, st3, 'sTM', [128, 1024], F32, 4)
            HN = Ring(nc, st3, 'sHN', [128, 1024], F32, 4)
            junk = st3.enter_context(nc.sbuf_tensor('sjunk' + Ring.suffix, [128, 1024], BF16))
            SSQ = Ring(nc, st3, 'sSSQ', [128, 1], F32, 4)
            RSTD = Ring(nc, st3, 'sRSTD', [128, 1], F32, 4)
            iters = []
            for j in range(jstart, NT):
                e0, e1, e2 = [], [], []
                S.rec = e0
                s = 1 if j < 2 else 0
                yg, ygk = YG.next()
                S.dma_fn('pool', lambda e, yg=yg, j=j: e.indirect_dma_start(
                    out=yg[:, :], out_offset=None, in_=g.YS[:, :],
                    in_offset=bass.IndirectOffsetOnAxis(ap=posI[:, j:j + 1], axis=0),
                    bounds_check=S.reg(NCAP - 1), oob_is_err=False), R=['sPOSI'], W=[ygk])
                ht, htk = HT.next()
                load_h_tile(g, l, j, ht, htk, False)
                S.rec = e1
                tm, tmk = TM.next()
                hn, hnk = HN.next()
                g2b = bcast_gate(g, 1, s)
                S.op('dve', lambda e, tm=tm, yg=yg, g2b=g2b: e.tensor_tensor(out=tm[:], in0=yg[:], in1=g2b, op=ALU.mult),
                     R=[ygk, 'gateb'], W=[tmk])
                S.op('dve', lambda e, hn=hn, tm=tm, ht=ht: e.tensor_tensor(out=hn[:], in0=tm[:], in1=ht[:], op=ALU.add), R=[tmk, htk], W=[hnk])
                if not last:
                    for (rs_, p0, npart) in tile_rows(l, j):
                        S.dma('sp', g.Hs[rs_, :], hn[p0:p0 + npart, :], R=[hnk])
                else:
                    S.rec = e2
                    sq, sqk = SSQ.next()
                    rstd, rstdk = RSTD.next()
                    rms_rstd(g, hn, hnk, junk, 'sjunk', sq, sqk, rstd, rstdk)
                    S.op('dve', lambda e, tm=tm, hn=hn, rstd=rstd: e.scalar_tensor_tensor(
                        out=tm[:], in0=hn[:], scalar=rstd[:, 0:1], in1=g.fgb[:], op0=ALU.mult, op1=ALU.mult), R=[hnk, rstdk, 'fgb'], W=[tmk])
                    for (rs_, p0, npart) in tile_rows(l, j):
                        ro = slice(rs_.start - CTX, rs_.stop - CTX, rs_.step)
                        S.dma('sp', g.out[ro, :], tm[p0:p0 + npart, :], R=[tmk])
                S.rec = None
                iters.append([e0, e1, e2])
            S.pipeline(iters)
            S.barrier()
            S.flush()


def col128(v):
    v = np.asarray(v, np.float32)
    return np.ascontiguousarray(v.reshape(-1, 128).T)


def make_pf(inp, b):
    pf = np.zeros((128, NPF), np.float32)

    def put(name, arr):
        o, n = PF[name]
        arr = np.asarray(arr, np.float32).reshape(128, -1)
        assert arr.shape[1] == n, (name, arr.shape, n)
        pf[:, o:o + n] = arr
    c2 = np.stack([col128(inp['c'][b]), col128(inp['c_ctx'])], axis=-1)
    put('c2', c2)
    put('b_mod', np.stack([col128(inp['b_mod'][l]) for l in range(DEPTH)], axis=1))
    put('g1', np.stack([col128(inp['norm1_g'][l]) for l in range(DEPTH)], axis=1))
    put('g2', np.stack([col128(inp['norm2_g'][l]) for l in range(DEPTH)], axis=1))
    put('fg', col128(inp['final_g']))
    cw = np.zeros((128, DEPTH, 2, 31), np.float32)
    for l in range(DEPTH):
        for c in range(2):
            cw[:, l, c, :] = inp['conv_w'][l][:, c * 128:(c + 1) * 128].T
    put('conv_w', cw)
    for nm, key in [('conv_b', 'conv_b'), ('conv_ln_g', 'conv_ln_g'), ('conv_ln_b', 'conv_ln_b')]:
        put(nm, np.stack([col128(inp[key][l]) for l in range(DEPTH)], axis=1))
    put('mnorm_g', np.stack([col128(inp['mlstm_norm_g'][l]) for l in range(DEPTH)], axis=1))
    lcw = np.zeros((128, DEPTH, 2, 2, 4), np.float32)
    for l in range(DEPTH):
        for d in range(2):
            for c in range(2):
                lcw[:, l, d, c, :] = inp['lru_conv_w'][l, d][:, c * 128:(c + 1) * 128].T
    put('lru_cw', lcw)
    for nm, key in [('lru_cb', 'lru_conv_b'), ('lru_ba', 'lru_b_a'), ('lru_bx', 'lru_b_x'), ('lru_lam', 'lru_lambda')]:
        a = np.zeros((128, DEPTH, 2, 2), np.float32)
        for l in range(DEPTH):
            for d in range(2):
                a[:, l, d, :] = col128(inp[key][l, d])
        put(nm, a)
    brb = np.zeros((128, DEPTH, 20), np.float32)
    for l in range(DEPTH):
        brb[:, l, 0:4] = inp['b_rg'][l][None, :]
        brb[:, l, 4:20] = inp['b_re'][l][None, :]
    put('brb', brb)
    gb = np.zeros((128, DEPTH, 2), np.float32)
    for l in range(DEPTH):
        for d in range(2):
            gb[32 * d:32 * d + 4, l, 0] = inp['mlstm_b_i'][l, d]
            gb[32 * d:32 * d + 4, l, 1] = inp['mlstm_b_f'][l, d]
    put('gb', gb)
    return pf


def make_shared(inp):
    sh = {}
    sh['ident'] = np.eye(128, dtype=np.float32)
    sh['tri'] = np.triu(np.ones((128, 128), np.float32))
    cst = np.zeros((128, 32), np.float32)
    cst[:, 0:9] = 512.0 * np.arange(9)[None, :]
    cst[:, 9:9 + SL] = 512.0 * np.arange(SL)[None, :]
    for e in range(4):
        cst[:, 22 + e] = e * 128 + np.arange(128)
    sh['cst'] = cst
    for k in ['w_mod', 'w_in', 'w_out', 'w_gate', 'w_up', 'w_down']:
        sh[k] = np.ascontiguousarray(np.asarray(inp[k], np.float32))
    lw = np.zeros((128, DEPTH, 2, 2, 2, 128), np.float32)
    for l in range(DEPTH):
        for d in range(2):
            for gi, key in enumerate(['lru_w_a', 'lru_w_x']):
                for c in range(2):
                    for a in range(2):
                        lw[64 * a:64 * a + 64, l, d, gi, c, 64 * a:64 * a + 64] = inp[key][l, d, 2 * c + a]
    sh['lruW'] = lw.reshape(128, -1)
    wr = np.zeros((128, DEPTH, 8, 20), np.float32)
    for l in range(DEPTH):
        wcat = np.concatenate([inp['w_rg'][l], inp['w_re'][l]], axis=1)
        wr[:, l] = wcat.reshape(8, 128, 20).transpose(1, 0, 2)
    sh['wr'] = wr.reshape(128, -1)
    return sh


def kernel(**inp):
    inp = {k: np.asarray(v) for k, v in inp.items()}
    nc = build()
    sh = make_shared(inp)
    in_maps = []
    for b in range(8):
        m = dict(sh)
        m['x'] = np.ascontiguousarray(inp['x'][b], np.float32)
        m['ctx'] = np.ascontiguousarray(inp['ctx'][b], np.float32)
        m['pf'] = make_pf(inp, b)
        in_maps.append(m)
    res = run_bass_kernel_spmd(nc, in_maps, core_ids=list(range(8)))
    return np.stack([np.asarray(r['out'], np.float32) for r in res.results], axis=0)
```
